# Optimizing a Trainium2 kernel written in Bass

```python
import math, functools
import jax, jax.numpy as jnp
from jax import lax
import numpy as np

D_MODEL = 1024
BATCH = 4
SEQ = 8192
DEPTH = 4

CONV_CH = 512
N_HEADS = 8
HEAD_DIM = 64
ATTN_CH = N_HEADS * HEAD_DIM
D_MIX = CONV_CH + ATTN_CH
D_IN = 2 * CONV_CH + 3 * ATTN_CH
CONV_K = 31
GRID_W = 64
WIN_R = 8
WIN_C = 16
Q_CB = 16
N_CB = GRID_W // Q_CB
BAND_C = Q_CB + WIN_C
D_FF = 2816
N_EXPERTS = 8
TOP_K = 2
D_FF_EXPERT = 3584
MOE_BLK = 128
N_DENSE = (DEPTH + 1) // 2
N_MOE = DEPTH // 2
D_PLE = 256
LN_EPS = 1e-5
DEEPNORM_ALPHA = (2.0 * DEPTH) ** 0.25
DEEPNORM_BETA = (8.0 * DEPTH) ** -0.25

kernel_name = "hybrid_conformer_natten_moe_deepnorm_encoder"


def layer_norm(x, g, b):
    xf = x.astype(jnp.float32)
    mu = jnp.mean(xf, axis=-1, keepdims=True)
    xc = xf - mu
    var = jnp.mean(xc * xc, axis=-1, keepdims=True)
    y = xc * lax.rsqrt(var + LN_EPS)
    return (y * g.astype(jnp.float32) + b.astype(jnp.float32)).astype(x.dtype)


def conformer_conv(a, g, w, b, ln_g, ln_b):
    u = a * jax.nn.sigmoid(g)
    c = u.shape[-1]
    pad = CONV_K // 2
    y = lax.conv_general_dilated(
        u, w[:, None, :], window_strides=(1,), padding=[(pad, pad)],
        dimension_numbers=("NWC", "WIO", "NWC"), feature_group_count=c)
    y = layer_norm(y + b, ln_g, ln_b)
    return jax.nn.silu(y)


def neighbourhood_attention(q, k, v, rpb):
    B, T, H, Dh = q.shape
    rows = T // GRID_W
    kr = min(WIN_R, rows)
    scale = Dh ** -0.5
    band0 = np.clip(np.arange(N_CB) * Q_CB - WIN_C // 2, 0, GRID_W - BAND_C)
    key_col = band0[:, None] + np.arange(BAND_C)
    q_col = np.arange(GRID_W).reshape(N_CB, Q_CB)
    win0 = np.clip(q_col - WIN_C // 2, 0, GRID_W - WIN_C)
    col_mask = ((key_col[:, None, :] >= win0[:, :, None]) &
                (key_col[:, None, :] < win0[:, :, None] + WIN_C))
    dc_idx = np.clip(key_col[:, None, :] - q_col[:, :, None] + WIN_C - 1, 0, 2 * WIN_C - 2)
    rpb_c = rpb[:, :, dc_idx]
    kg = k.reshape(B, rows, GRID_W, H, Dh)
    vg = v.reshape(B, rows, GRID_W, H, Dh)
    qr = jnp.moveaxis(q.reshape(B, rows, N_CB, Q_CB, H, Dh), 1, 0)

    def row_fn(args):
        r, q_row = args
        r0 = jnp.clip(r - kr // 2, 0, rows - kr)
        k_band = lax.dynamic_slice_in_dim(kg, r0, kr, axis=1)[:, :, key_col]
        v_band = lax.dynamic_slice_in_dim(vg, r0, kr, axis=1)[:, :, key_col]
        s = jnp.einsum("bjqhd,bijkhd->bhjqik", q_row, k_band).astype(jnp.float32) * scale
        dr_idx = r0 + jnp.arange(kr) - r + WIN_R - 1
        bias = jnp.transpose(jnp.take(rpb_c, dr_idx, axis=1), (0, 2, 3, 1, 4))
        s = jnp.where(col_mask[:, :, None, :], s + bias.astype(jnp.float32), -jnp.inf)
        w = jax.nn.softmax(s.reshape(B, H, N_CB, Q_CB, kr * BAND_C), axis=-1)
        w = w.reshape(s.shape).astype(v.dtype)
        return jnp.einsum("bhjqik,bijkhd->bjqhd", w, v_band)

    out = lax.map(row_fn, (jnp.arange(rows), qr))
    return jnp.moveaxis(out, 0, 1).reshape(B, T, H * Dh)


def swiglu(x, w1, w3, w2):
    return (jax.nn.silu(x @ w1) * (x @ w3)) @ w2


def moe_swiglu(x, w_router, w1, w3, w2):
    B, T, D = x.shape
    xf = x.reshape(-1, D)
    n = xf.shape[0]
    logits = (xf @ w_router).astype(jnp.float32)
    top_vals, top_idx = lax.top_k(logits, TOP_K)
    gates = jax.nn.softmax(top_vals, axis=-1)
    n_assign = n * TOP_K
    e_flat = top_idx.reshape(-1)
    tok_flat = jnp.repeat(jnp.arange(n, dtype=jnp.int32), TOP_K)
    g_flat = gates.reshape(-1)
    order = jnp.argsort(e_flat)
    e_sorted = e_flat[order]
    tok_sorted = tok_flat[order]
    g_sorted = g_flat[order]
    counts = jnp.bincount(e_flat, length=N_EXPERTS)
    starts = jnp.cumsum(counts) - counts
    padded = (counts + MOE_BLK - 1) // MOE_BLK * MOE_BLK
    pends = jnp.cumsum(padded)
    pstarts = pends - padded
    dest = pstarts[e_sorted] + (jnp.arange(n_assign) - starts[e_sorted])
    n_blocks = -(-n_assign // MOE_BLK) + N_EXPERTS
    n_slots = n_blocks * MOE_BLK
    tok_buf = jnp.zeros((n_slots,), jnp.int32).at[dest].set(tok_sorted)
    gate_buf = jnp.zeros((n_slots,), jnp.float32).at[dest].set(g_sorted)
    block_e = jnp.minimum(
        jnp.searchsorted(pends, jnp.arange(n_blocks) * MOE_BLK, side="right"), N_EXPERTS - 1)

    def block_fn(args):
        tok, e = args
        xb = xf[tok]
        return (jax.nn.silu(xb @ w1[e]) * (xb @ w3[e])) @ w2[e]

    y = lax.map(block_fn, (tok_buf.reshape(n_blocks, MOE_BLK), block_e)).reshape(n_slots, D)
    y = y * gate_buf[:, None].astype(y.dtype)
    out = jax.ops.segment_sum(y, tok_buf, num_segments=n)
    return out.reshape(B, T, D)


def setup_inputs(seed: int = 0) -> dict:
    key = jax.random.key(seed)
    ks = jax.random.split(key, 32)
    f32 = jnp.float32
    nrm = lambda k, shape, s: jax.random.normal(k, shape, f32) * s
    gain = lambda k, shape: 1.0 + 0.02 * jax.random.normal(k, shape, f32)
    return {
        "x": jax.random.normal(ks[0], (BATCH, SEQ, D_MODEL), f32),
        "p": jax.random.normal(ks[1], (DEPTH, BATCH, SEQ, D_PLE), f32),
        "ln_in_g": gain(ks[2], (D_MODEL,)),
        "ln_in_b": nrm(ks[3], (D_MODEL,), 0.02),
        "w_in": nrm(ks[4], (DEPTH, D_MODEL, D_IN), D_MODEL ** -0.5),
        "b_in": nrm(ks[5], (DEPTH, D_IN), 0.02),
        "conv_w": nrm(ks[6], (DEPTH, CONV_K, CONV_CH), CONV_K ** -0.5),
        "conv_b": nrm(ks[7], (DEPTH, CONV_CH), 0.02),
        "conv_ln_g": gain(ks[8], (DEPTH, CONV_CH)),
        "conv_ln_b": nrm(ks[9], (DEPTH, CONV_CH), 0.02),
        "rpb": nrm(ks[10], (DEPTH, N_HEADS, 2 * WIN_R - 1, 2 * WIN_C - 1), 0.02),
        "w_out": nrm(ks[11], (DEPTH, D_MIX, D_MODEL), D_MIX ** -0.5 * DEEPNORM_BETA),
        "b_out": nrm(ks[12], (DEPTH, D_MODEL), 0.02),
        "ln1_g": gain(ks[13], (DEPTH, D_MODEL)),
        "ln1_b": nrm(ks[14], (DEPTH, D_MODEL), 0.02),
        "ffn_w1": nrm(ks[15], (N_DENSE, D_MODEL, D_FF), D_MODEL ** -0.5),
        "ffn_w3": nrm(ks[16], (N_DENSE, D_MODEL, D_FF), D_MODEL ** -0.5),
        "ffn_w2": nrm(ks[17], (N_DENSE, D_FF, D_MODEL), D_FF ** -0.5 * DEEPNORM_BETA),
        "w_router": nrm(ks[18], (N_MOE, D_MODEL, N_EXPERTS), D_MODEL ** -0.5),
        "moe_w1": nrm(ks[19], (N_MOE, N_EXPERTS, D_MODEL, D_FF_EXPERT), D_MODEL ** -0.5),
        "moe_w3": nrm(ks[20], (N_MOE, N_EXPERTS, D_MODEL, D_FF_EXPERT), D_MODEL ** -0.5),
        "moe_w2": nrm(ks[21], (N_MOE, N_EXPERTS, D_FF_EXPERT, D_MODEL), D_FF_EXPERT ** -0.5 * DEEPNORM_BETA),
        "ple_w": nrm(ks[22], (DEPTH, D_PLE, D_MODEL), D_PLE ** -0.5 * DEEPNORM_BETA),
        "ple_gate_w": nrm(ks[23], (DEPTH, D_MODEL, D_MODEL), D_MODEL ** -0.5),
        "ple_gate_b": nrm(ks[24], (DEPTH, D_MODEL), 0.02),
        "ln2_g": gain(ks[25], (DEPTH, D_MODEL)),
        "ln2_b": nrm(ks[26], (DEPTH, D_MODEL), 0.02),
    }


def reference(x, p, ln_in_g, ln_in_b, w_in, b_in, conv_w, conv_b, conv_ln_g, conv_ln_b,
              rpb, w_out, b_out, ln1_g, ln1_b, ffn_w1, ffn_w3, ffn_w2, w_router,
              moe_w1, moe_w3, moe_w2, ple_w, ple_gate_w, ple_gate_b, ln2_g, ln2_b):
    B, T, _ = x.shape
    h = layer_norm(x, ln_in_g, ln_in_b)
    for i in range(DEPTH):
        proj = h @ w_in[i] + b_in[i]
        a = proj[..., :CONV_CH]
        g = proj[..., CONV_CH:2 * CONV_CH]
        qkv = proj[..., 2 * CONV_CH:].reshape(B, T, 3, N_HEADS, HEAD_DIM)
        conv_out = conformer_conv(a, g, conv_w[i], conv_b[i], conv_ln_g[i], conv_ln_b[i])
        attn_out = neighbourhood_attention(qkv[:, :, 0], qkv[:, :, 1], qkv[:, :, 2], rpb[i])
        mix = jnp.concatenate([conv_out, attn_out], axis=-1) @ w_out[i] + b_out[i]
        h = layer_norm(DEEPNORM_ALPHA * h + mix, ln1_g[i], ln1_b[i])
        j = i // 2
        if i % 2 == 0:
            f = swiglu(h, ffn_w1[j], ffn_w3[j], ffn_w2[j])
        else:
            f = moe_swiglu(h, w_router[j], moe_w1[j], moe_w3[j], moe_w2[j])
        e = jax.nn.sigmoid(h @ ple_gate_w[i] + ple_gate_b[i]) * (p[i] @ ple_w[i])
        h = layer_norm(DEEPNORM_ALPHA * h + f + e, ln2_g[i], ln2_b[i])
    return h
```

```python
import os
import numpy as np
from contextlib import ExitStack
import concourse.bass as bass
import concourse.mybir as mybir
from concourse.bass_utils import run_bass_kernel_spmd

F32 = mybir.dt.float32
BF16 = mybir.dt.bfloat16
AF = mybir.ActivationFunctionType
ALU = mybir.AluOpType
AX = mybir.AxisListType

DEPTH = 4
D = 1024
DIN = 2560
NEXP = 8
FF = 2816
FFE = 3584
DPLE = 256
ALPHA = (2.0 * DEPTH) ** 0.25
EPS = 1e-5
MASKV = -200.0
CAP = 1536
NCH = CAP // 128
NV = 220
C_BIN, C_CW, C_CB, C_CG, C_CBB, C_BOUT, C_L1G, C_L1B, C_PGB, C_L2G, C_L2B, C_LIG, C_LIB = (
    0, 20, 144, 148, 152, 156, 164, 172, 180, 188, 196, 204, 212)


class Trk:
    __slots__ = ("w", "r", "dsem", "dcnt", "const")

    def __init__(self, const=False):
        self.w = None
        self.r = []
        self.dsem = None
        self.dcnt = 0
        self.const = const


class Tl:
    def __init__(self, t, const=False):
        self.t = t
        self.k = Trk(const)

    def __getitem__(self, key):
        return self.t[key]


class Eng:
    def __init__(self, name, eng, sem):
        self.name = name
        self.eng = eng
        self.sem = sem
        self.cnt = 0
        self.seen = {}


class KB:
    def __init__(self, nc, es):
        self.nc = nc
        self.es = es
        self.sems = []
        mk = lambda n: es.enter_context(nc.semaphore(n))
        self.pe = Eng("pe", nc.tensor, mk("s_pe"))
        self.act = Eng("act", nc.scalar, mk("s_act"))
        self.dve = Eng("dve", nc.vector, mk("s_dve"))
        self.pool = Eng("pool", nc.gpsimd, mk("s_pool"))
        self.sp = Eng("sp", nc.sync, mk("s_sp"))
        self.engs = [self.pe, self.act, self.dve, self.pool, self.sp]
        self.dsems = []
        self.free_dsems = []
        self.nsem = 0
        self.phase_tiles = []

    def sb(self, es, name, shape, dt, const=False):
        self.uid = getattr(self, "uid", 0) + 1
        t = Tl(es.enter_context(self.nc.sbuf_tensor("%s_%d" % (name, self.uid), shape, dt)), const)
        if es is not self.es:
            self.phase_tiles.append(t)
        return t

    def ps(self, es, name, shape, dt):
        return Tl(es.enter_context(self.nc.psum_tensor(name, shape, dt)))

    def _dsem(self, trk):
        if trk.dsem is None:
            if self.free_dsems:
                trk.dsem, trk.dcnt = self.free_dsems.pop()
            else:
                self.nsem += 1
                trk.dsem = self.es.enter_context(self.nc.semaphore("d%d" % self.nsem))
                trk.dcnt = 0
            self.dsems.append(trk)
        return trk.dsem

    def end_phase(self):
        self.barrier()
        for t in self.phase_tiles:
            if t.k.dsem is not None:
                self.free_dsems.append((t.k.dsem, t.k.dcnt))
                self.dsems.remove(t.k)
                t.k.dsem = None
        self.phase_tiles = []

    def _need(self, E, dep):
        if dep is None:
            return
        sem, val = dep
        key = id(sem)
        if E.seen.get(key, 0) < val:
            E.eng.wait_ge(sem, val)
            E.seen[key] = val

    def _resolve(self, E, R, W):
        for t in R:
            self._need(E, t.k.w)
        for t in W:
            self._need(E, t.k.w)
            for d in t.k.r:
                self._need(E, d)

    def _record(self, v, R, W):
        for t in R:
            if not t.k.const:
                t.k.r.append(v)
        for t in W:
            t.k.w = v
            t.k.r = []

    def op(self, E, fn, R=(), W=(), mark=True):
        self._resolve(E, R, W)
        ins = fn(E.eng)
        if mark:
            E.cnt += 1
            ins.then_inc(E.sem, 1)
            v = (E.sem, E.cnt)
            E.seen[id(E.sem)] = E.seen.get(id(E.sem), 0)
            self._record(v, R, W)
        return ins

    def dma(self, Q, out, in_, R=(), W=(), owner=None):
        self._resolve(Q, R, W)
        sem = self._dsem(owner.k)
        ins = Q.eng.dma_start(out=out, in_=in_)
        owner.k.dcnt += 16
        ins.then_inc(sem, 16)
        v = (sem, owner.k.dcnt)
        self._record(v, R, W)
        return ins

    def barrier(self):
        deps = [(e.sem, e.cnt) for e in self.engs if e.cnt > 0]
        deps += [(t.dsem, t.dcnt) for t in self.dsems if t.dcnt > 0]
        for E in self.engs:
            for d in deps:
                if d[0] is E.sem:
                    continue
                self._need(E, d)


class Ring:
    def __init__(self, tiles):
        self.tiles = tiles
        self.i = 0

    def next(self):
        t = self.tiles[self.i % len(self.tiles)]
        self.i += 1
        return t


def build_nc(NB=10, layers=(0, 1, 2, 3), dbg=False, phases=None):
    NT = NB * 512
    NROW = NB * 8
    nc = bass.Bass("TRN2", target_bir_lowering=False)
    dram = lambda n, s, d, k: nc.dram_tensor(n, s, d, kind=k).ap()
    xT = dram("xT", [D, NT], F32, "ExternalInput")
    pT = dram("pT", [DEPTH, DPLE, NT], F32, "ExternalInput")
    vecs = dram("vecs", [DEPTH, 128, NV], F32, "ExternalInput")
    vbias = dram("vbias", [DEPTH, 128, 512], F32, "ExternalInput")
    g2h = dram("g2h", [DEPTH, 128, 8 * 14 * 64], F32, "ExternalInput")
    w_in = dram("w_in", [DEPTH, D, DIN], F32, "ExternalInput")
    w_out = dram("w_out", [DEPTH, D, D], F32, "ExternalInput")
    ffn_w1 = dram("ffn_w1", [2, D, FF], F32, "ExternalInput")
    ffn_w3 = dram("ffn_w3", [2, D, FF], F32, "ExternalInput")
    ffn_w2 = dram("ffn_w2", [2, FF, D], F32, "ExternalInput")
    w_router = dram("w_router", [2, D, NEXP], F32, "ExternalInput")
    moe_w1 = dram("moe_w1", [2, NEXP, D, FFE], F32, "ExternalInput")
    moe_w3 = dram("moe_w3", [2, NEXP, D, FFE], F32, "ExternalInput")
    moe_w2 = dram("moe_w2", [2, NEXP, FFE, D], F32, "ExternalInput")
    ple_w = dram("ple_w", [DEPTH, DPLE, D], F32, "ExternalInput")
    ple_gate_w = dram("ple_gate_w", [DEPTH, D, D], F32, "ExternalInput")
    OUT = dram("outT", [D, NT], F32, "ExternalOutput")
    SK = "ExternalOutput" if dbg else "Internal"
    H = dram("H", [D, NT], F32, SK)
    U = dram("U", [512, NT + 30], BF16, SK)
    Qd = dram("Qd", [512, NT], BF16, SK)
    Kd = dram("Kd", [512, NT], BF16, SK)
    Ve = dram("Ve", [NB * 4, 128, 520], BF16, SK)
    Vo = dram("Vo", [NB * 4, 128, 520], BF16, SK)
    dbg_out = {}
    if dbg:
        dbg_out["mix"] = dram("dbg_mix", [128, 8 * 512], F32, "ExternalOutput")
        dbg_out["gat"] = dram("dbg_gat", [128, 32], F32, "ExternalOutput")
        dbg_out["pos"] = dram("dbg_pos", [128, NB * 32], F32, "ExternalOutput")

    fm = lambda ap: ap.rearrange("(c p) t -> p c t", p=128)
    hm = lambda ap: ap.rearrange("(h d) t -> d h t", d=64)

    with ExitStack() as es:
        kb = KB(nc, es)
        pe, act, dve, pool, sp = kb.pe, kb.act, kb.dve, kb.pool, kb.sp
        identf = kb.sb(es, "identf", [128, 128], F32)
        identb = kb.sb(es, "identb", [128, 128], BF16, const=False)
        ones1k = kb.sb(es, "ones1k", [128, 128], BF16)
        ones512 = kb.sb(es, "ones512", [128, 128], BF16)
        zt = kb.sb(es, "zt", [128, 4, 15], BF16)
        PS = [kb.ps(es, "ps%d" % i, [128, 512], F32) for i in range(4)]
        PSB = kb.ps(es, "psb", [128, 2048], F32)
        kb.op(pool, lambda e: e.memset(identf[:], 0.0), W=[identf])
        kb.op(pool, lambda e: e.affine_select(out=identf[:], in_=identf[:], compare_op=ALU.not_equal, fill=1.0,
                                              base=0, pattern=[[-1, 128]], channel_multiplier=1),
              R=[identf], W=[identf])
        kb.op(pool, lambda e: e.tensor_copy(identb[:], identf[:]), R=[identf], W=[identb])
        kb.op(pool, lambda e: e.memset(ones1k[:], 1.0 / 1024.0), W=[ones1k])
        kb.op(pool, lambda e: e.memset(ones512[:], 1.0 / 512.0), W=[ones512])
        kb.op(pool, lambda e: e.memset(zt[:], 0.0), W=[zt])
        Up = U.rearrange("(c p) t -> p c t", p=128)
        kb.dma(sp, Up[:, :, 0:15], zt[:], R=[zt], owner=zt)
        kb.dma(sp, Up[:, :, NT + 15:NT + 30], zt[:], R=[zt], owner=zt)
        identb.k.const = True
        identf.k.const = True
        ones1 = kb.sb(es, "ones1", [128, 128], BF16)
        UT = kb.sb(es, "UT", [128, 128], BF16)
        iota_f = kb.sb(es, "iota_f", [128, 512], F32)
        iota_p = kb.sb(es, "iota_p", [128, 1], F32)
        with ExitStack() as tmp:
            utf = kb.sb(tmp, "utf", [128, 128], F32)
            iota_i = kb.sb(tmp, "iota_i", [128, 512], mybir.dt.int32)
            iota_pi = kb.sb(tmp, "iota_pi", [128, 1], mybir.dt.int32)
            kb.op(pool, lambda e: e.memset(ones1[:], 1.0), W=[ones1])
            kb.op(pool, lambda e: e.memset(utf[:], 1.0), W=[utf])
            kb.op(pool, lambda e: e.affine_select(out=utf[:], in_=utf[:], compare_op=ALU.is_gt, fill=0.0,
                                                  base=0, pattern=[[1, 128]], channel_multiplier=-1), R=[utf], W=[utf])
            kb.op(pool, lambda e: e.tensor_copy(UT[:], utf[:]), R=[utf], W=[UT])
            kb.op(pool, lambda e: e.iota(iota_i[:], pattern=[[1, 512]], base=0, channel_multiplier=0), W=[iota_i])
            kb.op(pool, lambda e: e.tensor_copy(iota_f[:], iota_i[:]), R=[iota_i], W=[iota_f])
            kb.op(pool, lambda e: e.iota(iota_pi[:], pattern=[[0, 1]], base=0, channel_multiplier=1), W=[iota_pi])
            kb.op(pool, lambda e: e.tensor_copy(iota_p[:], iota_pi[:]), R=[iota_pi], W=[iota_p])
            kb.end_phase()
        for t_ in (ones1, UT, iota_f, iota_p):
            t_.k.const = True
        ones1k.k.const = True
        ones512.k.const = True

        psi = [0]

        def nps():
            t = PS[psi[0] % 4]
            psi[0] += 1
            return t

        def mm_acc(ps_ap, pst, pairs, R):
            n = len(pairs)
            for i, (l, r) in enumerate(pairs):
                last = i == n - 1
                kb.op(pe, lambda e, l=l, r=r, i=i, last=last: e.matmul(ps_ap, l, r, start=(i == 0), stop=last),
                      R=R if last else (R if i == 0 else ()), W=[pst], mark=last)

        def ln_fm(ph, t, C, ones, gcol, bcol, wk, out_fn):
            tb, tsq, mean, msq, rstd = wk["tb"], wk["tsq"], wk["mean"], wk["msq"], wk["rstd"]
            kb.op(act, lambda e: e.activation(out=tb[:, 0:C, :], in_=t[:, 0:C, :], func=AF.Copy), R=[t], W=[tb])
            kb.op(pool, lambda e: e.tensor_tensor(out=tsq[:, 0:C, :], in0=t[:, 0:C, :], in1=t[:, 0:C, :], op=ALU.mult),
                  R=[t], W=[tsq])
            pm = nps()
            mm_acc(pm[:], pm, [(ones[:], tb[:, c, :]) for c in range(C)], [tb, ones])
            pq = nps()
            mm_acc(pq[:], pq, [(ones[:], tsq[:, c, :]) for c in range(C)], [tsq, ones])
            kb.op(act, lambda e: e.activation(out=mean[:], in_=pm[:], func=AF.Copy), R=[pm], W=[mean])
            kb.op(dve, lambda e: e.tensor_tensor(out=msq[:], in0=mean[:], in1=mean[:], op=ALU.mult), R=[mean], W=[msq])
            kb.op(dve, lambda e: e.tensor_tensor(out=msq[:], in0=pq[:], in1=msq[:], op=ALU.subtract), R=[pq, msq], W=[msq])
            kb.op(dve, lambda e: e.tensor_scalar(out=msq[:], in0=msq[:], scalar1=0.0, scalar2=EPS, op0=ALU.max, op1=ALU.add),
                  R=[msq], W=[msq])
            kb.op(act, lambda e: e.activation(out=msq[:], in_=msq[:], func=AF.Sqrt), R=[msq], W=[msq])
            kb.op(dve, lambda e: e.reciprocal(rstd[:], msq[:]), R=[msq], W=[rstd])
            for c in range(C):
                kb.op(dve, lambda e, c=c: e.tensor_tensor(out=t[:, c, :], in0=t[:, c, :], in1=mean[:], op=ALU.subtract),
                      R=[t, mean], W=[t])
                kb.op(dve, lambda e, c=c: e.tensor_tensor(out=t[:, c, :], in0=t[:, c, :], in1=rstd[:], op=ALU.mult),
                      R=[t, rstd], W=[t])
                out_fn(c)

        def load_vec(ph, l):
            vt = kb.sb(ph, "vecs", [128, NV], F32)
            kb.dma(sp, vt[:], vecs[l], W=[vt], owner=vt)
            vt.k.const = True
            return vt

        def load_w(tile, view_ap, src_ap):
            kb.dma(pool, view_ap, src_ap, W=[tile], owner=tile)

        def dbg_store(name, tile_ap, tl):
            if name in dbg_out:
                kb.dma(pool, dbg_out[name], tile_ap, R=[tl], owner=tl)

        def phase0():
            with ExitStack() as ph:
                vt = load_vec(ph, 0)
                tt = [kb.sb(ph, "p0t%d" % i, [128, 8, 512], F32) for i in range(2)]
                wk = dict(tb=kb.sb(ph, "tb", [128, 8, 512], BF16), tsq=kb.sb(ph, "tsq", [128, 8, 512], BF16),
                          mean=kb.sb(ph, "mean", [128, 512], F32), msq=kb.sb(ph, "msq", [128, 512], F32),
                          rstd=kb.sb(ph, "rstd", [128, 512], F32))
                for b in range(NB):
                    t = tt[b % 2]
                    kb.dma(sp, t[:], fm(xT)[:, :, b * 512:(b + 1) * 512], W=[t], owner=t)

                    def outf(c, t=t):
                        kb.op(act, lambda e: e.activation(out=t[:, c, :], in_=t[:, c, :], func=AF.Identity,
                                                          scale=vt[:, C_LIG + c:C_LIG + c + 1],
                                                          bias=vt[:, C_LIB + c:C_LIB + c + 1]), R=[t, vt], W=[t])
                    ln_fm(ph, t, 8, ones1k, None, None, wk, outf)
                    kb.dma(sp, fm(H)[:, :, b * 512:(b + 1) * 512], t[:], R=[t], owner=t)
                kb.end_phase()

        def phase1a(l):
            with ExitStack() as ph:
                vt = load_vec(ph, l)
                win = kb.sb(ph, "win", [128, 8, DIN], BF16)
                wv = w_in[l].rearrange("(c p) n -> p c n", p=128)
                for g in range(5):
                    load_w(win, win[:, :, g * 512:(g + 1) * 512], wv[:, :, g * 512:(g + 1) * 512])
                vb = kb.sb(ph, "vb", [128, 512], F32)
                kb.dma(sp, vb[:], vbias[l], W=[vb], owner=vb)
                hbs = Ring([kb.sb(ph, "hb%d" % i, [128, 8, 576], BF16) for i in range(2)])
                sg = kb.sb(ph, "sg", [128, 4, 512], F32)
                usts = Ring([kb.sb(ph, "ust%d" % i, [128, 4, 512], BF16) for i in range(2)])
                qsts = Ring([kb.sb(ph, "qst%d" % i, [128, 4, 512], BF16) for i in range(2)])
                ksts = Ring([kb.sb(ph, "kst%d" % i, [128, 4, 512], BF16) for i in range(2)])
                vsts = Ring([kb.sb(ph, "vst%d" % i, [128, 8, 8, 65], BF16) for i in range(2)])
                for v in vsts.tiles:
                    kb.op(pool, lambda e, v=v: e.memset(v[:], 1.0), W=[v])
                Hf = fm(H)

                def load_hb(b):
                    hb = hbs.next()
                    n = 576 if b < NB - 1 else 512
                    kb.dma(pool, hb[:, :, 0:n], Hf[:, :, b * 512:b * 512 + n], W=[hb], owner=hb)
                    return hb
                hb_next = load_hb(0)
                for b in range(NB):
                    hb = hb_next
                    if b + 1 < NB:
                        hb_next = load_hb(b + 1)
                    ust, qst, kst, vst = usts.next(), qsts.next(), ksts.next(), vsts.next()
                    for oc in (4, 0, 5, 1, 6, 2, 7, 3, 8, 9, 10, 11, 12, 13, 14, 15):
                        p = nps()
                        mm_acc(p[:], p, [(win[:, kc, oc * 128:(oc + 1) * 128], hb[:, kc, 0:512]) for kc in range(8)],
                               [win, hb])
                        bcol = vt[:, C_BIN + oc:C_BIN + oc + 1]
                        if 4 <= oc < 8:
                            kb.op(act, lambda e, p=p, oc=oc, bcol=bcol: e.activation(
                                out=sg[:, oc - 4, :], in_=p[:], func=AF.Sigmoid, bias=bcol, scale=1.0),
                                R=[p, vt], W=[sg])
                        elif oc < 4:
                            kb.op(dve, lambda e, p=p, oc=oc, bcol=bcol: e.scalar_tensor_tensor(
                                out=ust[:, oc, :], in0=p[:], scalar=bcol, in1=sg[:, oc, :], op0=ALU.add, op1=ALU.mult),
                                R=[p, vt, sg], W=[ust])
                        elif oc < 12:
                            kb.op(act, lambda e, p=p, oc=oc, bcol=bcol: e.activation(
                                out=qst[:, oc - 8, :], in_=p[:], func=AF.Identity, bias=bcol, scale=1.0),
                                R=[p, vt], W=[qst])
                        else:
                            kb.op(dve, lambda e, p=p, oc=oc, bcol=bcol: e.tensor_scalar(
                                out=kst[:, oc - 12, :], in0=p[:], scalar1=bcol, scalar2=None, op0=ALU.add),
                                R=[p, vt], W=[kst])
                    kb.dma(sp, Up[:, :, 15 + b * 512:15 + (b + 1) * 512], ust[:], R=[ust], owner=ust)
                    kb.dma(sp, fm(Qd)[:, :, b * 512:(b + 1) * 512], qst[:], R=[qst], owner=qst)
                    kb.dma(sp, fm(Kd)[:, :, b * 512:(b + 1) * 512], kst[:], R=[kst], owner=kst)
                    for s in range(8):
                        jl = s % 4
                        off = jl * 128 + (64 if s >= 4 else 0)
                        if s >= 4 and b == NB - 1 and jl == 3:
                            continue
                        p = nps()
                        mm_acc(p[:], p, [(hb[:, kc, off:off + 128], win[:, kc, 2048:2560]) for kc in range(8)],
                               [win, hb])
                        kb.op(dve, lambda e, p=p, s=s: e.tensor_tensor(
                            out=vst[:, s, :, 0:64], in0=p[:].rearrange("p (h d) -> p h d", h=8),
                            in1=vb[:].rearrange("p (h d) -> p h d", h=8), op=ALU.add), R=[p, vb], W=[vst])
                    vv = vst[:].rearrange("p s h d -> p s (h d)")
                    kb.dma(sp, Ve[b * 4:(b + 1) * 4].rearrange("j p n -> p j n"), vv[:, 0:4, :], R=[vst], owner=vst)
                    kb.dma(sp, Vo[b * 4:(b + 1) * 4].rearrange("j p n -> p j n"), vv[:, 4:8, :], R=[vst], owner=vst)
                kb.end_phase()

        def phase1b(l):
            with ExitStack() as ph:
                vt = load_vec(ph, l)
                wo = kb.sb(ph, "wo", [128, 8, D], BF16)
                wv = w_out[l].rearrange("(c p) n -> p c n", p=128)
                for g in range(2):
                    load_w(wo, wo[:, :, g * 512:(g + 1) * 512], wv[:, :, g * 512:(g + 1) * 512])
                g2 = kb.sb(ph, "g2", [128, 8, 14, 64], BF16)
                with ExitStack() as tmp:
                    g2s = [kb.sb(tmp, "g2s%d" % i, [128, 14 * 64], F32) for i in range(2)]
                    for h in range(8):
                        s = g2s[h % 2]
                        kb.dma(sp, s[:], g2h[l][:, h * 896:(h + 1) * 896], W=[s], owner=s)
                        kb.op(act, lambda e, s=s, h=h: e.activation(out=g2[:, h, :, :].rearrange("p a b -> p (a b)"),
                                                                    in_=s[:], func=AF.Exp), R=[s], W=[g2])
                    kb.barrier()
                g2.k.const = True
                ubs = Ring([kb.sb(ph, "ub%d" % i, [128, 4, 542], BF16) for i in range(2)])
                qbs = Ring([kb.sb(ph, "qb%d" % i, [64, 8, 512], BF16) for i in range(1)])
                kbs = Ring([kb.sb(ph, "kb%d" % i, [64, 8, 960], BF16) for i in range(2)])
                ves = Ring([kb.sb(ph, "ve%d" % i, [128, 8, 520], BF16) for i in range(2)])
                vos = Ring([kb.sb(ph, "vo%d" % i, [128, 8, 520], BF16) for i in range(2)])
                hres_t = kb.sb(ph, "hres", [128, 8, 512], F32)
                y = kb.sb(ph, "y", [128, 4, 512], F32)
                tt = kb.sb(ph, "tt", [128, 8, 512], F32)
                mixT = kb.sb(ph, "mixT", [128, 8, 512], BF16)
                wk = dict(tb=kb.sb(ph, "tb", [128, 8, 512], BF16), tsq=kb.sb(ph, "tsq", [128, 8, 512], BF16),
                          mean=kb.sb(ph, "mean", [128, 512], F32), msq=kb.sb(ph, "msq", [128, 512], F32),
                          rstd=kb.sb(ph, "rstd", [128, 512], F32))
                pexs = Ring([kb.sb(ph, "pex%d" % i, [128, 2, 256], BF16) for i in range(12)])
                dgs = Ring([kb.sb(ph, "dg%d" % i, [128, 128], BF16) for i in range(8)])
                arow = Ring([kb.sb(ph, "arow%d" % i, [64, 8, 64], BF16) for i in range(3)])
                rcp = Ring([kb.sb(ph, "rcp%d" % i, [64, 8], F32) for i in range(3)])
                tmpo = kb.sb(ph, "tmpo", [128, 512], F32)
                PSO = Tl(PSB.t[:, 0:1024])
                PST = Tl(PSB.t[:, 1024:1536].bitcast(BF16)[:, 0:256])
                PSC = Tl(PSB.t[:, 1536:2048])

                def loads(b):
                    ub, qb, kbt, ve, vo = ubs.next(), qbs.next(), kbs.next(), ves.next(), vos.next()
                    kb.dma(sp, ub[:], Up[:, :, b * 512:b * 512 + 542], W=[ub], owner=ub)
                    klo = max(0, 8 * b - 4)
                    khi = min(NROW, 8 * b + 11)
                    kb.dma(sp, kbt[:, :, 0:(khi - klo) * 64], hm(Kd)[:, :, klo * 64:khi * 64], W=[kbt], owner=kbt)
                    jlo = klo // 2
                    jhi = min(NB * 4, jlo + 8)
                    kb.dma(sp, ve[:, 0:jhi - jlo, :], Ve[jlo:jhi].rearrange("j p n -> p j n"), W=[ve], owner=ve)
                    jho = min(NB * 4 - 1, jlo + 8)
                    kb.dma(sp, vo[:, 0:jho - jlo, :], Vo[jlo:jho].rearrange("j p n -> p j n"), W=[vo], owner=vo)
                    return ub, qb, kbt, ve, vo, klo, jlo
                nxt = loads(0)
                for b in range(NB):
                    ub, qb, kbt, ve, vo, klo, jlo = nxt
                    if b + 1 < NB:
                        nxt = loads(b + 1)
                    kb.dma(sp, qb[:], hm(Qd)[:, :, b * 512:(b + 1) * 512], W=[qb], owner=qb)
                    kb.dma(sp, hres_t[:], fm(H)[:, :, b * 512:(b + 1) * 512], W=[hres_t], owner=hres_t)
                    conv_steps = []

                    def mk_step(c, j):
                        def step():
                            dg = dgs.next()
                            wcol = vt[:, C_CW + c * 31 + j:C_CW + c * 31 + j + 1]
                            kb.op(pool, lambda e: e.tensor_scalar(
                                out=dg[:], in0=identb[:], scalar1=wcol, scalar2=0.0, op0=ALU.mult, op1=ALU.add),
                                R=[identb, vt], W=[dg])
                            kb.op(pe, lambda e: e.matmul(
                                PSC[:], dg[:], ub[:, c, j:j + 512], start=(j == 0), stop=(j == 30)),
                                R=[dg, ub], W=[PSC], mark=True)
                            if j == 30:
                                kb.op(act, lambda e: e.activation(out=y[:, c, :], in_=PSC[:], func=AF.Identity,
                                                                  bias=vt[:, C_CB + c:C_CB + c + 1], scale=1.0),
                                      R=[PSC, vt], W=[y])
                        return step
                    for c in range(4):
                        for j in range(31):
                            conv_steps.append(mk_step(c, j))

                    def emit_conv(n):
                        for _ in range(min(n, len(conv_steps))):
                            conv_steps.pop(0)()

                    def outf_conv(c):
                        kb.op(act, lambda e: e.activation(out=mixT[:, c, :], in_=y[:, c, :], func=AF.Silu,
                                                          scale=vt[:, C_CG + c:C_CG + c + 1],
                                                          bias=vt[:, C_CBB + c:C_CBB + c + 1]), R=[y, vt], W=[mixT])
                    def rowinfo(rr):
                        r = 8 * b + rr
                        if r < 4:
                            fk = 0
                        elif r > NROW - 4:
                            fk = NROW - 8
                        else:
                            fk = r - 4
                        e0 = fk - r + 7
                        vt_par = ve if fk % 2 == 0 else vo
                        j0 = (fk // 2 if fk % 2 == 0 else (fk - 1) // 2) - jlo
                        return fk, e0, vt_par, j0

                    def stageA(rr):
                        fk, e0, vt_par, j0 = rowinfo(rr)
                        out = []
                        for hp in range(4):
                            pss = nps()
                            for hh in range(2):
                                h = hp * 2 + hh
                                for t in range(4):
                                    ko = (fk + 2 * t - klo) * 64
                                    first = (hh == 0 and t == 0)
                                    last = (hh == 1 and t == 3)
                                    kb.op(pe, lambda e, h=h, ko=ko, hh=hh, t=t, pss=pss: e.matmul(
                                        pss[:, hh * 256 + t * 64:hh * 256 + (t + 1) * 64],
                                        kbt[0:64, h, ko:ko + 128], qb[0:64, h, rr * 64:(rr + 1) * 64],
                                        start=True, stop=True),
                                        R=[kbt, qb] if (first or last) else (), W=[pss], mark=last)
                            pex = pexs.next()
                            kb.op(act, lambda e, pex=pex, pss=pss: e.activation(
                                out=pex[:].rearrange("p h n -> p (h n)"), in_=pss[:], func=AF.Exp, scale=0.125),
                                R=[pss], W=[pex])
                            kb.op(dve, lambda e, pex=pex, hp=hp, e0=e0: e.tensor_tensor(
                                out=pex[:].rearrange("p h (t q) -> p h t q", t=4),
                                in0=pex[:].rearrange("p h (t q) -> p h t q", t=4),
                                in1=g2[:, hp * 2:(hp + 1) * 2, e0:e0 + 7:2, :], op=ALU.mult), R=[pex, g2], W=[pex])
                            out.append(pex)
                        return out

                    def stageB(rr, pxs):
                        fk, e0, vt_par, j0 = rowinfo(rr)
                        ar = arow.next()
                        rc = rcp.next()
                        for hp in range(4):
                            pex = pxs[hp]
                            for hh in range(2):
                                h = hp * 2 + hh
                                oc0 = (h // 4) * 512 + (h % 4) * 65
                                for t in range(4):
                                    first = (hh == 0 and t == 0)
                                    last = (hh == 1 and t == 3)
                                    kb.op(pe, lambda e, h=h, hh=hh, t=t, pex=pex, oc0=oc0: e.matmul(
                                        PSO[0:64, oc0:oc0 + 65],
                                        pex[:, hh, t * 64:(t + 1) * 64], vt_par[:, j0 + t, h * 65:(h + 1) * 65],
                                        start=(t == 0), stop=(t == 3)),
                                        R=[pex, vt_par] if (first or last) else (), W=[PSO], mark=last)
                        for hf in range(2):
                            pv = PSO[0:64, hf * 512:hf * 512 + 260].rearrange("p (h d) -> p h d", d=65)
                            kb.op(dve, lambda e, rc=rc, pv=pv, hf=hf: e.reciprocal(rc[:, hf * 4:(hf + 1) * 4], pv[:, :, 64]),
                                  R=[PSO], W=[rc])
                            kb.op(dve, lambda e, rc=rc, pv=pv, hf=hf, ar=ar: e.tensor_tensor(
                                out=ar[:, hf * 4:(hf + 1) * 4, :], in0=pv[:, :, 0:64],
                                in1=rc[:, hf * 4:(hf + 1) * 4].unsqueeze(2).to_broadcast([64, 4, 64]), op=ALU.mult),
                                R=[PSO, rc], W=[ar])
                        return ar

                    def stageC(rr, ar):
                        arf = ar[:].rearrange("p h d -> p (h d)")
                        for c in range(4):
                            kb.op(pe, lambda e, c=c, arf=arf: e.transpose(
                                PST[:, c * 64:(c + 1) * 64], arf[:, c * 128:(c + 1) * 128], identb[0:64, 0:64]),
                                R=[ar, identb] if c in (0, 3) else (), W=[PST], mark=(c == 3))
                        kb.op(act, lambda e, rr=rr: e.activation(
                            out=mixT[:, 4:8, rr * 64:(rr + 1) * 64], in_=PST[:].rearrange("p (c q) -> p c q", c=4),
                            func=AF.Copy), R=[PST], W=[mixT])

                    emit_conv(8)
                    pxn = stageA(0)
                    prev = None
                    for rr in range(8):
                        pxc = pxn
                        if rr + 1 < 8:
                            pxn = stageA(rr + 1)
                        emit_conv(8)
                        ar = stageB(rr, pxc)
                        emit_conv(7)
                        if prev is not None:
                            stageC(*prev)
                        prev = (rr, ar)
                    stageC(*prev)
                    emit_conv(1000)
                    ln_fm(ph, y, 4, ones512, None, None, wk, outf_conv)
                    if b == 0:
                        dbg_store("mix", mixT[:].rearrange("p c t -> p (c t)"), mixT)
                    for oc in range(8):
                        p = nps()
                        mm_acc(p[:], p, [(wo[:, kc, oc * 128:(oc + 1) * 128], mixT[:, kc, :]) for kc in range(8)],
                               [wo, mixT])
                        kb.op(act, lambda e, p=p, oc=oc: e.activation(out=tmpo[:], in_=p[:], func=AF.Identity,
                                                                      bias=vt[:, C_BOUT + oc:C_BOUT + oc + 1], scale=1.0),
                              R=[p, vt], W=[tmpo])
                        kb.op(dve, lambda e, oc=oc: e.scalar_tensor_tensor(
                            out=tt[:, oc, :], in0=hres_t[:, oc, :], scalar=ALPHA, in1=tmpo[:], op0=ALU.mult, op1=ALU.add),
                            R=[hres_t, tmpo], W=[tt])

                    def outf1(c):
                        kb.op(act, lambda e: e.activation(out=tt[:, c, :], in_=tt[:, c, :], func=AF.Identity,
                                                          scale=vt[:, C_L1G + c:C_L1G + c + 1],
                                                          bias=vt[:, C_L1B + c:C_L1B + c + 1]), R=[tt, vt], W=[tt])
                    ln_fm(ph, tt, 8, ones1k, None, None, wk, outf1)
                    kb.dma(sp, fm(H)[:, :, b * 512:(b + 1) * 512], tt[:], R=[tt], owner=tt)
                kb.end_phase()

        def phase2(l, last_layer):
            moe = (l % 2 == 1)
            j = l // 2
            NF = (FFE if moe else FF) // 128
            with ExitStack() as ph:
                vt = load_vec(ph, l)
                wg = kb.sb(ph, "wg", [128, 8, D], BF16)
                wgv = ple_gate_w[l].rearrange("(c p) n -> p c n", p=128)
                for g in range(2):
                    load_w(wg, wg[:, :, g * 512:(g + 1) * 512], wgv[:, :, g * 512:(g + 1) * 512])
                wp = kb.sb(ph, "wp", [128, 2, D], BF16)
                load_w(wp, wp[:], ple_w[l].rearrange("(c p) n -> p c n", p=128))
                if moe:
                    wr = kb.sb(ph, "wr", [128, 8, NEXP], BF16)
                    with nc.allow_non_contiguous_dma(reason="tiny router weight"):
                        load_w(wr, wr[:], w_router[j].rearrange("(c p) n -> p c n", p=128))
                    lg = kb.sb(ph, "lg", [128, 4, 8], F32)
                    lg2 = kb.sb(ph, "lg2", [128, 4, 8], F32)
                    m1 = kb.sb(ph, "m1", [128, 4], F32)
                    m2 = kb.sb(ph, "m2", [128, 4], F32)
                    gat = kb.sb(ph, "gat", [128, 4, 8], F32)
                    gatb = kb.sb(ph, "gatb", [128, 4, 8, 128], BF16)
                    gsb = kb.sb(ph, "gsb", [128, 8, 512], BF16)
                    tmpu = Ring([kb.sb(ph, "tmpu%d" % i, [128, 512], F32) for i in range(2)])
                h1bs = Ring([kb.sb(ph, "h1b%d" % i, [128, 8, 512], BF16) for i in range(2)])
                pbs = Ring([kb.sb(ph, "pb%d" % i, [128, 2, 512], BF16) for i in range(2)])
                accR = Ring([kb.sb(ph, "acc%d" % i, [128, 8, 512], F32) for i in range(2)])
                w1s = Ring([kb.sb(ph, "w1s%d" % i, [128, 8, 512], BF16) for i in range(2)])
                w3s = Ring([kb.sb(ph, "w3s%d" % i, [128, 8, 512], BF16) for i in range(2)])
                w2s = Ring([kb.sb(ph, "w2s%d" % i, [128, 8, 512], BF16) for i in range(2)])
                actb = kb.sb(ph, "actb", [128, 28, 512], BF16)
                sgl = Ring([kb.sb(ph, "sgl%d" % i, [128, 512], F32) for i in range(2)])
                tbq = kb.sb(ph, "tbq", [128, 16, 512], BF16)
                wk = dict(tb=Tl(tbq.t[:, 0:8, :]), tsq=Tl(tbq.t[:, 8:16, :]),
                          mean=kb.sb(ph, "mean", [128, 512], F32), msq=kb.sb(ph, "msq", [128, 512], F32),
                          rstd=kb.sb(ph, "rstd", [128, 512], F32))
                Hf = fm(H)
                ACCS = [Tl(PSB.t[:, i * 512:(i + 1) * 512]) for i in range(4)]

                def loads(b):
                    h1b, pb, acc = h1bs.next(), pbs.next(), accR.next()
                    kb.dma(sp, acc[:], Hf[:, :, b * 512:(b + 1) * 512], W=[acc], owner=acc)
                    kb.dma(pool, h1b[:], Hf[:, :, b * 512:(b + 1) * 512], W=[h1b], owner=h1b)
                    kb.dma(pool, pb[:], pT[l].rearrange("(c p) t -> p c t", p=128)[:, :, b * 512:(b + 1) * 512],
                           W=[pb], owner=pb)
                    return h1b, pb, acc
                nxt = loads(0)

                def ffn_expert(h1b, w1d, w3d, w2d, gate_e, acc):
                    w1v = w1d.rearrange("(c p) n -> p c n", p=128)
                    w3v = w3d.rearrange("(c p) n -> p c n", p=128)
                    for fg in range((NF + 3) // 4):
                        a, c3 = w1s.next(), w3s.next()
                        ncf = min(4, NF - fg * 4)
                        load_w(a, a[:, :, 0:ncf * 128], w1v[:, :, fg * 512:fg * 512 + ncf * 128])
                        load_w(c3, c3[:, :, 0:ncf * 128], w3v[:, :, fg * 512:fg * 512 + ncf * 128])
                        for fc in range(ncf):
                            f = fg * 4 + fc
                            pg_ = nps()
                            mm_acc(pg_[:], pg_, [(a[:, kc, fc * 128:(fc + 1) * 128], h1b[:, kc, :]) for kc in range(8)],
                                   [a, h1b])
                            pu_ = nps()
                            mm_acc(pu_[:], pu_, [(c3[:, kc, fc * 128:(fc + 1) * 128], h1b[:, kc, :]) for kc in range(8)],
                                   [c3, h1b])
                            s_ = sgl.next()
                            kb.op(act, lambda e, s_=s_, pg_=pg_: e.activation(out=s_[:], in_=pg_[:], func=AF.Silu),
                                  R=[pg_], W=[s_])
                            if gate_e is None:
                                kb.op(dve, lambda e, s_=s_, pu_=pu_, f=f: e.tensor_tensor(
                                    out=actb[:, f, :], in0=pu_[:], in1=s_[:], op=ALU.mult), R=[pu_, s_], W=[actb])
                            else:
                                tu = tmpu.next()
                                kb.op(dve, lambda e, tu=tu, pu_=pu_: e.tensor_tensor(
                                    out=tu[:], in0=pu_[:], in1=gsb[:, gate_e, :], op=ALU.mult), R=[pu_, gsb], W=[tu])
                                kb.op(pool, lambda e, tu=tu, s_=s_, f=f: e.tensor_tensor(
                                    out=actb[:, f, :], in0=tu[:], in1=s_[:], op=ALU.mult), R=[tu, s_], W=[actb])
                    ngrp = (NF + 7) // 8
                    for half in range(2):
                        accs = ACCS
                        for fg in range(ngrp):
                            nfc = min(8, NF - fg * 8)
                            w2t = w2s.next()
                            load_w(w2t, w2t[:, 0:nfc, :],
                                   w2d[fg * 1024:fg * 1024 + nfc * 128, half * 512:(half + 1) * 512].rearrange(
                                       "(c p) n -> p c n", p=128))
                            for o4 in range(4):
                                for fc in range(nfc):
                                    first = (fg == 0 and fc == 0)
                                    lastm = (fg == ngrp - 1 and fc == nfc - 1)
                                    lastg = (fc == nfc - 1)
                                    kb.op(pe, lambda e, o4=o4, fc=fc, fg=fg, first=first, lastm=lastm, w2t=w2t: e.matmul(
                                        accs[o4][:], w2t[:, fc, o4 * 128:(o4 + 1) * 128], actb[:, fg * 8 + fc, :],
                                        start=first, stop=lastm),
                                        R=[w2t, actb] if (fc == 0 or lastg) else (), W=[accs[o4]], mark=lastg)
                        for o4 in range(4):
                            oc = half * 4 + o4
                            kb.op(dve, lambda e, o4=o4, oc=oc, acc=acc: e.tensor_tensor(
                                out=acc[:, oc, :], in0=accs[o4][:], in1=acc[:, oc, :], op=ALU.add),
                                R=[accs[o4], acc], W=[acc])

                for b in range(NB):
                    h1b, pb, acc = nxt
                    if b + 1 < NB:
                        nxt = loads(b + 1)
                    for oc in range(8):
                        pg_ = nps()
                        mm_acc(pg_[:], pg_, [(wg[:, kc, oc * 128:(oc + 1) * 128], h1b[:, kc, :]) for kc in range(8)],
                               [wg, h1b])
                        pp_ = nps()
                        mm_acc(pp_[:], pp_, [(wp[:, kc, oc * 128:(oc + 1) * 128], pb[:, kc, :]) for kc in range(2)],
                               [wp, pb])
                        s_ = sgl.next()
                        kb.op(act, lambda e, s_=s_, pg_=pg_, oc=oc: e.activation(
                            out=s_[:], in_=pg_[:], func=AF.Sigmoid, bias=vt[:, C_PGB + oc:C_PGB + oc + 1], scale=1.0),
                            R=[pg_, vt], W=[s_])
                        kb.op(dve, lambda e, s_=s_, pp_=pp_: e.tensor_tensor(out=s_[:], in0=pp_[:], in1=s_[:], op=ALU.mult),
                              R=[pp_, s_], W=[s_])
                        kb.op(dve, lambda e, s_=s_, oc=oc, acc=acc: e.scalar_tensor_tensor(
                            out=acc[:, oc, :], in0=acc[:, oc, :], scalar=ALPHA, in1=s_[:], op0=ALU.mult, op1=ALU.add),
                            R=[acc, s_], W=[acc])
                    if not moe:
                        ffn_expert(h1b, ffn_w1[j], ffn_w3[j], ffn_w2[j], None, acc)
                    else:
                        for jl in range(4):
                            pr = nps()
                            mm_acc(pr[:, 0:8], pr, [(h1b[:, kc, jl * 128:(jl + 1) * 128], wr[:, kc, :]) for kc in range(8)],
                                   [wr, h1b])
                            kb.op(act, lambda e, pr=pr, jl=jl: e.activation(out=lg[:, jl, :], in_=pr[:, 0:8], func=AF.Copy),
                                  R=[pr], W=[lg])
                        kb.op(dve, lambda e: e.tensor_reduce(out=m1[:], in_=lg[:], axis=AX.X, op=ALU.max), R=[lg], W=[m1])
                        kb.op(dve, lambda e: e.tensor_tensor(out=lg2[:], in0=lg[:],
                                                             in1=m1[:].unsqueeze(2).to_broadcast([128, 4, 8]),
                                                             op=ALU.is_equal), R=[lg, m1], W=[lg2])
                        kb.op(dve, lambda e: e.scalar_tensor_tensor(out=lg2[:], in0=lg2[:], scalar=-1e9, in1=lg[:],
                                                                    op0=ALU.mult, op1=ALU.add), R=[lg, lg2], W=[lg2])
                        kb.op(dve, lambda e: e.tensor_reduce(out=m2[:], in_=lg2[:], axis=AX.X, op=ALU.max), R=[lg2], W=[m2])
                        kb.op(dve, lambda e: e.tensor_tensor(out=lg2[:], in0=lg[:],
                                                             in1=m2[:].unsqueeze(2).to_broadcast([128, 4, 8]),
                                                             op=ALU.is_ge), R=[lg, m2], W=[lg2])
                        kb.op(dve, lambda e: e.tensor_tensor(out=gat[:], in0=lg[:],
                                                             in1=m1[:].unsqueeze(2).to_broadcast([128, 4, 8]),
                                                             op=ALU.subtract), R=[lg, m1], W=[gat])
                        kb.op(act, lambda e: e.activation(out=gat[:], in_=gat[:], func=AF.Exp), R=[gat], W=[gat])
                        kb.op(dve, lambda e: e.tensor_tensor(out=gat[:], in0=gat[:], in1=lg2[:], op=ALU.mult),
                              R=[gat, lg2], W=[gat])
                        kb.op(dve, lambda e: e.tensor_reduce(out=m2[:], in_=gat[:], axis=AX.X, op=ALU.add), R=[gat], W=[m2])
                        kb.op(dve, lambda e: e.reciprocal(m1[:], m2[:]), R=[m2], W=[m1])
                        kb.op(dve, lambda e: e.tensor_tensor(out=gat[:], in0=gat[:],
                                                             in1=m1[:].unsqueeze(2).to_broadcast([128, 4, 8]),
                                                             op=ALU.mult), R=[gat, m1], W=[gat])
                        if b == 0:
                            dbg_store("gat", gat[:].rearrange("p a b -> p (a b)"), gat)
                        kb.op(dve, lambda e: e.tensor_copy(gatb[:], gat[:].unsqueeze(3).to_broadcast([128, 4, 8, 128])),
                              R=[gat], W=[gatb])
                        for ex in range(NEXP):
                            pgt = nps()
                            for jl in range(4):
                                kb.op(pe, lambda e, jl=jl, ex=ex, pgt=pgt: e.matmul(
                                    pgt[:, jl * 128:(jl + 1) * 128], gatb[:, jl, ex, :], identb[:], start=True, stop=True),
                                    R=[gatb, identb] if jl in (0, 3) else (), W=[pgt], mark=(jl == 3))
                            kb.op(act, lambda e, ex=ex, pgt=pgt: e.activation(out=gsb[:, ex, :], in_=pgt[:], func=AF.Copy),
                                  R=[pgt], W=[gsb])
                        for ex in range(NEXP):
                            ffn_expert(h1b, moe_w1[j, ex], moe_w3[j, ex], moe_w2[j, ex], ex, acc)

                    def outf2(c, acc=acc):
                        kb.op(act, lambda e: e.activation(out=acc[:, c, :], in_=acc[:, c, :], func=AF.Identity,
                                                          scale=vt[:, C_L2G + c:C_L2G + c + 1],
                                                          bias=vt[:, C_L2B + c:C_L2B + c + 1]), R=[acc, vt], W=[acc])
                    wk["tb"].k = tbq.k
                    wk["tsq"].k = tbq.k
                    ln_fm(ph, acc, 8, ones1k, None, None, wk, outf2)
                    dst = OUT if last_layer else H
                    kb.dma(sp, fm(dst)[:, :, b * 512:(b + 1) * 512], acc[:], R=[acc], owner=acc)
                kb.end_phase()

        I32 = mybir.dt.int32
        CB = 192
        NS = NB * CB
        H1T = dram("H1T", [NB * 4, 128, D], BF16, "Internal")
        YD = dram("YD", [NEXP, NS, D], BF16, "Internal")
        iota_p128 = kb.sb(es, "iota_p128", [128, 1], F32)
        kb.op(dve, lambda e: e.tensor_scalar(out=iota_p128[:], in0=iota_p[:], scalar1=128.0, scalar2=None, op0=ALU.add),
              R=[iota_p], W=[iota_p128])
        iota_p128.k.const = True

        def moe_layer(l, last_layer):
            j = l // 2
            NF = FFE // 128
            NTL = NB * 4
            with ExitStack() as lay:
                posall = kb.sb(lay, "posall", [128, NTL, 8], F32)
                gatall = kb.sb(lay, "gatall", [128, NTL, 8], F32)
                kb.phase_tiles = []
                with ExitStack() as ph:
                    vt = load_vec(ph, l)
                    wg = kb.sb(ph, "wg", [128, 8, D], BF16)
                    wgv = ple_gate_w[l].rearrange("(c p) n -> p c n", p=128)
                    for g in range(2):
                        load_w(wg, wg[:, :, g * 512:(g + 1) * 512], wgv[:, :, g * 512:(g + 1) * 512])
                    wp = kb.sb(ph, "wp", [128, 2, D], BF16)
                    load_w(wp, wp[:], ple_w[l].rearrange("(c p) n -> p c n", p=128))
                    wr = kb.sb(ph, "wr", [128, 8, NEXP], BF16)
                    load_w(wr, wr[:], w_router[j].rearrange("(c p) n -> p c n", p=128))
                    for t_ in (wg, wp, wr):
                        t_.k.const = True
                    lg = kb.sb(ph, "lg", [128, 4, 8], F32)
                    lg2 = kb.sb(ph, "lg2", [128, 4, 8], F32)
                    m1 = kb.sb(ph, "m1", [128, 4], F32)
                    m2 = kb.sb(ph, "m2", [128, 4], F32)
                    gat = kb.sb(ph, "gat", [128, 4, 8], F32)
                    selb = kb.sb(ph, "selb", [128, 4, 8], BF16)
                    posf = kb.sb(ph, "posf", [128, 4, 8], F32)
                    msk = kb.sb(ph, "msk", [128, 4, 8], F32)
                    h1bs = Ring([kb.sb(ph, "h1b%d" % i, [128, 8, 512], BF16) for i in range(2)])
                    pbs = Ring([kb.sb(ph, "pb%d" % i, [128, 2, 512], BF16) for i in range(2)])
                    accs_ = Ring([kb.sb(ph, "accA%d" % i, [128, 8, 512], F32) for i in range(2)])
                    sgl = Ring([kb.sb(ph, "sgl%d" % i, [128, 512], F32) for i in range(2)])
                    h1tm = Ring([kb.sb(ph, "h1tm%d" % i, [128, 4, D], BF16) for i in range(2)])
                    PTR = [Tl(PSB.t[:, i * 512:(i + 1) * 512].bitcast(BF16)) for i in range(4)]
                    Hf = fm(H)

                    def loadsA(b):
                        h1b, pb, acc = h1bs.next(), pbs.next(), accs_.next()
                        kb.dma(pool, h1b[:], Hf[:, :, b * 512:(b + 1) * 512], W=[h1b], owner=h1b)
                        kb.dma(pool, pb[:], pT[l].rearrange("(c p) t -> p c t", p=128)[:, :, b * 512:(b + 1) * 512],
                               W=[pb], owner=pb)
                        kb.dma(sp, acc[:], Hf[:, :, b * 512:(b + 1) * 512], W=[acc], owner=acc)
                        return h1b, pb, acc
                    nxt = loadsA(0)
                    for b in range(NB):
                        h1b, pb, acc = nxt
                        if b + 1 < NB:
                            nxt = loadsA(b + 1)
                        for oc in range(8):
                            pg_ = nps()
                            mm_acc(pg_[:], pg_, [(wg[:, kc, oc * 128:(oc + 1) * 128], h1b[:, kc, :]) for kc in range(8)],
                                   [wg, h1b])
                            pp_ = nps()
                            mm_acc(pp_[:], pp_, [(wp[:, kc, oc * 128:(oc + 1) * 128], pb[:, kc, :]) for kc in range(2)],
                                   [wp, pb])
                            s_ = sgl.next()
                            kb.op(act, lambda e, s_=s_, pg_=pg_, oc=oc: e.activation(
                                out=s_[:], in_=pg_[:], func=AF.Sigmoid, bias=vt[:, C_PGB + oc:C_PGB + oc + 1], scale=1.0),
                                R=[pg_, vt], W=[s_])
                            kb.op(dve, lambda e, s_=s_, pp_=pp_: e.tensor_tensor(out=s_[:], in0=pp_[:], in1=s_[:], op=ALU.mult),
                                  R=[pp_, s_], W=[s_])
                            kb.op(dve, lambda e, s_=s_, oc=oc, acc=acc: e.scalar_tensor_tensor(
                                out=acc[:, oc, :], in0=acc[:, oc, :], scalar=ALPHA, in1=s_[:], op0=ALU.mult, op1=ALU.add),
                                R=[acc, s_], W=[acc])
                        kb.dma(sp, Hf[:, :, b * 512:(b + 1) * 512], acc[:], R=[acc], owner=acc)
                        ht = h1tm.next()
                        for jl in range(4):
                            ptr = PTR[jl]
                            for c in range(8):
                                kb.op(pe, lambda e, jl=jl, c=c, ptr=ptr: e.transpose(
                                    ptr[:, c * 128:(c + 1) * 128], h1b[:, c, jl * 128:(jl + 1) * 128], identb[:]),
                                    R=[h1b, identb] if c in (0, 7) else (), W=[ptr], mark=(c == 7))
                            kb.op(act if jl % 2 == 0 else dve,
                                  (lambda e, jl=jl, ptr=ptr, ht=ht: e.activation(out=ht[:, jl, :], in_=ptr[:], func=AF.Copy))
                                  if jl % 2 == 0 else
                                  (lambda e, jl=jl, ptr=ptr, ht=ht: e.tensor_copy(ht[:, jl, :], ptr[:])),
                                  R=[ptr], W=[ht])
                        kb.dma(sp, H1T[b * 4:(b + 1) * 4].rearrange("j p n -> p j n"), ht[:], R=[ht], owner=ht)
                        for jl in range(4):
                            pr = nps()
                            mm_acc(pr[:, 0:8], pr, [(h1b[:, kc, jl * 128:(jl + 1) * 128], wr[:, kc, :]) for kc in range(8)],
                                   [wr, h1b])
                            kb.op(act, lambda e, pr=pr, jl=jl: e.activation(out=lg[:, jl, :], in_=pr[:, 0:8], func=AF.Copy),
                                  R=[pr], W=[lg])
                        bc = lambda t_: t_[:].unsqueeze(2).to_broadcast([128, 4, 8])
                        kb.op(dve, lambda e: e.tensor_reduce(out=m1[:], in_=lg[:], axis=AX.X, op=ALU.max), R=[lg], W=[m1])
                        kb.op(dve, lambda e: e.tensor_tensor(out=lg2[:], in0=lg[:], in1=bc(m1), op=ALU.is_equal),
                              R=[lg, m1], W=[lg2])
                        kb.op(dve, lambda e: e.scalar_tensor_tensor(out=lg2[:], in0=lg2[:], scalar=-1e9, in1=lg[:],
                                                                    op0=ALU.mult, op1=ALU.add), R=[lg, lg2], W=[lg2])
                        kb.op(dve, lambda e: e.tensor_reduce(out=m2[:], in_=lg2[:], axis=AX.X, op=ALU.max), R=[lg2], W=[m2])
                        kb.op(dve, lambda e: e.tensor_tensor(out=lg2[:], in0=lg[:], in1=bc(m2), op=ALU.is_ge),
                              R=[lg, m2], W=[lg2])
                        kb.op(dve, lambda e: e.tensor_tensor(out=gat[:], in0=lg[:], in1=bc(m1), op=ALU.subtract),
                              R=[lg, m1], W=[gat])
                        kb.op(act, lambda e: e.activation(out=gat[:], in_=gat[:], func=AF.Exp), R=[gat], W=[gat])
                        kb.op(dve, lambda e: e.tensor_tensor(out=gat[:], in0=gat[:], in1=lg2[:], op=ALU.mult),
                              R=[gat, lg2], W=[gat])
                        kb.op(dve, lambda e: e.tensor_reduce(out=m2[:], in_=gat[:], axis=AX.X, op=ALU.add), R=[gat], W=[m2])
                        kb.op(dve, lambda e: e.reciprocal(m1[:], m2[:]), R=[m2], W=[m1])
                        kb.op(dve, lambda e, b=b: e.tensor_tensor(out=gatall[:, b * 4:(b + 1) * 4, :], in0=gat[:], in1=bc(m1),
                                                                  op=ALU.mult), R=[gat, m1], W=[gatall])
                        if b == 0:
                            dbg_store("gat", gatall[:, 0:4, :].rearrange("p a b -> p (a b)"), gatall)
                        kb.op(dve, lambda e: e.tensor_copy(selb[:], lg2[:]), R=[lg2], W=[selb])
                        pp = nps()
                        for jl in range(4):
                            prs = [(UT[:], selb[:, jl, :])] + [(ones1[:], selb[:, j2, :]) for j2 in range(jl)]
                            mm_acc(pp[:, jl * 8:(jl + 1) * 8], pp, prs, [UT, ones1, selb])
                        kb.op(dve, lambda e, pp=pp: e.tensor_scalar(
                            out=msk[:], in0=pp[:, 0:32].rearrange("p (a b) -> p a b", a=4), scalar1=float(CB), scalar2=None,
                            op0=ALU.is_lt), R=[pp], W=[msk])
                        kb.op(dve, lambda e: e.tensor_tensor(out=msk[:], in0=msk[:], in1=lg2[:], op=ALU.mult),
                              R=[msk, lg2], W=[msk])
                        kb.op(dve, lambda e, pp=pp: e.scalar_tensor_tensor(
                            out=posf[:], in0=pp[:, 0:32].rearrange("p (a b) -> p a b", a=4), scalar=1.0, in1=msk[:],
                            op0=ALU.add, op1=ALU.mult), R=[pp, msk], W=[posf])
                        kb.op(dve, lambda e, b=b: e.tensor_scalar(out=posall[:, b * 4:(b + 1) * 4, :], in0=posf[:],
                                                                  scalar1=-1.0, scalar2=None, op0=ALU.add),
                              R=[posf], W=[posall])
                    dbg_store("pos", posall[:].rearrange("p a b -> p (a b)"), posall)
                    kb.end_phase()
                if os.environ.get("K_MOE_STOP") == "A":
                    return
                with ExitStack() as ph:
                    xg = kb.sb(ph, "xg", [128, 8, NS], BF16)
                    actb = kb.sb(ph, "actb", [128, NF, NS], BF16)
                    w1s = Ring([kb.sb(ph, "w1s%d" % i, [128, 8, 512], BF16) for i in range(2)])
                    w3s = Ring([kb.sb(ph, "w3s%d" % i, [128, 8, 512], BF16) for i in range(2)])
                    w2s = Ring([kb.sb(ph, "w2s%d" % i, [128, 8, 512], BF16) for i in range(2)])
                    pts = Ring([kb.sb(ph, "pt%d" % i, [128, CB], BF16) for i in range(4)])
                    hts = Ring([kb.sb(ph, "ht%d" % i, [128, D], BF16) for i in range(4)])
                    sgl = Ring([kb.sb(ph, "sgl%d" % i, [128, 512], F32) for i in range(2)])
                    yst = Ring([kb.sb(ph, "yst%d" % i, [128, 512], BF16) for i in range(4)])
                    ACC4 = [Tl(PSB.t[:, i * 512:(i + 1) * 512]) for i in range(4)]
                    ACC8 = PS + ACC4
                    nblk = [(s0, min(512, NS - s0)) for s0 in range(0, NS, 512)]
                    ntile = NS // 128
                    for ex in range(NEXP):
                        for b in range(NB):
                            tls = []
                            for t4 in range(4):
                                tl = b * 4 + t4
                                pt_, ht_ = pts.next(), hts.next()
                                kb.dma(sp, ht_[:], H1T[tl], W=[ht_], owner=ht_)
                                kb.op(dve, lambda e, pt_=pt_, tl=tl, ex=ex: e.tensor_scalar(
                                    out=pt_[:], in0=iota_f[:, 0:CB], scalar1=posall[:, tl, ex:ex + 1], scalar2=None,
                                    op0=ALU.is_equal), R=[iota_f, posall], W=[pt_])
                                tls.append((pt_, ht_))
                            for c in range(8):
                                bk = ACC4[c // 2]
                                mm_acc(bk[:, (c % 2) * 256:(c % 2) * 256 + CB], bk,
                                       [(ht_[:, c * 128:(c + 1) * 128], pt_[:]) for (pt_, ht_) in tls],
                                       [x_ for pr_ in tls for x_ in pr_])
                            gview = PSB.t[:].rearrange("p (c w) -> p c w", w=256)[:, :, 0:CB]
                            kb.op(act if b % 2 == 0 else dve,
                                  (lambda e, b=b: e.activation(out=xg[:, :, b * CB:(b + 1) * CB], in_=gview, func=AF.Copy))
                                  if b % 2 == 0 else
                                  (lambda e, b=b: e.tensor_copy(xg[:, :, b * CB:(b + 1) * CB], gview)),
                                  R=ACC4, W=[xg])
                        w1v = moe_w1[j, ex].rearrange("(c p) n -> p c n", p=128)
                        w3v = moe_w3[j, ex].rearrange("(c p) n -> p c n", p=128)
                        for fg in range(NF // 4):
                            a, c3 = w1s.next(), w3s.next()
                            load_w(a, a[:], w1v[:, :, fg * 512:(fg + 1) * 512])
                            load_w(c3, c3[:], w3v[:, :, fg * 512:(fg + 1) * 512])
                            for (s0, n) in nblk:
                                xs = slice(s0, s0 + n)
                                for fc in range(4):
                                    f = fg * 4 + fc
                                    pg_ = nps()
                                    mm_acc(pg_[:, 0:n], pg_, [(a[:, kc, fc * 128:(fc + 1) * 128], xg[:, kc, xs]) for kc in range(8)],
                                           [a, xg])
                                    pu_ = nps()
                                    mm_acc(pu_[:, 0:n], pu_, [(c3[:, kc, fc * 128:(fc + 1) * 128], xg[:, kc, xs]) for kc in range(8)],
                                           [c3, xg])
                                    s_ = sgl.next()
                                    kb.op(act, lambda e, s_=s_, pg_=pg_, n=n: e.activation(out=s_[:, 0:n], in_=pg_[:, 0:n], func=AF.Silu),
                                          R=[pg_], W=[s_])
                                    kb.op(dve, lambda e, s_=s_, pu_=pu_, f=f, xs=xs, n=n: e.tensor_tensor(
                                        out=actb[:, f, xs], in0=pu_[:, 0:n], in1=s_[:, 0:n], op=ALU.mult), R=[pu_, s_], W=[actb])
                        ngrp = (NF + 7) // 8
                        for half in range(2):
                            for t0 in range(0, ntile, 8):
                                tiles = list(range(t0, min(ntile, t0 + 8)))
                                for fg in range(ngrp):
                                    nfc = min(8, NF - fg * 8)
                                    w2t = w2s.next()
                                    load_w(w2t, w2t[:, 0:nfc, :],
                                           moe_w2[j, ex][fg * 1024:fg * 1024 + nfc * 128, half * 512:(half + 1) * 512].rearrange(
                                               "(c p) n -> p c n", p=128))
                                    for st in tiles:
                                        ac = ACC8[st - t0]
                                        for fc in range(nfc):
                                            first = (fg == 0 and fc == 0)
                                            lastm = (fg == ngrp - 1 and fc == nfc - 1)
                                            lastg = (fc == nfc - 1)
                                            kb.op(pe, lambda e, st=st, fc=fc, fg=fg, first=first, lastm=lastm, w2t=w2t, ac=ac:
                                                  e.matmul(ac[:], actb[:, fg * 8 + fc, st * 128:(st + 1) * 128], w2t[:, fc, :],
                                                           start=first, stop=lastm),
                                                  R=[w2t, actb] if (fc == 0 or lastg) else (), W=[ac], mark=lastg)
                                for st in tiles:
                                    ac = ACC8[st - t0]
                                    yt = yst.next()
                                    kb.op(act if st % 2 == 0 else dve,
                                          (lambda e, ac=ac, yt=yt: e.activation(out=yt[:], in_=ac[:], func=AF.Copy))
                                          if st % 2 == 0 else
                                          (lambda e, ac=ac, yt=yt: e.tensor_copy(yt[:], ac[:])),
                                          R=[ac], W=[yt])
                                    kb.dma(sp, YD[ex][st * 128:(st + 1) * 128, half * 512:(half + 1) * 512], yt[:],
                                           R=[yt], owner=yt)
                    kb.end_phase()
                if os.environ.get("K_MOE_STOP") == "B":
                    return
                with ExitStack() as ph:
                    vt = load_vec(ph, l)
                    acc = kb.sb(ph, "acc", [128, 8, 512], F32)
                    brdR = Ring([kb.sb(ph, "brd%d" % i, [128, 2, 4, 8, 128], BF16) for i in range(2)])
                    gsbR = Ring([kb.sb(ph, "gsb%d" % i, [128, 512], F32) for i in range(3)])
                    pgaR = Ring([kb.sb(ph, "pga%d" % i, [128, 8, 512], BF16) for i in range(2)])
                    pgbR = Ring([kb.sb(ph, "pgb%d" % i, [64, 8, 512], BF16) for i in range(2)])
                    yas = Ring([kb.sb(ph, "ya%d" % i, [128, 8, D], BF16) for i in range(2)])
                    ybs = Ring([kb.sb(ph, "yb%d" % i, [64, 8, D], BF16) for i in range(2)])
                    tbq = kb.sb(ph, "tbq", [128, 16, 512], BF16)
                    wk = dict(tb=Tl(tbq.t[:, 0:8, :]), tsq=Tl(tbq.t[:, 8:16, :]),
                              mean=kb.sb(ph, "mean", [128, 512], F32), msq=kb.sb(ph, "msq", [128, 512], F32),
                              rstd=kb.sb(ph, "rstd", [128, 512], F32))
                    wk["tb"].k = tbq.k
                    wk["tsq"].k = tbq.k
                    ACCS = [Tl(PSB.t[:, i * 512:(i + 1) * 512]) for i in range(4)]
                    Hf = fm(H)

                    def loadsC(b):
                        ya, yb_ = yas.next(), ybs.next()
                        for ex in range(NEXP):
                            kb.dma(sp, ya[:, ex, :], YD[ex][b * CB:b * CB + 128, :], W=[ya], owner=ya)
                            kb.dma(sp, yb_[:, ex, :], YD[ex][b * CB + 128:b * CB + CB, :], W=[yb_], owner=yb_)
                        return ya, yb_
                    nxt = loadsC(0)
                    for b in range(NB):
                        ya, yb_ = nxt
                        if b + 1 < NB:
                            nxt = loadsC(b + 1)
                        kb.dma(sp, acc[:], Hf[:, :, b * 512:(b + 1) * 512], W=[acc], owner=acc)
                        brd, pga, pgb = brdR.next(), pgaR.next(), pgbR.next()
                        for qi, src in enumerate((posall, gatall)):
                            kb.op(dve if qi == 0 else pool, lambda e, qi=qi, src=src, b=b, brd=brd: e.tensor_copy(
                                brd[:, qi, :, :, :], src[:, b * 4:(b + 1) * 4, :].unsqueeze(3).to_broadcast([128, 4, 8, 128])),
                                R=[src], W=[brd])
                        for ex in range(NEXP):
                            pbs_ = []
                            for qi in range(2):
                                pq = nps()
                                for jl in range(4):
                                    kb.op(pe, lambda e, jl=jl, ex=ex, qi=qi, pq=pq, brd=brd: e.matmul(
                                        pq[:, jl * 128:(jl + 1) * 128], brd[:, qi, jl, ex, :], identb[:], start=True, stop=True),
                                        R=[brd, identb] if jl in (0, 3) else (), W=[pq], mark=(jl == 3))
                                pbs_.append(pq)
                            gsb = gsbR.next()
                            kb.op(act, lambda e, pq=pbs_[1], gsb=gsb: e.activation(out=gsb[:], in_=pq[:], func=AF.Copy),
                                  R=[pbs_[1]], W=[gsb])
                            kb.op(dve, lambda e, ex=ex, pq=pbs_[0], gsb=gsb, pga=pga: e.scalar_tensor_tensor(
                                out=pga[:, ex, :], in0=pq[:], scalar=iota_p[:, 0:1], in1=gsb[:], op0=ALU.is_equal, op1=ALU.mult),
                                R=[pbs_[0], iota_p, gsb], W=[pga])
                            kb.op(dve, lambda e, ex=ex, pq=pbs_[0], gsb=gsb, pgb=pgb: e.scalar_tensor_tensor(
                                out=pgb[:, ex, :], in0=pq[0:64, :], scalar=iota_p128[0:64, 0:1], in1=gsb[0:64, :],
                                op0=ALU.is_equal, op1=ALU.mult), R=[pbs_[0], iota_p128, gsb], W=[pgb])
                        for half in range(2):
                            for o4 in range(4):
                                oc = half * 4 + o4
                                prs = []
                                for ex in range(NEXP):
                                    prs.append((ya[:, ex, oc * 128:(oc + 1) * 128], pga[:, ex, :]))
                                    prs.append((yb_[0:64, ex, oc * 128:(oc + 1) * 128], pgb[0:64, ex, :]))
                                mm_acc(ACCS[o4][:], ACCS[o4], prs, [ya, yb_, pga, pgb])
                                kb.op(dve, lambda e, o4=o4, oc=oc: e.tensor_tensor(
                                    out=acc[:, oc, :], in0=ACCS[o4][:], in1=acc[:, oc, :], op=ALU.add),
                                    R=[ACCS[o4], acc], W=[acc])

                        def outf2(c):
                            kb.op(act, lambda e: e.activation(out=acc[:, c, :], in_=acc[:, c, :], func=AF.Identity,
                                                              scale=vt[:, C_L2G + c:C_L2G + c + 1],
                                                              bias=vt[:, C_L2B + c:C_L2B + c + 1]), R=[acc, vt], W=[acc])
                        ln_fm(ph, acc, 8, ones1k, None, None, wk, outf2)
                        dst = OUT if last_layer else H
                        kb.dma(sp, fm(dst)[:, :, b * 512:(b + 1) * 512], acc[:], R=[acc], owner=acc)
                    kb.end_phase()

        if phases is None:
            phases = ["0"] + [x for l in layers for x in ("1a%d" % l, "1b%d" % l, "2%d" % l)]
        for phn in phases:
            if phn == "0":
                phase0()
            elif phn.startswith("1a"):
                phase1a(int(phn[2:]))
            elif phn.startswith("1b"):
                phase1b(int(phn[2:]))
            else:
                l_ = int(phn[1:])
                if l_ % 2 == 1 and not os.environ.get("K_DENSE_MOE"):
                    moe_layer(l_, l_ == layers[-1])
                else:
                    phase2(l_, l_ == layers[-1])
        kb.barrier()
    return nc


def _cols(v, C):
    return np.ascontiguousarray(v.reshape(C, 128).T)


def _prep_shared(inp):
    f32 = np.float32
    vecs = np.zeros((DEPTH, 128, NV), f32)
    vb = np.zeros((DEPTH, 128, 512), f32)
    g2 = np.full((DEPTH, 128, 8, 14, 64), MASKV, f32)
    kc = np.arange(64)[:, None]
    qc = np.arange(64)[None, :]
    win0 = np.clip(qc - 8, 0, 48)
    colvalid = (kc >= win0) & (kc < win0 + 16)
    dc = np.clip(kc - qc + 15, 0, 30)
    for l in range(DEPTH):
        v = vecs[l]
        v[:, C_BIN:C_BIN + 20] = _cols(inp["b_in"][l], 20)
        v[:, C_CW:C_CW + 124] = inp["conv_w"][l].T.reshape(4, 128, 31).transpose(1, 0, 2).reshape(128, 124)
        v[:, C_CB:C_CB + 4] = _cols(inp["conv_b"][l], 4)
        v[:, C_CG:C_CG + 4] = _cols(inp["conv_ln_g"][l], 4)
        v[:, C_CBB:C_CBB + 4] = _cols(inp["conv_ln_b"][l], 4)
        v[:, C_BOUT:C_BOUT + 8] = _cols(inp["b_out"][l], 8)
        v[:, C_L1G:C_L1G + 8] = _cols(inp["ln1_g"][l], 8)
        v[:, C_L1B:C_L1B + 8] = _cols(inp["ln1_b"][l], 8)
        v[:, C_PGB:C_PGB + 8] = _cols(inp["ple_gate_b"][l], 8)
        v[:, C_L2G:C_L2G + 8] = _cols(inp["ln2_g"][l], 8)
        v[:, C_L2B:C_L2B + 8] = _cols(inp["ln2_b"][l], 8)
        v[:, C_LIG:C_LIG + 8] = _cols(inp["ln_in_g"], 8)
        v[:, C_LIB:C_LIB + 8] = _cols(inp["ln_in_b"], 8)
        vb[l] = np.broadcast_to(inp["b_in"][l][2048:2560][None, :], (128, 512))
        rpb = inp["rpb"][l]
        for kr in range(2):
            for ei in range(14):
                dr = ei - 7 + kr
                if dr < -7 or dr > 7:
                    continue
                tab = rpb[:, dr + 7, :][:, dc]
                tab = np.where(colvalid[None], tab, f32(MASKV))
                g2[l, kr * 64:(kr + 1) * 64, :, ei, :] = tab.transpose(1, 0, 2)
    return vecs, vb, g2.reshape(DEPTH, 128, 8 * 14 * 64)


_NC_CACHE = {}


def kernel(**inp):
    inp = {k: np.asarray(v) for k, v in inp.items()}
    NB = 10
    NT = NB * 512
    key = ("full",)
    if key not in _NC_CACHE:
        _NC_CACHE[key] = build_nc(NB, (0, 1, 2, 3))
    nc = _NC_CACHE[key]
    vecs, vb, g2 = _prep_shared(inp)
    shared = dict(vecs=vecs, vbias=vb, g2h=g2)
    for n in ("w_in", "w_out", "ffn_w1", "ffn_w3", "ffn_w2", "w_router", "moe_w1", "moe_w3", "moe_w2", "ple_w",
              "ple_gate_w"):
        shared[n] = np.ascontiguousarray(inp[n], dtype=np.float32)
    in_maps = []
    for c in range(8):
        b, half = c // 2, c % 2
        t0 = 0 if half == 0 else 48 * 64
        m = dict(shared)
        m["xT"] = np.ascontiguousarray(inp["x"][b, t0:t0 + NT, :].T)
        m["pT"] = np.ascontiguousarray(inp["p"][:, b, t0:t0 + NT, :].transpose(0, 2, 1))
        in_maps.append(m)
    res = run_bass_kernel_spmd(nc, in_maps, core_ids=list(range(8)))
    out = np.empty((4, 8192, D), np.float32)
    for c in range(8):
        b, half = c // 2, c % 2
        o = res.results[c]["outT"]
        if half == 0:
            out[b, 0:4096, :] = o[:, 0:4096].T
        else:
            out[b, 4096:8192, :] = o[:, 1024:5120].T
    return out
```

```python
import os
import numpy as np
from contextlib import ExitStack
import concourse.bass as bass
import concourse.mybir as mybir
from concourse.bass_utils import run_bass_kernel_spmd

F32 = mybir.dt.float32
BF16 = mybir.dt.bfloat16
AF = mybir.ActivationFunctionType
ALU = mybir.AluOpType
AX = mybir.AxisListType

DEPTH = 4
D = 1024
DIN = 2560
NEXP = 8
FF = 2816
FFE = 3584
DPLE = 256
ALPHA = (2.0 * DEPTH) ** 0.25
EPS = 1e-5
MASKV = -200.0
CAP = 1536
NCH = CAP // 128
NV = 220
C_BIN, C_CW, C_CB, C_CG, C_CBB, C_BOUT, C_L1G, C_L1B, C_PGB, C_L2G, C_L2B, C_LIG, C_LIB = (
    0, 20, 144, 148, 152, 156, 164, 172, 180, 188, 196, 204, 212)


class Trk:
    __slots__ = ("w", "r", "dsem", "dcnt", "const")

    def __init__(self, const=False):
        self.w = None
        self.r = []
        self.dsem = None
        self.dcnt = 0
        self.const = const


class Tl:
    def __init__(self, t, const=False):
        self.t = t
        self.k = Trk(const)

    def __getitem__(self, key):
        return self.t[key]


class Eng:
    def __init__(self, name, eng, sem):
        self.name = name
        self.eng = eng
        self.sem = sem
        self.cnt = 0
        self.seen = {}


class KB:
    def __init__(self, nc, es):
        self.nc = nc
        self.es = es
        self.sems = []
        mk = lambda n: es.enter_context(nc.semaphore(n))
        self.pe = Eng("pe", nc.tensor, mk("s_pe"))
        self.act = Eng("act", nc.scalar, mk("s_act"))
        self.dve = Eng("dve", nc.vector, mk("s_dve"))
        self.pool = Eng("pool", nc.gpsimd, mk("s_pool"))
        self.sp = Eng("sp", nc.sync, mk("s_sp"))
        self.engs = [self.pe, self.act, self.dve, self.pool, self.sp]
        self.dsems = []
        self.free_dsems = []
        self.nsem = 0
        self.phase_tiles = []

    def sb(self, es, name, shape, dt, const=False):
        self.uid = getattr(self, "uid", 0) + 1
        t = Tl(es.enter_context(self.nc.sbuf_tensor("%s_%d" % (name, self.uid), shape, dt)), const)
        if es is not self.es:
            self.phase_tiles.append(t)
        return t

    def ps(self, es, name, shape, dt):
        return Tl(es.enter_context(self.nc.psum_tensor(name, shape, dt)))

    def _dsem(self, trk):
        if trk.dsem is None:
            if self.free_dsems:
                trk.dsem, trk.dcnt = self.free_dsems.pop()
            else:
                self.nsem += 1
                trk.dsem = self.es.enter_context(self.nc.semaphore("d%d" % self.nsem))
                trk.dcnt = 0
            self.dsems.append(trk)
        return trk.dsem

    def end_phase(self):
        self.barrier()
        for t in self.phase_tiles:
            if t.k.dsem is not None:
                self.free_dsems.append((t.k.dsem, t.k.dcnt))
                self.dsems.remove(t.k)
                t.k.dsem = None
        self.phase_tiles = []

    def _need(self, E, dep):
        if dep is None:
            return
        sem, val = dep
        key = id(sem)
        if E.seen.get(key, 0) < val:
            E.eng.wait_ge(sem, val)
            E.seen[key] = val

    def _resolve(self, E, R, W):
        for t in R:
            self._need(E, t.k.w)
        for t in W:
            self._need(E, t.k.w)
            for d in t.k.r:
                self._need(E, d)

    def _record(self, v, R, W):
        for t in R:
            if not t.k.const:
                t.k.r.append(v)
        for t in W:
            t.k.w = v
            t.k.r = []

    def op(self, E, fn, R=(), W=(), mark=True):
        self._resolve(E, R, W)
        ins = fn(E.eng)
        if mark:
            E.cnt += 1
            ins.then_inc(E.sem, 1)
            v = (E.sem, E.cnt)
            E.seen[id(E.sem)] = E.seen.get(id(E.sem), 0)
            self._record(v, R, W)
        return ins

    def dma(self, Q, out, in_, R=(), W=(), owner=None):
        self._resolve(Q, R, W)
        sem = self._dsem(owner.k)
        ins = Q.eng.dma_start(out=out, in_=in_)
        owner.k.dcnt += 16
        ins.then_inc(sem, 16)
        v = (sem, owner.k.dcnt)
        self._record(v, R, W)
        return ins

    def barrier(self):
        deps = [(e.sem, e.cnt) for e in self.engs if e.cnt > 0]
        deps += [(t.dsem, t.dcnt) for t in self.dsems if t.dcnt > 0]
        for E in self.engs:
            for d in deps:
                if d[0] is E.sem:
                    continue
                self._need(E, d)


class Ring:
    def __init__(self, tiles):
        self.tiles = tiles
        self.i = 0

    def next(self):
        t = self.tiles[self.i % len(self.tiles)]
        self.i += 1
        return t


def build_nc(NB=10, layers=(0, 1, 2, 3), dbg=False, phases=None):
    NT = NB * 512
    NROW = NB * 8
    nc = bass.Bass("TRN2", target_bir_lowering=False)
    dram = lambda n, s, d, k: nc.dram_tensor(n, s, d, kind=k).ap()
    xT = dram("xT", [D, NT], F32, "ExternalInput")
    pT = dram("pT", [DEPTH, DPLE, NT], F32, "ExternalInput")
    vecs = dram("vecs", [DEPTH, 128, NV], F32, "ExternalInput")
    vbias = dram("vbias", [DEPTH, 128, 512], F32, "ExternalInput")
    g2h = dram("g2h", [DEPTH, 128, 8 * 14 * 64], F32, "ExternalInput")
    w_in = dram("w_in", [DEPTH, D, DIN], F32, "ExternalInput")
    w_out = dram("w_out", [DEPTH, D, D], F32, "ExternalInput")
    ffn_w1 = dram("ffn_w1", [2, D, FF], F32, "ExternalInput")
    ffn_w3 = dram("ffn_w3", [2, D, FF], F32, "ExternalInput")
    ffn_w2 = dram("ffn_w2", [2, FF, D], F32, "ExternalInput")
    w_router = dram("w_router", [2, D, NEXP], F32, "ExternalInput")
    moe_w1 = dram("moe_w1", [2, NEXP, D, FFE], F32, "ExternalInput")
    moe_w3 = dram("moe_w3", [2, NEXP, D, FFE], F32, "ExternalInput")
    moe_w2 = dram("moe_w2", [2, NEXP, FFE, D], F32, "ExternalInput")
    ple_w = dram("ple_w", [DEPTH, DPLE, D], F32, "ExternalInput")
    ple_gate_w = dram("ple_gate_w", [DEPTH, D, D], F32, "ExternalInput")
    OUT = dram("outT", [D, NT], F32, "ExternalOutput")
    SK = "ExternalOutput" if dbg else "Internal"
    H = dram("H", [D, NT], F32, SK)
    U = dram("U", [512, NT + 30], BF16, SK)
    Qd = dram("Qd", [512, NT], BF16, SK)
    Kd = dram("Kd", [512, NT], BF16, SK)
    Ve = dram("Ve", [NB * 4, 128, 520], BF16, SK)
    Vo = dram("Vo", [NB * 4, 128, 520], BF16, SK)
    dbg_out = {}
    if dbg:
        dbg_out["mix"] = dram("dbg_mix", [128, 8 * 512], F32, "ExternalOutput")
        dbg_out["gat"] = dram("dbg_gat", [128, 32], F32, "ExternalOutput")
        dbg_out["pos"] = dram("dbg_pos", [128, NB * 32], F32, "ExternalOutput")

    fm = lambda ap: ap.rearrange("(c p) t -> p c t", p=128)
    hm = lambda ap: ap.rearrange("(h d) t -> d h t", d=64)

    with ExitStack() as es:
        kb = KB(nc, es)
        pe, act, dve, pool, sp = kb.pe, kb.act, kb.dve, kb.pool, kb.sp
        identf = kb.sb(es, "identf", [128, 128], F32)
        identb = kb.sb(es, "identb", [128, 128], BF16, const=False)
        ones1k = kb.sb(es, "ones1k", [128, 128], BF16)
        ones512 = kb.sb(es, "ones512", [128, 128], BF16)
        zt = kb.sb(es, "zt", [128, 4, 15], BF16)
        PS = [kb.ps(es, "ps%d" % i, [128, 512], F32) for i in range(4)]
        PSB = kb.ps(es, "psb", [128, 2048], F32)
        kb.op(pool, lambda e: e.memset(identf[:], 0.0), W=[identf])
        kb.op(pool, lambda e: e.affine_select(out=identf[:], in_=identf[:], compare_op=ALU.not_equal, fill=1.0,
                                              base=0, pattern=[[-1, 128]], channel_multiplier=1),
              R=[identf], W=[identf])
        kb.op(pool, lambda e: e.tensor_copy(identb[:], identf[:]), R=[identf], W=[identb])
        kb.op(pool, lambda e: e.memset(ones1k[:], 1.0 / 1024.0), W=[ones1k])
        kb.op(pool, lambda e: e.memset(ones512[:], 1.0 / 512.0), W=[ones512])
        kb.op(pool, lambda e: e.memset(zt[:], 0.0), W=[zt])
        Up = U.rearrange("(c p) t -> p c t", p=128)
        kb.dma(sp, Up[:, :, 0:15], zt[:], R=[zt], owner=zt)
        kb.dma(sp, Up[:, :, NT + 15:NT + 30], zt[:], R=[zt], owner=zt)
        identb.k.const = True
        identf.k.const = True
        ones1 = kb.sb(es, "ones1", [128, 128], BF16)
        UT = kb.sb(es, "UT", [128, 128], BF16)
        iota_f = kb.sb(es, "iota_f", [128, 512], F32)
        iota_p = kb.sb(es, "iota_p", [128, 1], F32)
        with ExitStack() as tmp:
            utf = kb.sb(tmp, "utf", [128, 128], F32)
            iota_i = kb.sb(tmp, "iota_i", [128, 512], mybir.dt.int32)
            iota_pi = kb.sb(tmp, "iota_pi", [128, 1], mybir.dt.int32)
            kb.op(pool, lambda e: e.memset(ones1[:], 1.0), W=[ones1])
            kb.op(pool, lambda e: e.memset(utf[:], 1.0), W=[utf])
            kb.op(pool, lambda e: e.affine_select(out=utf[:], in_=utf[:], compare_op=ALU.is_gt, fill=0.0,
                                                  base=0, pattern=[[1, 128]], channel_multiplier=-1), R=[utf], W=[utf])
            kb.op(pool, lambda e: e.tensor_copy(UT[:], utf[:]), R=[utf], W=[UT])
            kb.op(pool, lambda e: e.iota(iota_i[:], pattern=[[1, 512]], base=0, channel_multiplier=0), W=[iota_i])
            kb.op(pool, lambda e: e.tensor_copy(iota_f[:], iota_i[:]), R=[iota_i], W=[iota_f])
            kb.op(pool, lambda e: e.iota(iota_pi[:], pattern=[[0, 1]], base=0, channel_multiplier=1), W=[iota_pi])
            kb.op(pool, lambda e: e.tensor_copy(iota_p[:], iota_pi[:]), R=[iota_pi], W=[iota_p])
            kb.end_phase()
        for t_ in (ones1, UT, iota_f, iota_p):
            t_.k.const = True
        ones1k.k.const = True
        ones512.k.const = True

        psi = [0]

        def nps():
            t = PS[psi[0] % 4]
            psi[0] += 1
            return t

        def mm_acc(ps_ap, pst, pairs, R):
            n = len(pairs)
            for i, (l, r) in enumerate(pairs):
                last = i == n - 1
                kb.op(pe, lambda e, l=l, r=r, i=i, last=last: e.matmul(ps_ap, l, r, start=(i == 0), stop=last),
                      R=R if last else (R if i == 0 else ()), W=[pst], mark=last)

        def ln_fm(ph, t, C, ones, gcol, bcol, wk, out_fn):
            tb, tsq, mean, msq, rstd = wk["tb"], wk["tsq"], wk["mean"], wk["msq"], wk["rstd"]
            kb.op(act, lambda e: e.activation(out=tb[:, 0:C, :], in_=t[:, 0:C, :], func=AF.Copy), R=[t], W=[tb])
            kb.op(pool, lambda e: e.tensor_tensor(out=tsq[:, 0:C, :], in0=t[:, 0:C, :], in1=t[:, 0:C, :], op=ALU.mult),
                  R=[t], W=[tsq])
            pm = nps()
            mm_acc(pm[:], pm, [(ones[:], tb[:, c, :]) for c in range(C)], [tb, ones])
            pq = nps()
            mm_acc(pq[:], pq, [(ones[:], tsq[:, c, :]) for c in range(C)], [tsq, ones])
            kb.op(act, lambda e: e.activation(out=mean[:], in_=pm[:], func=AF.Copy), R=[pm], W=[mean])
            kb.op(dve, lambda e: e.tensor_tensor(out=msq[:], in0=mean[:], in1=mean[:], op=ALU.mult), R=[mean], W=[msq])
            kb.op(dve, lambda e: e.tensor_tensor(out=msq[:], in0=pq[:], in1=msq[:], op=ALU.subtract), R=[pq, msq], W=[msq])
            kb.op(dve, lambda e: e.tensor_scalar(out=msq[:], in0=msq[:], scalar1=0.0, scalar2=EPS, op0=ALU.max, op1=ALU.add),
                  R=[msq], W=[msq])
            kb.op(act, lambda e: e.activation(out=msq[:], in_=msq[:], func=AF.Sqrt), R=[msq], W=[msq])
            kb.op(dve, lambda e: e.reciprocal(rstd[:], msq[:]), R=[msq], W=[rstd])
            for c in range(C):
                kb.op(dve, lambda e, c=c: e.tensor_tensor(out=t[:, c, :], in0=t[:, c, :], in1=mean[:], op=ALU.subtract),
                      R=[t, mean], W=[t])
                kb.op(dve, lambda e, c=c: e.tensor_tensor(out=t[:, c, :], in0=t[:, c, :], in1=rstd[:], op=ALU.mult),
                      R=[t, rstd], W=[t])
                out_fn(c)

        def load_vec(ph, l):
            vt = kb.sb(ph, "vecs", [128, NV], F32)
            kb.dma(sp, vt[:], vecs[l], W=[vt], owner=vt)
            vt.k.const = True
            return vt

        def load_w(tile, view_ap, src_ap):
            kb.dma(pool, view_ap, src_ap, W=[tile], owner=tile)

        def dbg_store(name, tile_ap, tl):
            if name in dbg_out:
                kb.dma(pool, dbg_out[name], tile_ap, R=[tl], owner=tl)

        def phase0():
            with ExitStack() as ph:
                vt = load_vec(ph, 0)
                tt = [kb.sb(ph, "p0t%d" % i, [128, 8, 512], F32) for i in range(2)]
                wk = dict(tb=kb.sb(ph, "tb", [128, 8, 512], BF16), tsq=kb.sb(ph, "tsq", [128, 8, 512], BF16),
                          mean=kb.sb(ph, "mean", [128, 512], F32), msq=kb.sb(ph, "msq", [128, 512], F32),
                          rstd=kb.sb(ph, "rstd", [128, 512], F32))
                for b in range(NB):
                    t = tt[b % 2]
                    kb.dma(sp, t[:], fm(xT)[:, :, b * 512:(b + 1) * 512], W=[t], owner=t)

                    def outf(c, t=t):
                        kb.op(act, lambda e: e.activation(out=t[:, c, :], in_=t[:, c, :], func=AF.Identity,
                                                          scale=vt[:, C_LIG + c:C_LIG + c + 1],
                                                          bias=vt[:, C_LIB + c:C_LIB + c + 1]), R=[t, vt], W=[t])
                    ln_fm(ph, t, 8, ones1k, None, None, wk, outf)
                    kb.dma(sp, fm(H)[:, :, b * 512:(b + 1) * 512], t[:], R=[t], owner=t)
                kb.end_phase()

        def phase1a(l):
            with ExitStack() as ph:
                vt = load_vec(ph, l)
                win = kb.sb(ph, "win", [128, 8, DIN], BF16)
                wv = w_in[l].rearrange("(c p) n -> p c n", p=128)
                for g in range(5):
                    load_w(win, win[:, :, g * 512:(g + 1) * 512], wv[:, :, g * 512:(g + 1) * 512])
                vb = kb.sb(ph, "vb", [128, 512], F32)
                kb.dma(sp, vb[:], vbias[l], W=[vb], owner=vb)
                hbs = Ring([kb.sb(ph, "hb%d" % i, [128, 8, 576], BF16) for i in range(2)])
                sg = kb.sb(ph, "sg", [128, 4, 512], F32)
                usts = Ring([kb.sb(ph, "ust%d" % i, [128, 4, 512], BF16) for i in range(2)])
                qsts = Ring([kb.sb(ph, "qst%d" % i, [128, 4, 512], BF16) for i in range(2)])
                ksts = Ring([kb.sb(ph, "kst%d" % i, [128, 4, 512], BF16) for i in range(2)])
                vsts = Ring([kb.sb(ph, "vst%d" % i, [128, 8, 8, 65], BF16) for i in range(2)])
                for v in vsts.tiles:
                    kb.op(pool, lambda e, v=v: e.memset(v[:], 1.0), W=[v])
                Hf = fm(H)

                def load_hb(b):
                    hb = hbs.next()
                    n = 576 if b < NB - 1 else 512
                    kb.dma(pool, hb[:, :, 0:n], Hf[:, :, b * 512:b * 512 + n], W=[hb], owner=hb)
                    return hb
                hb_next = load_hb(0)
                for b in range(NB):
                    hb = hb_next
                    if b + 1 < NB:
                        hb_next = load_hb(b + 1)
                    ust, qst, kst, vst = usts.next(), qsts.next(), ksts.next(), vsts.next()
                    for oc in (4, 0, 5, 1, 6, 2, 7, 3, 8, 9, 10, 11, 12, 13, 14, 15):
                        p = nps()
                        mm_acc(p[:], p, [(win[:, kc, oc * 128:(oc + 1) * 128], hb[:, kc, 0:512]) for kc in range(8)],
                               [win, hb])
                        bcol = vt[:, C_BIN + oc:C_BIN + oc + 1]
                        if 4 <= oc < 8:
                            kb.op(act, lambda e, p=p, oc=oc, bcol=bcol: e.activation(
                                out=sg[:, oc - 4, :], in_=p[:], func=AF.Sigmoid, bias=bcol, scale=1.0),
                                R=[p, vt], W=[sg])
                        elif oc < 4:
                            kb.op(dve, lambda e, p=p, oc=oc, bcol=bcol: e.scalar_tensor_tensor(
                                out=ust[:, oc, :], in0=p[:], scalar=bcol, in1=sg[:, oc, :], op0=ALU.add, op1=ALU.mult),
                                R=[p, vt, sg], W=[ust])
                        elif oc < 12:
                            kb.op(act, lambda e, p=p, oc=oc, bcol=bcol: e.activation(
                                out=qst[:, oc - 8, :], in_=p[:], func=AF.Identity, bias=bcol, scale=1.0),
                                R=[p, vt], W=[qst])
                        else:
                            kb.op(dve, lambda e, p=p, oc=oc, bcol=bcol: e.tensor_scalar(
                                out=kst[:, oc - 12, :], in0=p[:], scalar1=bcol, scalar2=None, op0=ALU.add),
                                R=[p, vt], W=[kst])
                    kb.dma(sp, Up[:, :, 15 + b * 512:15 + (b + 1) * 512], ust[:], R=[ust], owner=ust)
                    kb.dma(sp, fm(Qd)[:, :, b * 512:(b + 1) * 512], qst[:], R=[qst], owner=qst)
                    kb.dma(sp, fm(Kd)[:, :, b * 512:(b + 1) * 512], kst[:], R=[kst], owner=kst)
                    for s in range(8):
                        jl = s % 4
                        off = jl * 128 + (64 if s >= 4 else 0)
                        if s >= 4 and b == NB - 1 and jl == 3:
                            continue
                        p = nps()
                        mm_acc(p[:], p, [(hb[:, kc, off:off + 128], win[:, kc, 2048:2560]) for kc in range(8)],
                               [win, hb])
                        kb.op(dve, lambda e, p=p, s=s: e.tensor_tensor(
                            out=vst[:, s, :, 0:64], in0=p[:].rearrange("p (h d) -> p h d", h=8),
                            in1=vb[:].rearrange("p (h d) -> p h d", h=8), op=ALU.add), R=[p, vb], W=[vst])
                    vv = vst[:].rearrange("p s h d -> p s (h d)")
                    kb.dma(sp, Ve[b * 4:(b + 1) * 4].rearrange("j p n -> p j n"), vv[:, 0:4, :], R=[vst], owner=vst)
                    kb.dma(sp, Vo[b * 4:(b + 1) * 4].rearrange("j p n -> p j n"), vv[:, 4:8, :], R=[vst], owner=vst)
                kb.end_phase()

        def phase1b(l):
            with ExitStack() as ph:
                vt = load_vec(ph, l)
                wo = kb.sb(ph, "wo", [128, 8, D], BF16)
                wv = w_out[l].rearrange("(c p) n -> p c n", p=128)
                for g in range(2):
                    load_w(wo, wo[:, :, g * 512:(g + 1) * 512], wv[:, :, g * 512:(g + 1) * 512])
                g2 = kb.sb(ph, "g2", [128, 8, 14, 64], BF16)
                with ExitStack() as tmp:
                    g2s = [kb.sb(tmp, "g2s%d" % i, [128, 14 * 64], F32) for i in range(2)]
                    for h in range(8):
                        s = g2s[h % 2]
                        kb.dma(sp, s[:], g2h[l][:, h * 896:(h + 1) * 896], W=[s], owner=s)
                        kb.op(act, lambda e, s=s, h=h: e.activation(out=g2[:, h, :, :].rearrange("p a b -> p (a b)"),
                                                                    in_=s[:], func=AF.Exp), R=[s], W=[g2])
                    kb.barrier()
                g2.k.const = True
                ubs = Ring([kb.sb(ph, "ub%d" % i, [128, 4, 542], BF16) for i in range(2)])
                qbs = Ring([kb.sb(ph, "qb%d" % i, [64, 8, 512], BF16) for i in range(1)])
                kbs = Ring([kb.sb(ph, "kb%d" % i, [64, 8, 960], BF16) for i in range(2)])
                ves = Ring([kb.sb(ph, "ve%d" % i, [128, 8, 520], BF16) for i in range(2)])
                vos = Ring([kb.sb(ph, "vo%d" % i, [128, 8, 520], BF16) for i in range(2)])
                hres_t = kb.sb(ph, "hres", [128, 8, 512], F32)
                y = kb.sb(ph, "y", [128, 4, 512], F32)
                tt = kb.sb(ph, "tt", [128, 8, 512], F32)
                mixT = kb.sb(ph, "mixT", [128, 8, 512], BF16)
                wk = dict(tb=kb.sb(ph, "tb", [128, 8, 512], BF16), tsq=kb.sb(ph, "tsq", [128, 8, 512], BF16),
                          mean=kb.sb(ph, "mean", [128, 512], F32), msq=kb.sb(ph, "msq", [128, 512], F32),
                          rstd=kb.sb(ph, "rstd", [128, 512], F32))
                pexs = Ring([kb.sb(ph, "pex%d" % i, [128, 2, 256], BF16) for i in range(12)])
                dgs = Ring([kb.sb(ph, "dg%d" % i, [128, 128], BF16) for i in range(8)])
                arow = Ring([kb.sb(ph, "arow%d" % i, [64, 8, 64], BF16) for i in range(3)])
                rcp = Ring([kb.sb(ph, "rcp%d" % i, [64, 8], F32) for i in range(3)])
                tmpo = kb.sb(ph, "tmpo", [128, 512], F32)
                PSO = Tl(PSB.t[:, 0:1024])
                PST = Tl(PSB.t[:, 1024:1536].bitcast(BF16)[:, 0:256])

                def loads(b):
                    ub, qb, kbt, ve, vo = ubs.next(), qbs.next(), kbs.next(), ves.next(), vos.next()
                    kb.dma(sp, ub[:], Up[:, :, b * 512:b * 512 + 542], W=[ub], owner=ub)
                    klo = max(0, 8 * b - 4)
                    khi = min(NROW, 8 * b + 11)
                    kb.dma(sp, kbt[:, :, 0:(khi - klo) * 64], hm(Kd)[:, :, klo * 64:khi * 64], W=[kbt], owner=kbt)
                    jlo = klo // 2
                    jhi = min(NB * 4, jlo + 8)
                    kb.dma(sp, ve[:, 0:jhi - jlo, :], Ve[jlo:jhi].rearrange("j p n -> p j n"), W=[ve], owner=ve)
                    jho = min(NB * 4 - 1, jlo + 8)
                    kb.dma(sp, vo[:, 0:jho - jlo, :], Vo[jlo:jho].rearrange("j p n -> p j n"), W=[vo], owner=vo)
                    return ub, qb, kbt, ve, vo, klo, jlo
                nxt = loads(0)
                for b in range(NB):
                    ub, qb, kbt, ve, vo, klo, jlo = nxt
                    if b + 1 < NB:
                        nxt = loads(b + 1)
                    kb.dma(sp, qb[:], hm(Qd)[:, :, b * 512:(b + 1) * 512], W=[qb], owner=qb)
                    kb.dma(sp, hres_t[:], fm(H)[:, :, b * 512:(b + 1) * 512], W=[hres_t], owner=hres_t)
                    for c in range(4):
                        pc = nps()
                        for j in range(31):
                            dg = dgs.next()
                            wcol = vt[:, C_CW + c * 31 + j:C_CW + c * 31 + j + 1]
                            kb.op(pool, lambda e, dg=dg, wcol=wcol: e.tensor_scalar(
                                out=dg[:], in0=identb[:], scalar1=wcol, scalar2=0.0, op0=ALU.mult, op1=ALU.add),
                                R=[identb, vt], W=[dg])
                            kb.op(pe, lambda e, dg=dg, c=c, j=j, pc=pc: e.matmul(
                                pc[:], dg[:], ub[:, c, j:j + 512], start=(j == 0), stop=(j == 30)),
                                R=[dg, ub], W=[pc], mark=True)
                        kb.op(act, lambda e, c=c, pc=pc: e.activation(out=y[:, c, :], in_=pc[:], func=AF.Identity,
                                                                      bias=vt[:, C_CB + c:C_CB + c + 1], scale=1.0),
                              R=[pc, vt], W=[y])

                    def outf_conv(c):
                        kb.op(act, lambda e: e.activation(out=mixT[:, c, :], in_=y[:, c, :], func=AF.Silu,
                                                          scale=vt[:, C_CG + c:C_CG + c + 1],
                                                          bias=vt[:, C_CBB + c:C_CBB + c + 1]), R=[y, vt], W=[mixT])
                    ln_fm(ph, y, 4, ones512, None, None, wk, outf_conv)
                    def rowinfo(rr):
                        r = 8 * b + rr
                        if r < 4:
                            fk = 0
                        elif r > NROW - 4:
                            fk = NROW - 8
                        else:
                            fk = r - 4
                        e0 = fk - r + 7
                        vt_par = ve if fk % 2 == 0 else vo
                        j0 = (fk // 2 if fk % 2 == 0 else (fk - 1) // 2) - jlo
                        return fk, e0, vt_par, j0

                    def stageA(rr):
                        fk, e0, vt_par, j0 = rowinfo(rr)
                        out = []
                        for hp in range(4):
                            pss = nps()
                            for hh in range(2):
                                h = hp * 2 + hh
                                for t in range(4):
                                    ko = (fk + 2 * t - klo) * 64
                                    first = (hh == 0 and t == 0)
                                    last = (hh == 1 and t == 3)
                                    kb.op(pe, lambda e, h=h, ko=ko, hh=hh, t=t, pss=pss: e.matmul(
                                        pss[:, hh * 256 + t * 64:hh * 256 + (t + 1) * 64],
                                        kbt[0:64, h, ko:ko + 128], qb[0:64, h, rr * 64:(rr + 1) * 64],
                                        start=True, stop=True),
                                        R=[kbt, qb] if (first or last) else (), W=[pss], mark=last)
                            pex = pexs.next()
                            kb.op(act, lambda e, pex=pex, pss=pss: e.activation(
                                out=pex[:].rearrange("p h n -> p (h n)"), in_=pss[:], func=AF.Exp, scale=0.125),
                                R=[pss], W=[pex])
                            kb.op(dve, lambda e, pex=pex, hp=hp, e0=e0: e.tensor_tensor(
                                out=pex[:].rearrange("p h (t q) -> p h t q", t=4),
                                in0=pex[:].rearrange("p h (t q) -> p h t q", t=4),
                                in1=g2[:, hp * 2:(hp + 1) * 2, e0:e0 + 7:2, :], op=ALU.mult), R=[pex, g2], W=[pex])
                            out.append(pex)
                        return out

                    def stageB(rr, pxs):
                        fk, e0, vt_par, j0 = rowinfo(rr)
                        ar = arow.next()
                        rc = rcp.next()
                        for hp in range(4):
                            pex = pxs[hp]
                            for hh in range(2):
                                h = hp * 2 + hh
                                oc0 = (h // 4) * 512 + (h % 4) * 65
                                for t in range(4):
                                    first = (hh == 0 and t == 0)
                                    last = (hh == 1 and t == 3)
                                    kb.op(pe, lambda e, h=h, hh=hh, t=t, pex=pex, oc0=oc0: e.matmul(
                                        PSO[0:64, oc0:oc0 + 65],
                                        pex[:, hh, t * 64:(t + 1) * 64], vt_par[:, j0 + t, h * 65:(h + 1) * 65],
                                        start=(t == 0), stop=(t == 3)),
                                        R=[pex, vt_par] if (first or last) else (), W=[PSO], mark=last)
                        for hf in range(2):
                            pv = PSO[0:64, hf * 512:hf * 512 + 260].rearrange("p (h d) -> p h d", d=65)
                            kb.op(dve, lambda e, rc=rc, pv=pv, hf=hf: e.reciprocal(rc[:, hf * 4:(hf + 1) * 4], pv[:, :, 64]),
                                  R=[PSO], W=[rc])
                            kb.op(dve, lambda e, rc=rc, pv=pv, hf=hf, ar=ar: e.tensor_tensor(
                                out=ar[:, hf * 4:(hf + 1) * 4, :], in0=pv[:, :, 0:64],
                                in1=rc[:, hf * 4:(hf + 1) * 4].unsqueeze(2).to_broadcast([64, 4, 64]), op=ALU.mult),
                                R=[PSO, rc], W=[ar])
                        return ar

                    def stageC(rr, ar):
                        arf = ar[:].rearrange("p h d -> p (h d)")
                        for c in range(4):
                            kb.op(pe, lambda e, c=c, arf=arf: e.transpose(
                                PST[:, c * 64:(c + 1) * 64], arf[:, c * 128:(c + 1) * 128], identb[0:64, 0:64]),
                                R=[ar, identb] if c in (0, 3) else (), W=[PST], mark=(c == 3))
                        kb.op(act, lambda e, rr=rr: e.activation(
                            out=mixT[:, 4:8, rr * 64:(rr + 1) * 64], in_=PST[:].rearrange("p (c q) -> p c q", c=4),
                            func=AF.Copy), R=[PST], W=[mixT])

                    pxn = stageA(0)
                    prev = None
                    for rr in range(8):
                        pxc = pxn
                        if rr + 1 < 8:
                            pxn = stageA(rr + 1)
                        ar = stageB(rr, pxc)
                        if prev is not None:
                            stageC(*prev)
                        prev = (rr, ar)
                    stageC(*prev)
                    if b == 0:
                        dbg_store("mix", mixT[:].rearrange("p c t -> p (c t)"), mixT)
                    for oc in range(8):
                        p = nps()
                        mm_acc(p[:], p, [(wo[:, kc, oc * 128:(oc + 1) * 128], mixT[:, kc, :]) for kc in range(8)],
                               [wo, mixT])
                        kb.op(act, lambda e, p=p, oc=oc: e.activation(out=tmpo[:], in_=p[:], func=AF.Identity,
                                                                      bias=vt[:, C_BOUT + oc:C_BOUT + oc + 1], scale=1.0),
                              R=[p, vt], W=[tmpo])
                        kb.op(dve, lambda e, oc=oc: e.scalar_tensor_tensor(
                            out=tt[:, oc, :], in0=hres_t[:, oc, :], scalar=ALPHA, in1=tmpo[:], op0=ALU.mult, op1=ALU.add),
                            R=[hres_t, tmpo], W=[tt])

                    def outf1(c):
                        kb.op(act, lambda e: e.activation(out=tt[:, c, :], in_=tt[:, c, :], func=AF.Identity,
                                                          scale=vt[:, C_L1G + c:C_L1G + c + 1],
                                                          bias=vt[:, C_L1B + c:C_L1B + c + 1]), R=[tt, vt], W=[tt])
                    ln_fm(ph, tt, 8, ones1k, None, None, wk, outf1)
                    kb.dma(sp, fm(H)[:, :, b * 512:(b + 1) * 512], tt[:], R=[tt], owner=tt)
                kb.end_phase()

        def phase2(l, last_layer):
            moe = (l % 2 == 1)
            j = l // 2
            NF = (FFE if moe else FF) // 128
            with ExitStack() as ph:
                vt = load_vec(ph, l)
                wg = kb.sb(ph, "wg", [128, 8, D], BF16)
                wgv = ple_gate_w[l].rearrange("(c p) n -> p c n", p=128)
                for g in range(2):
                    load_w(wg, wg[:, :, g * 512:(g + 1) * 512], wgv[:, :, g * 512:(g + 1) * 512])
                wp = kb.sb(ph, "wp", [128, 2, D], BF16)
                load_w(wp, wp[:], ple_w[l].rearrange("(c p) n -> p c n", p=128))
                if moe:
                    wr = kb.sb(ph, "wr", [128, 8, NEXP], BF16)
                    with nc.allow_non_contiguous_dma(reason="tiny router weight"):
                        load_w(wr, wr[:], w_router[j].rearrange("(c p) n -> p c n", p=128))
                    lg = kb.sb(ph, "lg", [128, 4, 8], F32)
                    lg2 = kb.sb(ph, "lg2", [128, 4, 8], F32)
                    m1 = kb.sb(ph, "m1", [128, 4], F32)
                    m2 = kb.sb(ph, "m2", [128, 4], F32)
                    gat = kb.sb(ph, "gat", [128, 4, 8], F32)
                    gatb = kb.sb(ph, "gatb", [128, 4, 8, 128], BF16)
                    gsb = kb.sb(ph, "gsb", [128, 8, 512], BF16)
                    tmpu = Ring([kb.sb(ph, "tmpu%d" % i, [128, 512], F32) for i in range(2)])
                h1bs = Ring([kb.sb(ph, "h1b%d" % i, [128, 8, 512], BF16) for i in range(2)])
                pbs = Ring([kb.sb(ph, "pb%d" % i, [128, 2, 512], BF16) for i in range(2)])
                accR = Ring([kb.sb(ph, "acc%d" % i, [128, 8, 512], F32) for i in range(2)])
                w1s = Ring([kb.sb(ph, "w1s%d" % i, [128, 8, 512], BF16) for i in range(2)])
                w3s = Ring([kb.sb(ph, "w3s%d" % i, [128, 8, 512], BF16) for i in range(2)])
                w2s = Ring([kb.sb(ph, "w2s%d" % i, [128, 8, 512], BF16) for i in range(2)])
                actb = kb.sb(ph, "actb", [128, 28, 512], BF16)
                sgl = Ring([kb.sb(ph, "sgl%d" % i, [128, 512], F32) for i in range(2)])
                tbq = kb.sb(ph, "tbq", [128, 16, 512], BF16)
                wk = dict(tb=Tl(tbq.t[:, 0:8, :]), tsq=Tl(tbq.t[:, 8:16, :]),
                          mean=kb.sb(ph, "mean", [128, 512], F32), msq=kb.sb(ph, "msq", [128, 512], F32),
                          rstd=kb.sb(ph, "rstd", [128, 512], F32))
                Hf = fm(H)
                ACCS = [Tl(PSB.t[:, i * 512:(i + 1) * 512]) for i in range(4)]

                def loads(b):
                    h1b, pb, acc = h1bs.next(), pbs.next(), accR.next()
                    kb.dma(sp, acc[:], Hf[:, :, b * 512:(b + 1) * 512], W=[acc], owner=acc)
                    kb.dma(pool, h1b[:], Hf[:, :, b * 512:(b + 1) * 512], W=[h1b], owner=h1b)
                    kb.dma(pool, pb[:], pT[l].rearrange("(c p) t -> p c t", p=128)[:, :, b * 512:(b + 1) * 512],
                           W=[pb], owner=pb)
                    return h1b, pb, acc
                nxt = loads(0)

                def ffn_expert(h1b, w1d, w3d, w2d, gate_e, acc):
                    w1v = w1d.rearrange("(c p) n -> p c n", p=128)
                    w3v = w3d.rearrange("(c p) n -> p c n", p=128)
                    for fg in range((NF + 3) // 4):
                        a, c3 = w1s.next(), w3s.next()
                        ncf = min(4, NF - fg * 4)
                        load_w(a, a[:, :, 0:ncf * 128], w1v[:, :, fg * 512:fg * 512 + ncf * 128])
                        load_w(c3, c3[:, :, 0:ncf * 128], w3v[:, :, fg * 512:fg * 512 + ncf * 128])
                        for fc in range(ncf):
                            f = fg * 4 + fc
                            pg_ = nps()
                            mm_acc(pg_[:], pg_, [(a[:, kc, fc * 128:(fc + 1) * 128], h1b[:, kc, :]) for kc in range(8)],
                                   [a, h1b])
                            pu_ = nps()
                            mm_acc(pu_[:], pu_, [(c3[:, kc, fc * 128:(fc + 1) * 128], h1b[:, kc, :]) for kc in range(8)],
                                   [c3, h1b])
                            s_ = sgl.next()
                            kb.op(act, lambda e, s_=s_, pg_=pg_: e.activation(out=s_[:], in_=pg_[:], func=AF.Silu),
                                  R=[pg_], W=[s_])
                            if gate_e is None:
                                kb.op(dve, lambda e, s_=s_, pu_=pu_, f=f: e.tensor_tensor(
                                    out=actb[:, f, :], in0=pu_[:], in1=s_[:], op=ALU.mult), R=[pu_, s_], W=[actb])
                            else:
                                tu = tmpu.next()
                                kb.op(dve, lambda e, tu=tu, pu_=pu_: e.tensor_tensor(
                                    out=tu[:], in0=pu_[:], in1=gsb[:, gate_e, :], op=ALU.mult), R=[pu_, gsb], W=[tu])
                                kb.op(pool, lambda e, tu=tu, s_=s_, f=f: e.tensor_tensor(
                                    out=actb[:, f, :], in0=tu[:], in1=s_[:], op=ALU.mult), R=[tu, s_], W=[actb])
                    ngrp = (NF + 7) // 8
                    for half in range(2):
                        accs = ACCS
                        for fg in range(ngrp):
                            nfc = min(8, NF - fg * 8)
                            w2t = w2s.next()
                            load_w(w2t, w2t[:, 0:nfc, :],
                                   w2d[fg * 1024:fg * 1024 + nfc * 128, half * 512:(half + 1) * 512].rearrange(
                                       "(c p) n -> p c n", p=128))
                            for o4 in range(4):
                                for fc in range(nfc):
                                    first = (fg == 0 and fc == 0)
                                    lastm = (fg == ngrp - 1 and fc == nfc - 1)
                                    lastg = (fc == nfc - 1)
                                    kb.op(pe, lambda e, o4=o4, fc=fc, fg=fg, first=first, lastm=lastm, w2t=w2t: e.matmul(
                                        accs[o4][:], w2t[:, fc, o4 * 128:(o4 + 1) * 128], actb[:, fg * 8 + fc, :],
                                        start=first, stop=lastm),
                                        R=[w2t, actb] if (fc == 0 or lastg) else (), W=[accs[o4]], mark=lastg)
                        for o4 in range(4):
                            oc = half * 4 + o4
                            kb.op(dve, lambda e, o4=o4, oc=oc, acc=acc: e.tensor_tensor(
                                out=acc[:, oc, :], in0=accs[o4][:], in1=acc[:, oc, :], op=ALU.add),
                                R=[accs[o4], acc], W=[acc])

                for b in range(NB):
                    h1b, pb, acc = nxt
                    if b + 1 < NB:
                        nxt = loads(b + 1)
                    for oc in range(8):
                        pg_ = nps()
                        mm_acc(pg_[:], pg_, [(wg[:, kc, oc * 128:(oc + 1) * 128], h1b[:, kc, :]) for kc in range(8)],
                               [wg, h1b])
                        pp_ = nps()
                        mm_acc(pp_[:], pp_, [(wp[:, kc, oc * 128:(oc + 1) * 128], pb[:, kc, :]) for kc in range(2)],
                               [wp, pb])
                        s_ = sgl.next()
                        kb.op(act, lambda e, s_=s_, pg_=pg_, oc=oc: e.activation(
                            out=s_[:], in_=pg_[:], func=AF.Sigmoid, bias=vt[:, C_PGB + oc:C_PGB + oc + 1], scale=1.0),
                            R=[pg_, vt], W=[s_])
                        kb.op(dve, lambda e, s_=s_, pp_=pp_: e.tensor_tensor(out=s_[:], in0=pp_[:], in1=s_[:], op=ALU.mult),
                              R=[pp_, s_], W=[s_])
                        kb.op(dve, lambda e, s_=s_, oc=oc, acc=acc: e.scalar_tensor_tensor(
                            out=acc[:, oc, :], in0=acc[:, oc, :], scalar=ALPHA, in1=s_[:], op0=ALU.mult, op1=ALU.add),
                            R=[acc, s_], W=[acc])
                    if not moe:
                        ffn_expert(h1b, ffn_w1[j], ffn_w3[j], ffn_w2[j], None, acc)
                    else:
                        for jl in range(4):
                            pr = nps()
                            mm_acc(pr[:, 0:8], pr, [(h1b[:, kc, jl * 128:(jl + 1) * 128], wr[:, kc, :]) for kc in range(8)],
                                   [wr, h1b])
                            kb.op(act, lambda e, pr=pr, jl=jl: e.activation(out=lg[:, jl, :], in_=pr[:, 0:8], func=AF.Copy),
                                  R=[pr], W=[lg])
                        kb.op(dve, lambda e: e.tensor_reduce(out=m1[:], in_=lg[:], axis=AX.X, op=ALU.max), R=[lg], W=[m1])
                        kb.op(dve, lambda e: e.tensor_tensor(out=lg2[:], in0=lg[:],
                                                             in1=m1[:].unsqueeze(2).to_broadcast([128, 4, 8]),
                                                             op=ALU.is_equal), R=[lg, m1], W=[lg2])
                        kb.op(dve, lambda e: e.scalar_tensor_tensor(out=lg2[:], in0=lg2[:], scalar=-1e9, in1=lg[:],
                                                                    op0=ALU.mult, op1=ALU.add), R=[lg, lg2], W=[lg2])
                        kb.op(dve, lambda e: e.tensor_reduce(out=m2[:], in_=lg2[:], axis=AX.X, op=ALU.max), R=[lg2], W=[m2])
                        kb.op(dve, lambda e: e.tensor_tensor(out=lg2[:], in0=lg[:],
                                                             in1=m2[:].unsqueeze(2).to_broadcast([128, 4, 8]),
                                                             op=ALU.is_ge), R=[lg, m2], W=[lg2])
                        kb.op(dve, lambda e: e.tensor_tensor(out=gat[:], in0=lg[:],
                                                             in1=m1[:].unsqueeze(2).to_broadcast([128, 4, 8]),
                                                             op=ALU.subtract), R=[lg, m1], W=[gat])
                        kb.op(act, lambda e: e.activation(out=gat[:], in_=gat[:], func=AF.Exp), R=[gat], W=[gat])
                        kb.op(dve, lambda e: e.tensor_tensor(out=gat[:], in0=gat[:], in1=lg2[:], op=ALU.mult),
                              R=[gat, lg2], W=[gat])
                        kb.op(dve, lambda e: e.tensor_reduce(out=m2[:], in_=gat[:], axis=AX.X, op=ALU.add), R=[gat], W=[m2])
                        kb.op(dve, lambda e: e.reciprocal(m1[:], m2[:]), R=[m2], W=[m1])
                        kb.op(dve, lambda e: e.tensor_tensor(out=gat[:], in0=gat[:],
                                                             in1=m1[:].unsqueeze(2).to_broadcast([128, 4, 8]),
                                                             op=ALU.mult), R=[gat, m1], W=[gat])
                        if b == 0:
                            dbg_store("gat", gat[:].rearrange("p a b -> p (a b)"), gat)
                        kb.op(dve, lambda e: e.tensor_copy(gatb[:], gat[:].unsqueeze(3).to_broadcast([128, 4, 8, 128])),
                              R=[gat], W=[gatb])
                        for ex in range(NEXP):
                            pgt = nps()
                            for jl in range(4):
                                kb.op(pe, lambda e, jl=jl, ex=ex, pgt=pgt: e.matmul(
                                    pgt[:, jl * 128:(jl + 1) * 128], gatb[:, jl, ex, :], identb[:], start=True, stop=True),
                                    R=[gatb, identb] if jl in (0, 3) else (), W=[pgt], mark=(jl == 3))
                            kb.op(act, lambda e, ex=ex, pgt=pgt: e.activation(out=gsb[:, ex, :], in_=pgt[:], func=AF.Copy),
                                  R=[pgt], W=[gsb])
                        for ex in range(NEXP):
                            ffn_expert(h1b, moe_w1[j, ex], moe_w3[j, ex], moe_w2[j, ex], ex, acc)

                    def outf2(c, acc=acc):
                        kb.op(act, lambda e: e.activation(out=acc[:, c, :], in_=acc[:, c, :], func=AF.Identity,
                                                          scale=vt[:, C_L2G + c:C_L2G + c + 1],
                                                          bias=vt[:, C_L2B + c:C_L2B + c + 1]), R=[acc, vt], W=[acc])
                    wk["tb"].k = tbq.k
                    wk["tsq"].k = tbq.k
                    ln_fm(ph, acc, 8, ones1k, None, None, wk, outf2)
                    dst = OUT if last_layer else H
                    kb.dma(sp, fm(dst)[:, :, b * 512:(b + 1) * 512], acc[:], R=[acc], owner=acc)
                kb.end_phase()

        I32 = mybir.dt.int32
        CB = 192
        NS = NB * CB
        H1T = dram("H1T", [NB * 4, 128, D], BF16, "Internal")
        YD = dram("YD", [NEXP, NS, D], BF16, "Internal")
        iota_p128 = kb.sb(es, "iota_p128", [128, 1], F32)
        kb.op(dve, lambda e: e.tensor_scalar(out=iota_p128[:], in0=iota_p[:], scalar1=128.0, scalar2=None, op0=ALU.add),
              R=[iota_p], W=[iota_p128])
        iota_p128.k.const = True

        def moe_layer(l, last_layer):
            j = l // 2
            NF = FFE // 128
            NTL = NB * 4
            with ExitStack() as lay:
                posall = kb.sb(lay, "posall", [128, NTL, 8], F32)
                gatall = kb.sb(lay, "gatall", [128, NTL, 8], F32)
                kb.phase_tiles = []
                with ExitStack() as ph:
                    vt = load_vec(ph, l)
                    wg = kb.sb(ph, "wg", [128, 8, D], BF16)
                    wgv = ple_gate_w[l].rearrange("(c p) n -> p c n", p=128)
                    for g in range(2):
                        load_w(wg, wg[:, :, g * 512:(g + 1) * 512], wgv[:, :, g * 512:(g + 1) * 512])
                    wp = kb.sb(ph, "wp", [128, 2, D], BF16)
                    load_w(wp, wp[:], ple_w[l].rearrange("(c p) n -> p c n", p=128))
                    wr = kb.sb(ph, "wr", [128, 8, NEXP], BF16)
                    load_w(wr, wr[:], w_router[j].rearrange("(c p) n -> p c n", p=128))
                    for t_ in (wg, wp, wr):
                        t_.k.const = True
                    lg = kb.sb(ph, "lg", [128, 4, 8], F32)
                    lg2 = kb.sb(ph, "lg2", [128, 4, 8], F32)
                    m1 = kb.sb(ph, "m1", [128, 4], F32)
                    m2 = kb.sb(ph, "m2", [128, 4], F32)
                    gat = kb.sb(ph, "gat", [128, 4, 8], F32)
                    selb = kb.sb(ph, "selb", [128, 4, 8], BF16)
                    posf = kb.sb(ph, "posf", [128, 4, 8], F32)
                    msk = kb.sb(ph, "msk", [128, 4, 8], F32)
                    h1bs = Ring([kb.sb(ph, "h1b%d" % i, [128, 8, 512], BF16) for i in range(2)])
                    pbs = Ring([kb.sb(ph, "pb%d" % i, [128, 2, 512], BF16) for i in range(2)])
                    accs_ = Ring([kb.sb(ph, "accA%d" % i, [128, 8, 512], F32) for i in range(2)])
                    sgl = Ring([kb.sb(ph, "sgl%d" % i, [128, 512], F32) for i in range(2)])
                    h1tm = Ring([kb.sb(ph, "h1tm%d" % i, [128, 4, D], BF16) for i in range(2)])
                    PTR = [Tl(PSB.t[:, i * 512:(i + 1) * 512].bitcast(BF16)) for i in range(4)]
                    Hf = fm(H)

                    def loadsA(b):
                        h1b, pb, acc = h1bs.next(), pbs.next(), accs_.next()
                        kb.dma(pool, h1b[:], Hf[:, :, b * 512:(b + 1) * 512], W=[h1b], owner=h1b)
                        kb.dma(pool, pb[:], pT[l].rearrange("(c p) t -> p c t", p=128)[:, :, b * 512:(b + 1) * 512],
                               W=[pb], owner=pb)
                        kb.dma(sp, acc[:], Hf[:, :, b * 512:(b + 1) * 512], W=[acc], owner=acc)
                        return h1b, pb, acc
                    nxt = loadsA(0)
                    for b in range(NB):
                        h1b, pb, acc = nxt
                        if b + 1 < NB:
                            nxt = loadsA(b + 1)
                        for oc in range(8):
                            pg_ = nps()
                            mm_acc(pg_[:], pg_, [(wg[:, kc, oc * 128:(oc + 1) * 128], h1b[:, kc, :]) for kc in range(8)],
                                   [wg, h1b])
                            pp_ = nps()
                            mm_acc(pp_[:], pp_, [(wp[:, kc, oc * 128:(oc + 1) * 128], pb[:, kc, :]) for kc in range(2)],
                                   [wp, pb])
                            s_ = sgl.next()
                            kb.op(act, lambda e, s_=s_, pg_=pg_, oc=oc: e.activation(
                                out=s_[:], in_=pg_[:], func=AF.Sigmoid, bias=vt[:, C_PGB + oc:C_PGB + oc + 1], scale=1.0),
                                R=[pg_, vt], W=[s_])
                            kb.op(dve, lambda e, s_=s_, pp_=pp_: e.tensor_tensor(out=s_[:], in0=pp_[:], in1=s_[:], op=ALU.mult),
                                  R=[pp_, s_], W=[s_])
                            kb.op(dve, lambda e, s_=s_, oc=oc, acc=acc: e.scalar_tensor_tensor(
                                out=acc[:, oc, :], in0=acc[:, oc, :], scalar=ALPHA, in1=s_[:], op0=ALU.mult, op1=ALU.add),
                                R=[acc, s_], W=[acc])
                        kb.dma(sp, Hf[:, :, b * 512:(b + 1) * 512], acc[:], R=[acc], owner=acc)
                        ht = h1tm.next()
                        for jl in range(4):
                            ptr = PTR[jl]
                            for c in range(8):
                                kb.op(pe, lambda e, jl=jl, c=c, ptr=ptr: e.transpose(
                                    ptr[:, c * 128:(c + 1) * 128], h1b[:, c, jl * 128:(jl + 1) * 128], identb[:]),
                                    R=[h1b, identb] if c in (0, 7) else (), W=[ptr], mark=(c == 7))
                            kb.op(act if jl % 2 == 0 else dve,
                                  (lambda e, jl=jl, ptr=ptr, ht=ht: e.activation(out=ht[:, jl, :], in_=ptr[:], func=AF.Copy))
                                  if jl % 2 == 0 else
                                  (lambda e, jl=jl, ptr=ptr, ht=ht: e.tensor_copy(ht[:, jl, :], ptr[:])),
                                  R=[ptr], W=[ht])
                        kb.dma(sp, H1T[b * 4:(b + 1) * 4].rearrange("j p n -> p j n"), ht[:], R=[ht], owner=ht)
                        for jl in range(4):
                            pr = nps()
                            mm_acc(pr[:, 0:8], pr, [(h1b[:, kc, jl * 128:(jl + 1) * 128], wr[:, kc, :]) for kc in range(8)],
                                   [wr, h1b])
                            kb.op(act, lambda e, pr=pr, jl=jl: e.activation(out=lg[:, jl, :], in_=pr[:, 0:8], func=AF.Copy),
                                  R=[pr], W=[lg])
                        bc = lambda t_: t_[:].unsqueeze(2).to_broadcast([128, 4, 8])
                        kb.op(dve, lambda e: e.tensor_reduce(out=m1[:], in_=lg[:], axis=AX.X, op=ALU.max), R=[lg], W=[m1])
                        kb.op(dve, lambda e: e.tensor_tensor(out=lg2[:], in0=lg[:], in1=bc(m1), op=ALU.is_equal),
                              R=[lg, m1], W=[lg2])
                        kb.op(dve, lambda e: e.scalar_tensor_tensor(out=lg2[:], in0=lg2[:], scalar=-1e9, in1=lg[:],
                                                                    op0=ALU.mult, op1=ALU.add), R=[lg, lg2], W=[lg2])
                        kb.op(dve, lambda e: e.tensor_reduce(out=m2[:], in_=lg2[:], axis=AX.X, op=ALU.max), R=[lg2], W=[m2])
                        kb.op(dve, lambda e: e.tensor_tensor(out=lg2[:], in0=lg[:], in1=bc(m2), op=ALU.is_ge),
                              R=[lg, m2], W=[lg2])
                        kb.op(dve, lambda e: e.tensor_tensor(out=gat[:], in0=lg[:], in1=bc(m1), op=ALU.subtract),
                              R=[lg, m1], W=[gat])
                        kb.op(act, lambda e: e.activation(out=gat[:], in_=gat[:], func=AF.Exp), R=[gat], W=[gat])
                        kb.op(dve, lambda e: e.tensor_tensor(out=gat[:], in0=gat[:], in1=lg2[:], op=ALU.mult),
                              R=[gat, lg2], W=[gat])
                        kb.op(dve, lambda e: e.tensor_reduce(out=m2[:], in_=gat[:], axis=AX.X, op=ALU.add), R=[gat], W=[m2])
                        kb.op(dve, lambda e: e.reciprocal(m1[:], m2[:]), R=[m2], W=[m1])
                        kb.op(dve, lambda e, b=b: e.tensor_tensor(out=gatall[:, b * 4:(b + 1) * 4, :], in0=gat[:], in1=bc(m1),
                                                                  op=ALU.mult), R=[gat, m1], W=[gatall])
                        if b == 0:
                            dbg_store("gat", gatall[:, 0:4, :].rearrange("p a b -> p (a b)"), gatall)
                        kb.op(dve, lambda e: e.tensor_copy(selb[:], lg2[:]), R=[lg2], W=[selb])
                        pp = nps()
                        for jl in range(4):
                            prs = [(UT[:], selb[:, jl, :])] + [(ones1[:], selb[:, j2, :]) for j2 in range(jl)]
                            mm_acc(pp[:, jl * 8:(jl + 1) * 8], pp, prs, [UT, ones1, selb])
                        kb.op(dve, lambda e, pp=pp: e.tensor_scalar(
                            out=msk[:], in0=pp[:, 0:32].rearrange("p (a b) -> p a b", a=4), scalar1=float(CB), scalar2=None,
                            op0=ALU.is_lt), R=[pp], W=[msk])
                        kb.op(dve, lambda e: e.tensor_tensor(out=msk[:], in0=msk[:], in1=lg2[:], op=ALU.mult),
                              R=[msk, lg2], W=[msk])
                        kb.op(dve, lambda e, pp=pp: e.scalar_tensor_tensor(
                            out=posf[:], in0=pp[:, 0:32].rearrange("p (a b) -> p a b", a=4), scalar=1.0, in1=msk[:],
                            op0=ALU.add, op1=ALU.mult), R=[pp, msk], W=[posf])
                        kb.op(dve, lambda e, b=b: e.tensor_scalar(out=posall[:, b * 4:(b + 1) * 4, :], in0=posf[:],
                                                                  scalar1=-1.0, scalar2=None, op0=ALU.add),
                              R=[posf], W=[posall])
                    dbg_store("pos", posall[:].rearrange("p a b -> p (a b)"), posall)
                    kb.end_phase()
                if os.environ.get("K_MOE_STOP") == "A":
                    return
                with ExitStack() as ph:
                    xg = kb.sb(ph, "xg", [128, 8, NS], BF16)
                    actb = kb.sb(ph, "actb", [128, NF, NS], BF16)
                    wsr = Ring([kb.sb(ph, "ws%d" % i, [128, 8, 512], BF16) for i in range(5)])
                    w1s = w3s = w2s = wsr
                    pts = Ring([kb.sb(ph, "pt%d" % i, [128, CB], BF16) for i in range(4)])
                    hts = Ring([kb.sb(ph, "ht%d" % i, [128, 4, D], BF16) for i in range(2)])
                    sgl = Ring([kb.sb(ph, "sgl%d" % i, [128, 512], F32) for i in range(2)])
                    yst = Ring([kb.sb(ph, "yst%d" % i, [128, 512], BF16) for i in range(4)])
                    ACC4 = [Tl(PSB.t[:, i * 512:(i + 1) * 512]) for i in range(4)]
                    ACC8 = PS + ACC4
                    nblk = [(s0, min(512, NS - s0)) for s0 in range(0, NS, 512)]
                    ntile = NS // 128
                    for ex in range(NEXP):
                        def load_ht(b_):
                            ht_ = hts.next()
                            kb.dma(sp, ht_[:], H1T[b_ * 4:(b_ + 1) * 4].rearrange("j p n -> p j n"), W=[ht_], owner=ht_)
                            return ht_
                        htn = load_ht(0)
                        for b in range(NB):
                            htb = htn
                            if b + 1 < NB:
                                htn = load_ht(b + 1)
                            tls = []
                            for t4 in range(4):
                                tl = b * 4 + t4
                                pt_ = pts.next()
                                kb.op(dve, lambda e, pt_=pt_, tl=tl, ex=ex: e.tensor_scalar(
                                    out=pt_[:], in0=iota_f[:, 0:CB], scalar1=posall[:, tl, ex:ex + 1], scalar2=None,
                                    op0=ALU.is_equal), R=[iota_f, posall], W=[pt_])
                                tls.append((pt_, t4))
                            for c in range(8):
                                bk = ACC4[c // 2]
                                mm_acc(bk[:, (c % 2) * 256:(c % 2) * 256 + CB], bk,
                                       [(htb[:, t4_, c * 128:(c + 1) * 128], pt_[:]) for (pt_, t4_) in tls],
                                       [htb] + [pt_ for (pt_, t4_) in tls])
                            gview = PSB.t[:].rearrange("p (c w) -> p c w", w=256)[:, :, 0:CB]
                            kb.op(act if b % 2 == 0 else dve,
                                  (lambda e, b=b: e.activation(out=xg[:, :, b * CB:(b + 1) * CB], in_=gview, func=AF.Copy))
                                  if b % 2 == 0 else
                                  (lambda e, b=b: e.tensor_copy(xg[:, :, b * CB:(b + 1) * CB], gview)),
                                  R=ACC4, W=[xg])
                        w1v = moe_w1[j, ex].rearrange("(c p) n -> p c n", p=128)
                        w3v = moe_w3[j, ex].rearrange("(c p) n -> p c n", p=128)
                        for fg in range(NF // 4):
                            a, c3 = w1s.next(), w3s.next()
                            load_w(a, a[:], w1v[:, :, fg * 512:(fg + 1) * 512])
                            load_w(c3, c3[:], w3v[:, :, fg * 512:(fg + 1) * 512])
                            for (s0, n) in nblk:
                                xs = slice(s0, s0 + n)
                                for fc in range(4):
                                    f = fg * 4 + fc
                                    pg_ = nps()
                                    mm_acc(pg_[:, 0:n], pg_, [(a[:, kc, fc * 128:(fc + 1) * 128], xg[:, kc, xs]) for kc in range(8)],
                                           [a, xg])
                                    pu_ = nps()
                                    mm_acc(pu_[:, 0:n], pu_, [(c3[:, kc, fc * 128:(fc + 1) * 128], xg[:, kc, xs]) for kc in range(8)],
                                           [c3, xg])
                                    s_ = sgl.next()
                                    kb.op(act, lambda e, s_=s_, pg_=pg_, n=n: e.activation(out=s_[:, 0:n], in_=pg_[:, 0:n], func=AF.Silu),
                                          R=[pg_], W=[s_])
                                    kb.op(dve, lambda e, s_=s_, pu_=pu_, f=f, xs=xs, n=n: e.tensor_tensor(
                                        out=actb[:, f, xs], in0=pu_[:, 0:n], in1=s_[:, 0:n], op=ALU.mult), R=[pu_, s_], W=[actb])
                        ngrp = (NF + 7) // 8
                        for half in range(2):
                            for t0 in range(0, ntile, 8):
                                tiles = list(range(t0, min(ntile, t0 + 8)))
                                for fg in range(ngrp):
                                    nfc = min(8, NF - fg * 8)
                                    w2t = w2s.next()
                                    load_w(w2t, w2t[:, 0:nfc, :],
                                           moe_w2[j, ex][fg * 1024:fg * 1024 + nfc * 128, half * 512:(half + 1) * 512].rearrange(
                                               "(c p) n -> p c n", p=128))
                                    for st in tiles:
                                        ac = ACC8[st - t0]
                                        for fc in range(nfc):
                                            first = (fg == 0 and fc == 0)
                                            lastm = (fg == ngrp - 1 and fc == nfc - 1)
                                            lastg = (fc == nfc - 1)
                                            kb.op(pe, lambda e, st=st, fc=fc, fg=fg, first=first, lastm=lastm, w2t=w2t, ac=ac:
                                                  e.matmul(ac[:], actb[:, fg * 8 + fc, st * 128:(st + 1) * 128], w2t[:, fc, :],
                                                           start=first, stop=lastm),
                                                  R=[w2t, actb] if (fc == 0 or lastg) else (), W=[ac], mark=lastg)
                                for st in tiles:
                                    ac = ACC8[st - t0]
                                    yt = yst.next()
                                    kb.op(act if st % 2 == 0 else dve,
                                          (lambda e, ac=ac, yt=yt: e.activation(out=yt[:], in_=ac[:], func=AF.Copy))
                                          if st % 2 == 0 else
                                          (lambda e, ac=ac, yt=yt: e.tensor_copy(yt[:], ac[:])),
                                          R=[ac], W=[yt])
                                    kb.dma(sp, YD[ex][st * 128:(st + 1) * 128, half * 512:(half + 1) * 512], yt[:],
                                           R=[yt], owner=yt)
                    kb.end_phase()
                if os.environ.get("K_MOE_STOP") == "B":
                    return
                with ExitStack() as ph:
                    vt = load_vec(ph, l)
                    acc = kb.sb(ph, "acc", [128, 8, 512], F32)
                    brdR = Ring([kb.sb(ph, "brd%d" % i, [128, 2, 4, 8, 128], BF16) for i in range(2)])
                    gsbR = Ring([kb.sb(ph, "gsb%d" % i, [128, 512], F32) for i in range(3)])
                    pgaR = Ring([kb.sb(ph, "pga%d" % i, [128, 8, 512], BF16) for i in range(2)])
                    pgbR = Ring([kb.sb(ph, "pgb%d" % i, [64, 8, 512], BF16) for i in range(2)])
                    yas = Ring([kb.sb(ph, "ya%d" % i, [128, 8, D], BF16) for i in range(2)])
                    ybs = Ring([kb.sb(ph, "yb%d" % i, [64, 8, D], BF16) for i in range(2)])
                    tbq = kb.sb(ph, "tbq", [128, 16, 512], BF16)
                    wk = dict(tb=Tl(tbq.t[:, 0:8, :]), tsq=Tl(tbq.t[:, 8:16, :]),
                              mean=kb.sb(ph, "mean", [128, 512], F32), msq=kb.sb(ph, "msq", [128, 512], F32),
                              rstd=kb.sb(ph, "rstd", [128, 512], F32))
                    wk["tb"].k = tbq.k
                    wk["tsq"].k = tbq.k
                    ACCS = [Tl(PSB.t[:, i * 512:(i + 1) * 512]) for i in range(4)]
                    Hf = fm(H)

                    def loadsC(b):
                        ya, yb_ = yas.next(), ybs.next()
                        for ex in range(NEXP):
                            kb.dma(sp, ya[:, ex, :], YD[ex][b * CB:b * CB + 128, :], W=[ya], owner=ya)
                            kb.dma(sp, yb_[:, ex, :], YD[ex][b * CB + 128:b * CB + CB, :], W=[yb_], owner=yb_)
                        return ya, yb_
                    nxt = loadsC(0)
                    for b in range(NB):
                        ya, yb_ = nxt
                        if b + 1 < NB:
                            nxt = loadsC(b + 1)
                        kb.dma(sp, acc[:], Hf[:, :, b * 512:(b + 1) * 512], W=[acc], owner=acc)
                        brd, pga, pgb = brdR.next(), pgaR.next(), pgbR.next()
                        for qi, src in enumerate((posall, gatall)):
                            kb.op(dve if qi == 0 else pool, lambda e, qi=qi, src=src, b=b, brd=brd: e.tensor_copy(
                                brd[:, qi, :, :, :], src[:, b * 4:(b + 1) * 4, :].unsqueeze(3).to_broadcast([128, 4, 8, 128])),
                                R=[src], W=[brd])
                        for ex in range(NEXP):
                            pbs_ = []
                            for qi in range(2):
                                pq = nps()
                                for jl in range(4):
                                    kb.op(pe, lambda e, jl=jl, ex=ex, qi=qi, pq=pq, brd=brd: e.matmul(
                                        pq[:, jl * 128:(jl + 1) * 128], brd[:, qi, jl, ex, :], identb[:], start=True, stop=True),
                                        R=[brd, identb] if jl in (0, 3) else (), W=[pq], mark=(jl == 3))
                                pbs_.append(pq)
                            gsb = gsbR.next()
                            kb.op(act, lambda e, pq=pbs_[1], gsb=gsb: e.activation(out=gsb[:], in_=pq[:], func=AF.Copy),
                                  R=[pbs_[1]], W=[gsb])
                            kb.op(dve, lambda e, ex=ex, pq=pbs_[0], gsb=gsb, pga=pga: e.scalar_tensor_tensor(
                                out=pga[:, ex, :], in0=pq[:], scalar=iota_p[:, 0:1], in1=gsb[:], op0=ALU.is_equal, op1=ALU.mult),
                                R=[pbs_[0], iota_p, gsb], W=[pga])
                            kb.op(dve, lambda e, ex=ex, pq=pbs_[0], gsb=gsb, pgb=pgb: e.scalar_tensor_tensor(
                                out=pgb[:, ex, :], in0=pq[0:64, :], scalar=iota_p128[0:64, 0:1], in1=gsb[0:64, :],
                                op0=ALU.is_equal, op1=ALU.mult), R=[pbs_[0], iota_p128, gsb], W=[pgb])
                        for half in range(2):
                            for o4 in range(4):
                                oc = half * 4 + o4
                                prs = []
                                for ex in range(NEXP):
                                    prs.append((ya[:, ex, oc * 128:(oc + 1) * 128], pga[:, ex, :]))
                                    prs.append((yb_[0:64, ex, oc * 128:(oc + 1) * 128], pgb[0:64, ex, :]))
                                mm_acc(ACCS[o4][:], ACCS[o4], prs, [ya, yb_, pga, pgb])
                                kb.op(dve, lambda e, o4=o4, oc=oc: e.tensor_tensor(
                                    out=acc[:, oc, :], in0=ACCS[o4][:], in1=acc[:, oc, :], op=ALU.add),
                                    R=[ACCS[o4], acc], W=[acc])

                        def outf2(c):
                            kb.op(act, lambda e: e.activation(out=acc[:, c, :], in_=acc[:, c, :], func=AF.Identity,
                                                              scale=vt[:, C_L2G + c:C_L2G + c + 1],
                                                              bias=vt[:, C_L2B + c:C_L2B + c + 1]), R=[acc, vt], W=[acc])
                        ln_fm(ph, acc, 8, ones1k, None, None, wk, outf2)
                        dst = OUT if last_layer else H
                        kb.dma(sp, fm(dst)[:, :, b * 512:(b + 1) * 512], acc[:], R=[acc], owner=acc)
                    kb.end_phase()

        if phases is None:
            phases = ["0"] + [x for l in layers for x in ("1a%d" % l, "1b%d" % l, "2%d" % l)]
        for phn in phases:
            if phn == "0":
                phase0()
            elif phn.startswith("1a"):
                phase1a(int(phn[2:]))
            elif phn.startswith("1b"):
                phase1b(int(phn[2:]))
            else:
                l_ = int(phn[1:])
                if l_ % 2 == 1 and not os.environ.get("K_DENSE_MOE"):
                    moe_layer(l_, l_ == layers[-1])
                else:
                    phase2(l_, l_ == layers[-1])
        kb.barrier()
    return nc


def _cols(v, C):
    return np.ascontiguousarray(v.reshape(C, 128).T)


def _prep_shared(inp):
    f32 = np.float32
    vecs = np.zeros((DEPTH, 128, NV), f32)
    vb = np.zeros((DEPTH, 128, 512), f32)
    g2 = np.full((DEPTH, 128, 8, 14, 64), MASKV, f32)
    kc = np.arange(64)[:, None]
    qc = np.arange(64)[None, :]
    win0 = np.clip(qc - 8, 0, 48)
    colvalid = (kc >= win0) & (kc < win0 + 16)
    dc = np.clip(kc - qc + 15, 0, 30)
    for l in range(DEPTH):
        v = vecs[l]
        v[:, C_BIN:C_BIN + 20] = _cols(inp["b_in"][l], 20)
        v[:, C_CW:C_CW + 124] = inp["conv_w"][l].T.reshape(4, 128, 31).transpose(1, 0, 2).reshape(128, 124)
        v[:, C_CB:C_CB + 4] = _cols(inp["conv_b"][l], 4)
        v[:, C_CG:C_CG + 4] = _cols(inp["conv_ln_g"][l], 4)
        v[:, C_CBB:C_CBB + 4] = _cols(inp["conv_ln_b"][l], 4)
        v[:, C_BOUT:C_BOUT + 8] = _cols(inp["b_out"][l], 8)
        v[:, C_L1G:C_L1G + 8] = _cols(inp["ln1_g"][l], 8)
        v[:, C_L1B:C_L1B + 8] = _cols(inp["ln1_b"][l], 8)
        v[:, C_PGB:C_PGB + 8] = _cols(inp["ple_gate_b"][l], 8)
        v[:, C_L2G:C_L2G + 8] = _cols(inp["ln2_g"][l], 8)
        v[:, C_L2B:C_L2B + 8] = _cols(inp["ln2_b"][l], 8)
        v[:, C_LIG:C_LIG + 8] = _cols(inp["ln_in_g"], 8)
        v[:, C_LIB:C_LIB + 8] = _cols(inp["ln_in_b"], 8)
        vb[l] = np.broadcast_to(inp["b_in"][l][2048:2560][None, :], (128, 512))
        rpb = inp["rpb"][l]
        for kr in range(2):
            for ei in range(14):
                dr = ei - 7 + kr
                if dr < -7 or dr > 7:
                    continue
                tab = rpb[:, dr + 7, :][:, dc]
                tab = np.where(colvalid[None], tab, f32(MASKV))
                g2[l, kr * 64:(kr + 1) * 64, :, ei, :] = tab.transpose(1, 0, 2)
    return vecs, vb, g2.reshape(DEPTH, 128, 8 * 14 * 64)


_NC_CACHE = {}


def kernel(**inp):
    inp = {k: np.asarray(v) for k, v in inp.items()}
    NB = 10
    NT = NB * 512
    key = ("full",)
    if key not in _NC_CACHE:
        _NC_CACHE[key] = build_nc(NB, (0, 1, 2, 3))
    nc = _NC_CACHE[key]
    vecs, vb, g2 = _prep_shared(inp)
    shared = dict(vecs=vecs, vbias=vb, g2h=g2)
    for n in ("w_in", "w_out", "ffn_w1", "ffn_w3", "ffn_w2", "w_router", "moe_w1", "moe_w3", "moe_w2", "ple_w",
              "ple_gate_w"):
        shared[n] = np.ascontiguousarray(inp[n], dtype=np.float32)
    in_maps = []
    for c in range(8):
        b, half = c // 2, c % 2
        t0 = 0 if half == 0 else 48 * 64
        m = dict(shared)
        m["xT"] = np.ascontiguousarray(inp["x"][b, t0:t0 + NT, :].T)
        m["pT"] = np.ascontiguousarray(inp["p"][:, b, t0:t0 + NT, :].transpose(0, 2, 1))
        in_maps.append(m)
    res = run_bass_kernel_spmd(nc, in_maps, core_ids=list(range(8)))
    out = np.empty((4, 8192, D), np.float32)
    for c in range(8):
        b, half = c // 2, c % 2
        o = res.results[c]["outT"]
        if half == 0:
            out[b, 0:4096, :] = o[:, 0:4096].T
        else:
            out[b, 4096:8192, :] = o[:, 1024:5120].T
    return out
```

```python
import os
import numpy as np
from contextlib import ExitStack
import concourse.bass as bass
import concourse.mybir as mybir
from concourse.bass_utils import run_bass_kernel_spmd

F32 = mybir.dt.float32
BF16 = mybir.dt.bfloat16
AF = mybir.ActivationFunctionType
ALU = mybir.AluOpType
AX = mybir.AxisListType

DEPTH = 4
D = 1024
DIN = 2560
NEXP = 8
FF = 2816
FFE = 3584
DPLE = 256
ALPHA = (2.0 * DEPTH) ** 0.25
EPS = 1e-5
MASKV = -200.0
CAP = 1536
NCH = CAP // 128
NV = 220
C_BIN, C_CW, C_CB, C_CG, C_CBB, C_BOUT, C_L1G, C_L1B, C_PGB, C_L2G, C_L2B, C_LIG, C_LIB = (
    0, 20, 144, 148, 152, 156, 164, 172, 180, 188, 196, 204, 212)


class Trk:
    __slots__ = ("w", "r", "dsem", "dcnt", "const")

    def __init__(self, const=False):
        self.w = None
        self.r = []
        self.dsem = None
        self.dcnt = 0
        self.const = const


class Tl:
    def __init__(self, t, const=False):
        self.t = t
        self.k = Trk(const)

    def __getitem__(self, key):
        return self.t[key]


class Eng:
    def __init__(self, name, eng, sem):
        self.name = name
        self.eng = eng
        self.sem = sem
        self.cnt = 0
        self.seen = {}


class KB:
    def __init__(self, nc, es):
        self.nc = nc
        self.es = es
        self.sems = []
        mk = lambda n: es.enter_context(nc.semaphore(n))
        self.pe = Eng("pe", nc.tensor, mk("s_pe"))
        self.act = Eng("act", nc.scalar, mk("s_act"))
        self.dve = Eng("dve", nc.vector, mk("s_dve"))
        self.pool = Eng("pool", nc.gpsimd, mk("s_pool"))
        self.sp = Eng("sp", nc.sync, mk("s_sp"))
        self.engs = [self.pe, self.act, self.dve, self.pool, self.sp]
        self.dsems = []
        self.free_dsems = []
        self.nsem = 0
        self.phase_tiles = []

    def sb(self, es, name, shape, dt, const=False):
        self.uid = getattr(self, "uid", 0) + 1
        t = Tl(es.enter_context(self.nc.sbuf_tensor("%s_%d" % (name, self.uid), shape, dt)), const)
        if es is not self.es:
            self.phase_tiles.append(t)
        return t

    def ps(self, es, name, shape, dt):
        return Tl(es.enter_context(self.nc.psum_tensor(name, shape, dt)))

    def _dsem(self, trk):
        if trk.dsem is None:
            if self.free_dsems:
                trk.dsem, trk.dcnt = self.free_dsems.pop()
            else:
                self.nsem += 1
                trk.dsem = self.es.enter_context(self.nc.semaphore("d%d" % self.nsem))
                trk.dcnt = 0
            self.dsems.append(trk)
        return trk.dsem

    def end_phase(self):
        self.barrier()
        for t in self.phase_tiles:
            if t.k.dsem is not None:
                self.free_dsems.append((t.k.dsem, t.k.dcnt))
                self.dsems.remove(t.k)
                t.k.dsem = None
        self.phase_tiles = []

    def _need(self, E, dep):
        if dep is None:
            return
        sem, val = dep
        key = id(sem)
        if E.seen.get(key, 0) < val:
            E.eng.wait_ge(sem, val)
            E.seen[key] = val

    def _resolve(self, E, R, W):
        for t in R:
            self._need(E, t.k.w)
        for t in W:
            self._need(E, t.k.w)
            for d in t.k.r:
                self._need(E, d)

    def _record(self, v, R, W):
        for t in R:
            if not t.k.const:
                t.k.r.append(v)
        for t in W:
            t.k.w = v
            t.k.r = []

    def op(self, E, fn, R=(), W=(), mark=True):
        self._resolve(E, R, W)
        ins = fn(E.eng)
        if mark:
            E.cnt += 1
            ins.then_inc(E.sem, 1)
            v = (E.sem, E.cnt)
            E.seen[id(E.sem)] = E.seen.get(id(E.sem), 0)
            self._record(v, R, W)
        return ins

    def dma(self, Q, out, in_, R=(), W=(), owner=None):
        self._resolve(Q, R, W)
        sem = self._dsem(owner.k)
        ins = Q.eng.dma_start(out=out, in_=in_)
        owner.k.dcnt += 16
        ins.then_inc(sem, 16)
        v = (sem, owner.k.dcnt)
        self._record(v, R, W)
        return ins

    def barrier(self):
        deps = [(e.sem, e.cnt) for e in self.engs if e.cnt > 0]
        deps += [(t.dsem, t.dcnt) for t in self.dsems if t.dcnt > 0]
        for E in self.engs:
            for d in deps:
                if d[0] is E.sem:
                    continue
                self._need(E, d)


class Ring:
    def __init__(self, tiles):
        self.tiles = tiles
        self.i = 0

    def next(self):
        t = self.tiles[self.i % len(self.tiles)]
        self.i += 1
        return t


def build_nc(NB=10, layers=(0, 1, 2, 3), dbg=False, phases=None):
    NT = NB * 512
    NROW = NB * 8
    nc = bass.Bass("TRN2", target_bir_lowering=False)
    dram = lambda n, s, d, k: nc.dram_tensor(n, s, d, kind=k).ap()
    xT = dram("xT", [D, NT], F32, "ExternalInput")
    pT = dram("pT", [DEPTH, DPLE, NT], F32, "ExternalInput")
    vecs = dram("vecs", [DEPTH, 128, NV], F32, "ExternalInput")
    vbias = dram("vbias", [DEPTH, 128, 512], F32, "ExternalInput")
    g2h = dram("g2h", [DEPTH, 128, 8 * 14 * 64], F32, "ExternalInput")
    w_in = dram("w_in", [DEPTH, D, DIN], F32, "ExternalInput")
    w_out = dram("w_out", [DEPTH, D, D], F32, "ExternalInput")
    ffn_w1 = dram("ffn_w1", [2, D, FF], F32, "ExternalInput")
    ffn_w3 = dram("ffn_w3", [2, D, FF], F32, "ExternalInput")
    ffn_w2 = dram("ffn_w2", [2, FF, D], F32, "ExternalInput")
    w_router = dram("w_router", [2, D, NEXP], F32, "ExternalInput")
    moe_w1 = dram("moe_w1", [2, NEXP, D, FFE], F32, "ExternalInput")
    moe_w3 = dram("moe_w3", [2, NEXP, D, FFE], F32, "ExternalInput")
    moe_w2 = dram("moe_w2", [2, NEXP, FFE, D], F32, "ExternalInput")
    ple_w = dram("ple_w", [DEPTH, DPLE, D], F32, "ExternalInput")
    ple_gate_w = dram("ple_gate_w", [DEPTH, D, D], F32, "ExternalInput")
    OUT = dram("outT", [D, NT], F32, "ExternalOutput")
    SK = "ExternalOutput" if dbg else "Internal"
    H = dram("H", [D, NT], F32, SK)
    U = dram("U", [512, NT + 30], BF16, SK)
    Qd = dram("Qd", [512, NT], BF16, SK)
    Kd = dram("Kd", [512, NT], BF16, SK)
    Ve = dram("Ve", [NB * 4, 128, 520], BF16, SK)
    Vo = dram("Vo", [NB * 4, 128, 520], BF16, SK)
    dbg_out = {}
    if dbg:
        dbg_out["mix"] = dram("dbg_mix", [128, 8 * 512], F32, "ExternalOutput")
        dbg_out["gat"] = dram("dbg_gat", [128, 32], F32, "ExternalOutput")
        dbg_out["pos"] = dram("dbg_pos", [128, NB * 32], F32, "ExternalOutput")

    fm = lambda ap: ap.rearrange("(c p) t -> p c t", p=128)
    hm = lambda ap: ap.rearrange("(h d) t -> d h t", d=64)

    with ExitStack() as es:
        kb = KB(nc, es)
        pe, act, dve, pool, sp = kb.pe, kb.act, kb.dve, kb.pool, kb.sp
        identf = kb.sb(es, "identf", [128, 128], F32)
        identb = kb.sb(es, "identb", [128, 128], BF16, const=False)
        ones1k = kb.sb(es, "ones1k", [128, 128], BF16)
        ones512 = kb.sb(es, "ones512", [128, 128], BF16)
        zt = kb.sb(es, "zt", [128, 4, 15], BF16)
        PS = [kb.ps(es, "ps%d" % i, [128, 512], F32) for i in range(4)]
        PSB = kb.ps(es, "psb", [128, 2048], F32)
        kb.op(pool, lambda e: e.memset(identf[:], 0.0), W=[identf])
        kb.op(pool, lambda e: e.affine_select(out=identf[:], in_=identf[:], compare_op=ALU.not_equal, fill=1.0,
                                              base=0, pattern=[[-1, 128]], channel_multiplier=1),
              R=[identf], W=[identf])
        kb.op(pool, lambda e: e.tensor_copy(identb[:], identf[:]), R=[identf], W=[identb])
        kb.op(pool, lambda e: e.memset(ones1k[:], 1.0 / 1024.0), W=[ones1k])
        kb.op(pool, lambda e: e.memset(ones512[:], 1.0 / 512.0), W=[ones512])
        kb.op(pool, lambda e: e.memset(zt[:], 0.0), W=[zt])
        Up = U.rearrange("(c p) t -> p c t", p=128)
        kb.dma(sp, Up[:, :, 0:15], zt[:], R=[zt], owner=zt)
        kb.dma(sp, Up[:, :, NT + 15:NT + 30], zt[:], R=[zt], owner=zt)
        identb.k.const = True
        identf.k.const = True
        ones1 = kb.sb(es, "ones1", [128, 128], BF16)
        UT = kb.sb(es, "UT", [128, 128], BF16)
        iota_f = kb.sb(es, "iota_f", [128, 512], F32)
        iota_p = kb.sb(es, "iota_p", [128, 1], F32)
        with ExitStack() as tmp:
            utf = kb.sb(tmp, "utf", [128, 128], F32)
            iota_i = kb.sb(tmp, "iota_i", [128, 512], mybir.dt.int32)
            iota_pi = kb.sb(tmp, "iota_pi", [128, 1], mybir.dt.int32)
            kb.op(pool, lambda e: e.memset(ones1[:], 1.0), W=[ones1])
            kb.op(pool, lambda e: e.memset(utf[:], 1.0), W=[utf])
            kb.op(pool, lambda e: e.affine_select(out=utf[:], in_=utf[:], compare_op=ALU.is_gt, fill=0.0,
                                                  base=0, pattern=[[1, 128]], channel_multiplier=-1), R=[utf], W=[utf])
            kb.op(pool, lambda e: e.tensor_copy(UT[:], utf[:]), R=[utf], W=[UT])
            kb.op(pool, lambda e: e.iota(iota_i[:], pattern=[[1, 512]], base=0, channel_multiplier=0), W=[iota_i])
            kb.op(pool, lambda e: e.tensor_copy(iota_f[:], iota_i[:]), R=[iota_i], W=[iota_f])
            kb.op(pool, lambda e: e.iota(iota_pi[:], pattern=[[0, 1]], base=0, channel_multiplier=1), W=[iota_pi])
            kb.op(pool, lambda e: e.tensor_copy(iota_p[:], iota_pi[:]), R=[iota_pi], W=[iota_p])
            kb.end_phase()
        for t_ in (ones1, UT, iota_f, iota_p):
            t_.k.const = True
        ones1k.k.const = True
        ones512.k.const = True

        psi = [0]

        def nps():
            t = PS[psi[0] % 4]
            psi[0] += 1
            return t

        def mm_acc(ps_ap, pst, pairs, R):
            n = len(pairs)
            for i, (l, r) in enumerate(pairs):
                last = i == n - 1
                kb.op(pe, lambda e, l=l, r=r, i=i, last=last: e.matmul(ps_ap, l, r, start=(i == 0), stop=last),
                      R=R if last else (R if i == 0 else ()), W=[pst], mark=last)

        def ln_fm(ph, t, C, ones, gcol, bcol, wk, out_fn):
            tb, tsq, mean, msq, rstd = wk["tb"], wk["tsq"], wk["mean"], wk["msq"], wk["rstd"]
            kb.op(act, lambda e: e.activation(out=tb[:, 0:C, :], in_=t[:, 0:C, :], func=AF.Copy), R=[t], W=[tb])
            kb.op(pool, lambda e: e.tensor_tensor(out=tsq[:, 0:C, :], in0=t[:, 0:C, :], in1=t[:, 0:C, :], op=ALU.mult),
                  R=[t], W=[tsq])
            pm = nps()
            mm_acc(pm[:], pm, [(ones[:], tb[:, c, :]) for c in range(C)], [tb, ones])
            pq = nps()
            mm_acc(pq[:], pq, [(ones[:], tsq[:, c, :]) for c in range(C)], [tsq, ones])
            kb.op(act, lambda e: e.activation(out=mean[:], in_=pm[:], func=AF.Copy), R=[pm], W=[mean])
            kb.op(dve, lambda e: e.tensor_tensor(out=msq[:], in0=mean[:], in1=mean[:], op=ALU.mult), R=[mean], W=[msq])
            kb.op(dve, lambda e: e.tensor_tensor(out=msq[:], in0=pq[:], in1=msq[:], op=ALU.subtract), R=[pq, msq], W=[msq])
            kb.op(dve, lambda e: e.tensor_scalar(out=msq[:], in0=msq[:], scalar1=0.0, scalar2=EPS, op0=ALU.max, op1=ALU.add),
                  R=[msq], W=[msq])
            kb.op(act, lambda e: e.activation(out=msq[:], in_=msq[:], func=AF.Sqrt), R=[msq], W=[msq])
            kb.op(dve, lambda e: e.reciprocal(rstd[:], msq[:]), R=[msq], W=[rstd])
            for c in range(C):
                kb.op(dve, lambda e, c=c: e.tensor_tensor(out=t[:, c, :], in0=t[:, c, :], in1=mean[:], op=ALU.subtract),
                      R=[t, mean], W=[t])
                kb.op(dve, lambda e, c=c: e.tensor_tensor(out=t[:, c, :], in0=t[:, c, :], in1=rstd[:], op=ALU.mult),
                      R=[t, rstd], W=[t])
                out_fn(c)

        def load_vec(ph, l):
            vt = kb.sb(ph, "vecs", [128, NV], F32)
            kb.dma(sp, vt[:], vecs[l], W=[vt], owner=vt)
            vt.k.const = True
            return vt

        def load_w(tile, view_ap, src_ap):
            kb.dma(pool, view_ap, src_ap, W=[tile], owner=tile)

        def dbg_store(name, tile_ap, tl):
            if name in dbg_out:
                kb.dma(pool, dbg_out[name], tile_ap, R=[tl], owner=tl)

        def phase0():
            with ExitStack() as ph:
                vt = load_vec(ph, 0)
                tt = [kb.sb(ph, "p0t%d" % i, [128, 8, 512], F32) for i in range(2)]
                wk = dict(tb=kb.sb(ph, "tb", [128, 8, 512], BF16), tsq=kb.sb(ph, "tsq", [128, 8, 512], BF16),
                          mean=kb.sb(ph, "mean", [128, 512], F32), msq=kb.sb(ph, "msq", [128, 512], F32),
                          rstd=kb.sb(ph, "rstd", [128, 512], F32))
                for b in range(NB):
                    t = tt[b % 2]
                    kb.dma(sp, t[:], fm(xT)[:, :, b * 512:(b + 1) * 512], W=[t], owner=t)

                    def outf(c, t=t):
                        kb.op(act, lambda e: e.activation(out=t[:, c, :], in_=t[:, c, :], func=AF.Identity,
                                                          scale=vt[:, C_LIG + c:C_LIG + c + 1],
                                                          bias=vt[:, C_LIB + c:C_LIB + c + 1]), R=[t, vt], W=[t])
                    ln_fm(ph, t, 8, ones1k, None, None, wk, outf)
                    kb.dma(sp, fm(H)[:, :, b * 512:(b + 1) * 512], t[:], R=[t], owner=t)
                kb.end_phase()

        def phase1a(l):
            with ExitStack() as ph:
                vt = load_vec(ph, l)
                win = kb.sb(ph, "win", [128, 8, DIN], BF16)
                wv = w_in[l].rearrange("(c p) n -> p c n", p=128)
                for g in range(5):
                    load_w(win, win[:, :, g * 512:(g + 1) * 512], wv[:, :, g * 512:(g + 1) * 512])
                vb = kb.sb(ph, "vb", [128, 512], F32)
                kb.dma(sp, vb[:], vbias[l], W=[vb], owner=vb)
                hbs = Ring([kb.sb(ph, "hb%d" % i, [128, 8, 576], BF16) for i in range(2)])
                sg = kb.sb(ph, "sg", [128, 4, 512], F32)
                usts = Ring([kb.sb(ph, "ust%d" % i, [128, 4, 512], BF16) for i in range(2)])
                qsts = Ring([kb.sb(ph, "qst%d" % i, [128, 4, 512], BF16) for i in range(2)])
                ksts = Ring([kb.sb(ph, "kst%d" % i, [128, 4, 512], BF16) for i in range(2)])
                vsts = Ring([kb.sb(ph, "vst%d" % i, [128, 8, 8, 65], BF16) for i in range(2)])
                for v in vsts.tiles:
                    kb.op(pool, lambda e, v=v: e.memset(v[:], 1.0), W=[v])
                Hf = fm(H)

                def load_hb(b):
                    hb = hbs.next()
                    n = 576 if b < NB - 1 else 512
                    kb.dma(pool, hb[:, :, 0:n], Hf[:, :, b * 512:b * 512 + n], W=[hb], owner=hb)
                    return hb
                hb_next = load_hb(0)
                for b in range(NB):
                    hb = hb_next
                    if b + 1 < NB:
                        hb_next = load_hb(b + 1)
                    ust, qst, kst, vst = usts.next(), qsts.next(), ksts.next(), vsts.next()
                    for oc in (4, 0, 5, 1, 6, 2, 7, 3, 8, 9, 10, 11, 12, 13, 14, 15):
                        p = nps()
                        mm_acc(p[:], p, [(win[:, kc, oc * 128:(oc + 1) * 128], hb[:, kc, 0:512]) for kc in range(8)],
                               [win, hb])
                        bcol = vt[:, C_BIN + oc:C_BIN + oc + 1]
                        if 4 <= oc < 8:
                            kb.op(act, lambda e, p=p, oc=oc, bcol=bcol: e.activation(
                                out=sg[:, oc - 4, :], in_=p[:], func=AF.Sigmoid, bias=bcol, scale=1.0),
                                R=[p, vt], W=[sg])
                        elif oc < 4:
                            kb.op(dve, lambda e, p=p, oc=oc, bcol=bcol: e.scalar_tensor_tensor(
                                out=ust[:, oc, :], in0=p[:], scalar=bcol, in1=sg[:, oc, :], op0=ALU.add, op1=ALU.mult),
                                R=[p, vt, sg], W=[ust])
                        elif oc < 12:
                            kb.op(act, lambda e, p=p, oc=oc, bcol=bcol: e.activation(
                                out=qst[:, oc - 8, :], in_=p[:], func=AF.Identity, bias=bcol, scale=1.0),
                                R=[p, vt], W=[qst])
                        else:
                            kb.op(dve, lambda e, p=p, oc=oc, bcol=bcol: e.tensor_scalar(
                                out=kst[:, oc - 12, :], in0=p[:], scalar1=bcol, scalar2=None, op0=ALU.add),
                                R=[p, vt], W=[kst])
                    kb.dma(sp, Up[:, :, 15 + b * 512:15 + (b + 1) * 512], ust[:], R=[ust], owner=ust)
                    kb.dma(sp, fm(Qd)[:, :, b * 512:(b + 1) * 512], qst[:], R=[qst], owner=qst)
                    kb.dma(sp, fm(Kd)[:, :, b * 512:(b + 1) * 512], kst[:], R=[kst], owner=kst)
                    for s in range(8):
                        jl = s % 4
                        off = jl * 128 + (64 if s >= 4 else 0)
                        if s >= 4 and b == NB - 1 and jl == 3:
                            continue
                        p = nps()
                        mm_acc(p[:], p, [(hb[:, kc, off:off + 128], win[:, kc, 2048:2560]) for kc in range(8)],
                               [win, hb])
                        kb.op(dve, lambda e, p=p, s=s: e.tensor_tensor(
                            out=vst[:, s, :, 0:64], in0=p[:].rearrange("p (h d) -> p h d", h=8),
                            in1=vb[:].rearrange("p (h d) -> p h d", h=8), op=ALU.add), R=[p, vb], W=[vst])
                    vv = vst[:].rearrange("p s h d -> p s (h d)")
                    kb.dma(sp, Ve[b * 4:(b + 1) * 4].rearrange("j p n -> p j n"), vv[:, 0:4, :], R=[vst], owner=vst)
                    kb.dma(sp, Vo[b * 4:(b + 1) * 4].rearrange("j p n -> p j n"), vv[:, 4:8, :], R=[vst], owner=vst)
                kb.end_phase()

        def phase1b(l):
            with ExitStack() as ph:
                vt = load_vec(ph, l)
                wo = kb.sb(ph, "wo", [128, 8, D], BF16)
                wv = w_out[l].rearrange("(c p) n -> p c n", p=128)
                for g in range(2):
                    load_w(wo, wo[:, :, g * 512:(g + 1) * 512], wv[:, :, g * 512:(g + 1) * 512])
                g2 = kb.sb(ph, "g2", [128, 8, 14, 64], BF16)
                with ExitStack() as tmp:
                    g2s = [kb.sb(tmp, "g2s%d" % i, [128, 14 * 64], F32) for i in range(2)]
                    for h in range(8):
                        s = g2s[h % 2]
                        kb.dma(sp, s[:], g2h[l][:, h * 896:(h + 1) * 896], W=[s], owner=s)
                        kb.op(act, lambda e, s=s, h=h: e.activation(out=g2[:, h, :, :].rearrange("p a b -> p (a b)"),
                                                                    in_=s[:], func=AF.Exp), R=[s], W=[g2])
                    kb.barrier()
                g2.k.const = True
                ubs = Ring([kb.sb(ph, "ub%d" % i, [128, 4, 542], BF16) for i in range(2)])
                qbs = Ring([kb.sb(ph, "qb%d" % i, [64, 8, 512], BF16) for i in range(1)])
                kbs = Ring([kb.sb(ph, "kb%d" % i, [64, 8, 960], BF16) for i in range(2)])
                ves = Ring([kb.sb(ph, "ve%d" % i, [128, 8, 520], BF16) for i in range(2)])
                vos = Ring([kb.sb(ph, "vo%d" % i, [128, 8, 520], BF16) for i in range(2)])
                hres_t = kb.sb(ph, "hres", [128, 8, 512], F32)
                y = kb.sb(ph, "y", [128, 4, 512], F32)
                tt = kb.sb(ph, "tt", [128, 8, 512], F32)
                mixT = kb.sb(ph, "mixT", [128, 8, 512], BF16)
                wk = dict(tb=kb.sb(ph, "tb", [128, 8, 512], BF16), tsq=kb.sb(ph, "tsq", [128, 8, 512], BF16),
                          mean=kb.sb(ph, "mean", [128, 512], F32), msq=kb.sb(ph, "msq", [128, 512], F32),
                          rstd=kb.sb(ph, "rstd", [128, 512], F32))
                pexs = Ring([kb.sb(ph, "pex%d" % i, [128, 2, 256], BF16) for i in range(12)])
                dgs = Ring([kb.sb(ph, "dg%d" % i, [128, 128], BF16) for i in range(8)])
                arow = Ring([kb.sb(ph, "arow%d" % i, [64, 8, 64], BF16) for i in range(3)])
                rcp = Ring([kb.sb(ph, "rcp%d" % i, [64, 8], F32) for i in range(3)])
                tmpo = kb.sb(ph, "tmpo", [128, 512], F32)
                PSO = Tl(PSB.t[:, 0:1024])
                PST = Tl(PSB.t[:, 1024:1536].bitcast(BF16)[:, 0:256])

                def loads(b):
                    ub, qb, kbt, ve, vo = ubs.next(), qbs.next(), kbs.next(), ves.next(), vos.next()
                    kb.dma(sp, ub[:], Up[:, :, b * 512:b * 512 + 542], W=[ub], owner=ub)
                    klo = max(0, 8 * b - 4)
                    khi = min(NROW, 8 * b + 11)
                    kb.dma(sp, kbt[:, :, 0:(khi - klo) * 64], hm(Kd)[:, :, klo * 64:khi * 64], W=[kbt], owner=kbt)
                    jlo = klo // 2
                    jhi = min(NB * 4, jlo + 8)
                    kb.dma(sp, ve[:, 0:jhi - jlo, :], Ve[jlo:jhi].rearrange("j p n -> p j n"), W=[ve], owner=ve)
                    jho = min(NB * 4 - 1, jlo + 8)
                    kb.dma(sp, vo[:, 0:jho - jlo, :], Vo[jlo:jho].rearrange("j p n -> p j n"), W=[vo], owner=vo)
                    return ub, qb, kbt, ve, vo, klo, jlo
                nxt = loads(0)
                for b in range(NB):
                    ub, qb, kbt, ve, vo, klo, jlo = nxt
                    if b + 1 < NB:
                        nxt = loads(b + 1)
                    kb.dma(sp, qb[:], hm(Qd)[:, :, b * 512:(b + 1) * 512], W=[qb], owner=qb)
                    kb.dma(sp, hres_t[:], fm(H)[:, :, b * 512:(b + 1) * 512], W=[hres_t], owner=hres_t)
                    for c in range(4):
                        pc = nps()
                        for j in range(31):
                            dg = dgs.next()
                            wcol = vt[:, C_CW + c * 31 + j:C_CW + c * 31 + j + 1]
                            kb.op(pool, lambda e, dg=dg, wcol=wcol: e.tensor_scalar(
                                out=dg[:], in0=identb[:], scalar1=wcol, scalar2=0.0, op0=ALU.mult, op1=ALU.add),
                                R=[identb, vt], W=[dg])
                            kb.op(pe, lambda e, dg=dg, c=c, j=j, pc=pc: e.matmul(
                                pc[:], dg[:], ub[:, c, j:j + 512], start=(j == 0), stop=(j == 30)),
                                R=[dg, ub], W=[pc], mark=True)
                        kb.op(act, lambda e, c=c, pc=pc: e.activation(out=y[:, c, :], in_=pc[:], func=AF.Identity,
                                                                      bias=vt[:, C_CB + c:C_CB + c + 1], scale=1.0),
                              R=[pc, vt], W=[y])

                    def outf_conv(c):
                        kb.op(act, lambda e: e.activation(out=mixT[:, c, :], in_=y[:, c, :], func=AF.Silu,
                                                          scale=vt[:, C_CG + c:C_CG + c + 1],
                                                          bias=vt[:, C_CBB + c:C_CBB + c + 1]), R=[y, vt], W=[mixT])
                    ln_fm(ph, y, 4, ones512, None, None, wk, outf_conv)
                    def rowinfo(rr):
                        r = 8 * b + rr
                        if r < 4:
                            fk = 0
                        elif r > NROW - 4:
                            fk = NROW - 8
                        else:
                            fk = r - 4
                        e0 = fk - r + 7
                        vt_par = ve if fk % 2 == 0 else vo
                        j0 = (fk // 2 if fk % 2 == 0 else (fk - 1) // 2) - jlo
                        return fk, e0, vt_par, j0

                    def stageA(rr):
                        fk, e0, vt_par, j0 = rowinfo(rr)
                        out = []
                        for hp in range(4):
                            pss = nps()
                            for hh in range(2):
                                h = hp * 2 + hh
                                for t in range(4):
                                    ko = (fk + 2 * t - klo) * 64
                                    first = (hh == 0 and t == 0)
                                    last = (hh == 1 and t == 3)
                                    kb.op(pe, lambda e, h=h, ko=ko, hh=hh, t=t, pss=pss: e.matmul(
                                        pss[:, hh * 256 + t * 64:hh * 256 + (t + 1) * 64],
                                        kbt[0:64, h, ko:ko + 128], qb[0:64, h, rr * 64:(rr + 1) * 64],
                                        start=True, stop=True),
                                        R=[kbt, qb] if (first or last) else (), W=[pss], mark=last)
                            pex = pexs.next()
                            kb.op(act, lambda e, pex=pex, pss=pss: e.activation(
                                out=pex[:].rearrange("p h n -> p (h n)"), in_=pss[:], func=AF.Exp, scale=0.125),
                                R=[pss], W=[pex])
                            kb.op(dve, lambda e, pex=pex, hp=hp, e0=e0: e.tensor_tensor(
                                out=pex[:].rearrange("p h (t q) -> p h t q", t=4),
                                in0=pex[:].rearrange("p h (t q) -> p h t q", t=4),
                                in1=g2[:, hp * 2:(hp + 1) * 2, e0:e0 + 7:2, :], op=ALU.mult), R=[pex, g2], W=[pex])
                            out.append(pex)
                        return out

                    def stageB(rr, pxs):
                        fk, e0, vt_par, j0 = rowinfo(rr)
                        ar = arow.next()
                        rc = rcp.next()
                        for hp in range(4):
                            pex = pxs[hp]
                            for hh in range(2):
                                h = hp * 2 + hh
                                oc0 = (h // 4) * 512 + (h % 4) * 65
                                for t in range(4):
                                    first = (hh == 0 and t == 0)
                                    last = (hh == 1 and t == 3)
                                    kb.op(pe, lambda e, h=h, hh=hh, t=t, pex=pex, oc0=oc0: e.matmul(
                                        PSO[0:64, oc0:oc0 + 65],
                                        pex[:, hh, t * 64:(t + 1) * 64], vt_par[:, j0 + t, h * 65:(h + 1) * 65],
                                        start=(t == 0), stop=(t == 3)),
                                        R=[pex, vt_par] if (first or last) else (), W=[PSO], mark=last)
                        for hf in range(2):
                            pv = PSO[0:64, hf * 512:hf * 512 + 260].rearrange("p (h d) -> p h d", d=65)
                            kb.op(dve, lambda e, rc=rc, pv=pv, hf=hf: e.reciprocal(rc[:, hf * 4:(hf + 1) * 4], pv[:, :, 64]),
                                  R=[PSO], W=[rc])
                            kb.op(dve, lambda e, rc=rc, pv=pv, hf=hf, ar=ar: e.tensor_tensor(
                                out=ar[:, hf * 4:(hf + 1) * 4, :], in0=pv[:, :, 0:64],
                                in1=rc[:, hf * 4:(hf + 1) * 4].unsqueeze(2).to_broadcast([64, 4, 64]), op=ALU.mult),
                                R=[PSO, rc], W=[ar])
                        return ar

                    def stageC(rr, ar):
                        arf = ar[:].rearrange("p h d -> p (h d)")
                        for c in range(4):
                            kb.op(pe, lambda e, c=c, arf=arf: e.transpose(
                                PST[:, c * 64:(c + 1) * 64], arf[:, c * 128:(c + 1) * 128], identb[0:64, 0:64]),
                                R=[ar, identb] if c in (0, 3) else (), W=[PST], mark=(c == 3))
                        kb.op(act, lambda e, rr=rr: e.activation(
                            out=mixT[:, 4:8, rr * 64:(rr + 1) * 64], in_=PST[:].rearrange("p (c q) -> p c q", c=4),
                            func=AF.Copy), R=[PST], W=[mixT])

                    pxn = stageA(0)
                    prev = None
                    for rr in range(8):
                        pxc = pxn
                        if rr + 1 < 8:
                            pxn = stageA(rr + 1)
                        ar = stageB(rr, pxc)
                        if prev is not None:
                            stageC(*prev)
                        prev = (rr, ar)
                    stageC(*prev)
                    if b == 0:
                        dbg_store("mix", mixT[:].rearrange("p c t -> p (c t)"), mixT)
                    for oc in range(8):
                        p = nps()
                        mm_acc(p[:], p, [(wo[:, kc, oc * 128:(oc + 1) * 128], mixT[:, kc, :]) for kc in range(8)],
                               [wo, mixT])
                        kb.op(act, lambda e, p=p, oc=oc: e.activation(out=tmpo[:], in_=p[:], func=AF.Identity,
                                                                      bias=vt[:, C_BOUT + oc:C_BOUT + oc + 1], scale=1.0),
                              R=[p, vt], W=[tmpo])
                        kb.op(dve, lambda e, oc=oc: e.scalar_tensor_tensor(
                            out=tt[:, oc, :], in0=hres_t[:, oc, :], scalar=ALPHA, in1=tmpo[:], op0=ALU.mult, op1=ALU.add),
                            R=[hres_t, tmpo], W=[tt])

                    def outf1(c):
                        kb.op(act, lambda e: e.activation(out=tt[:, c, :], in_=tt[:, c, :], func=AF.Identity,
                                                          scale=vt[:, C_L1G + c:C_L1G + c + 1],
                                                          bias=vt[:, C_L1B + c:C_L1B + c + 1]), R=[tt, vt], W=[tt])
                    ln_fm(ph, tt, 8, ones1k, None, None, wk, outf1)
                    kb.dma(sp, fm(H)[:, :, b * 512:(b + 1) * 512], tt[:], R=[tt], owner=tt)
                kb.end_phase()

        def phase2(l, last_layer):
            moe = (l % 2 == 1)
            j = l // 2
            NF = (FFE if moe else FF) // 128
            with ExitStack() as ph:
                vt = load_vec(ph, l)
                wg = kb.sb(ph, "wg", [128, 8, D], BF16)
                wgv = ple_gate_w[l].rearrange("(c p) n -> p c n", p=128)
                for g in range(2):
                    load_w(wg, wg[:, :, g * 512:(g + 1) * 512], wgv[:, :, g * 512:(g + 1) * 512])
                wp = kb.sb(ph, "wp", [128, 2, D], BF16)
                load_w(wp, wp[:], ple_w[l].rearrange("(c p) n -> p c n", p=128))
                if moe:
                    wr = kb.sb(ph, "wr", [128, 8, NEXP], BF16)
                    with nc.allow_non_contiguous_dma(reason="tiny router weight"):
                        load_w(wr, wr[:], w_router[j].rearrange("(c p) n -> p c n", p=128))
                    lg = kb.sb(ph, "lg", [128, 4, 8], F32)
                    lg2 = kb.sb(ph, "lg2", [128, 4, 8], F32)
                    m1 = kb.sb(ph, "m1", [128, 4], F32)
                    m2 = kb.sb(ph, "m2", [128, 4], F32)
                    gat = kb.sb(ph, "gat", [128, 4, 8], F32)
                    gatb = kb.sb(ph, "gatb", [128, 4, 8, 128], BF16)
                    gsb = kb.sb(ph, "gsb", [128, 8, 512], BF16)
                    tmpu = Ring([kb.sb(ph, "tmpu%d" % i, [128, 512], F32) for i in range(2)])
                h1bs = Ring([kb.sb(ph, "h1b%d" % i, [128, 8, 512], BF16) for i in range(2)])
                pbs = Ring([kb.sb(ph, "pb%d" % i, [128, 2, 512], BF16) for i in range(2)])
                accR = Ring([kb.sb(ph, "acc%d" % i, [128, 8, 512], F32) for i in range(2)])
                w1s = Ring([kb.sb(ph, "w1s%d" % i, [128, 8, 512], BF16) for i in range(2)])
                w3s = Ring([kb.sb(ph, "w3s%d" % i, [128, 8, 512], BF16) for i in range(2)])
                w2s = Ring([kb.sb(ph, "w2s%d" % i, [128, 8, 512], BF16) for i in range(2)])
                actb = kb.sb(ph, "actb", [128, 28, 512], BF16)
                sgl = Ring([kb.sb(ph, "sgl%d" % i, [128, 512], F32) for i in range(2)])
                tbq = kb.sb(ph, "tbq", [128, 16, 512], BF16)
                wk = dict(tb=Tl(tbq.t[:, 0:8, :]), tsq=Tl(tbq.t[:, 8:16, :]),
                          mean=kb.sb(ph, "mean", [128, 512], F32), msq=kb.sb(ph, "msq", [128, 512], F32),
                          rstd=kb.sb(ph, "rstd", [128, 512], F32))
                Hf = fm(H)
                ACCS = [Tl(PSB.t[:, i * 512:(i + 1) * 512]) for i in range(4)]

                def loads(b):
                    h1b, pb, acc = h1bs.next(), pbs.next(), accR.next()
                    kb.dma(sp, acc[:], Hf[:, :, b * 512:(b + 1) * 512], W=[acc], owner=acc)
                    kb.dma(pool, h1b[:], Hf[:, :, b * 512:(b + 1) * 512], W=[h1b], owner=h1b)
                    kb.dma(pool, pb[:], pT[l].rearrange("(c p) t -> p c t", p=128)[:, :, b * 512:(b + 1) * 512],
                           W=[pb], owner=pb)
                    return h1b, pb, acc
                nxt = loads(0)

                def ffn_expert(h1b, w1d, w3d, w2d, gate_e, acc):
                    w1v = w1d.rearrange("(c p) n -> p c n", p=128)
                    w3v = w3d.rearrange("(c p) n -> p c n", p=128)
                    for fg in range((NF + 3) // 4):
                        a, c3 = w1s.next(), w3s.next()
                        ncf = min(4, NF - fg * 4)
                        load_w(a, a[:, :, 0:ncf * 128], w1v[:, :, fg * 512:fg * 512 + ncf * 128])
                        load_w(c3, c3[:, :, 0:ncf * 128], w3v[:, :, fg * 512:fg * 512 + ncf * 128])
                        for fc in range(ncf):
                            f = fg * 4 + fc
                            pg_ = nps()
                            mm_acc(pg_[:], pg_, [(a[:, kc, fc * 128:(fc + 1) * 128], h1b[:, kc, :]) for kc in range(8)],
                                   [a, h1b])
                            pu_ = nps()
                            mm_acc(pu_[:], pu_, [(c3[:, kc, fc * 128:(fc + 1) * 128], h1b[:, kc, :]) for kc in range(8)],
                                   [c3, h1b])
                            s_ = sgl.next()
                            kb.op(act, lambda e, s_=s_, pg_=pg_: e.activation(out=s_[:], in_=pg_[:], func=AF.Silu),
                                  R=[pg_], W=[s_])
                            if gate_e is None:
                                kb.op(dve, lambda e, s_=s_, pu_=pu_, f=f: e.tensor_tensor(
                                    out=actb[:, f, :], in0=pu_[:], in1=s_[:], op=ALU.mult), R=[pu_, s_], W=[actb])
                            else:
                                tu = tmpu.next()
                                kb.op(dve, lambda e, tu=tu, pu_=pu_: e.tensor_tensor(
                                    out=tu[:], in0=pu_[:], in1=gsb[:, gate_e, :], op=ALU.mult), R=[pu_, gsb], W=[tu])
                                kb.op(pool, lambda e, tu=tu, s_=s_, f=f: e.tensor_tensor(
                                    out=actb[:, f, :], in0=tu[:], in1=s_[:], op=ALU.mult), R=[tu, s_], W=[actb])
                    ngrp = (NF + 7) // 8
                    for half in range(2):
                        accs = ACCS
                        for fg in range(ngrp):
                            nfc = min(8, NF - fg * 8)
                            w2t = w2s.next()
                            load_w(w2t, w2t[:, 0:nfc, :],
                                   w2d[fg * 1024:fg * 1024 + nfc * 128, half * 512:(half + 1) * 512].rearrange(
                                       "(c p) n -> p c n", p=128))
                            for o4 in range(4):
                                for fc in range(nfc):
                                    first = (fg == 0 and fc == 0)
                                    lastm = (fg == ngrp - 1 and fc == nfc - 1)
                                    lastg = (fc == nfc - 1)
                                    kb.op(pe, lambda e, o4=o4, fc=fc, fg=fg, first=first, lastm=lastm, w2t=w2t: e.matmul(
                                        accs[o4][:], w2t[:, fc, o4 * 128:(o4 + 1) * 128], actb[:, fg * 8 + fc, :],
                                        start=first, stop=lastm),
                                        R=[w2t, actb] if (fc == 0 or lastg) else (), W=[accs[o4]], mark=lastg)
                        for o4 in range(4):
                            oc = half * 4 + o4
                            kb.op(dve, lambda e, o4=o4, oc=oc, acc=acc: e.tensor_tensor(
                                out=acc[:, oc, :], in0=accs[o4][:], in1=acc[:, oc, :], op=ALU.add),
                                R=[accs[o4], acc], W=[acc])

                for b in range(NB):
                    h1b, pb, acc = nxt
                    if b + 1 < NB:
                        nxt = loads(b + 1)
                    for oc in range(8):
                        pg_ = nps()
                        mm_acc(pg_[:], pg_, [(wg[:, kc, oc * 128:(oc + 1) * 128], h1b[:, kc, :]) for kc in range(8)],
                               [wg, h1b])
                        pp_ = nps()
                        mm_acc(pp_[:], pp_, [(wp[:, kc, oc * 128:(oc + 1) * 128], pb[:, kc, :]) for kc in range(2)],
                               [wp, pb])
                        s_ = sgl.next()
                        kb.op(act, lambda e, s_=s_, pg_=pg_, oc=oc: e.activation(
                            out=s_[:], in_=pg_[:], func=AF.Sigmoid, bias=vt[:, C_PGB + oc:C_PGB + oc + 1], scale=1.0),
                            R=[pg_, vt], W=[s_])
                        kb.op(dve, lambda e, s_=s_, pp_=pp_: e.tensor_tensor(out=s_[:], in0=pp_[:], in1=s_[:], op=ALU.mult),
                              R=[pp_, s_], W=[s_])
                        kb.op(dve, lambda e, s_=s_, oc=oc, acc=acc: e.scalar_tensor_tensor(
                            out=acc[:, oc, :], in0=acc[:, oc, :], scalar=ALPHA, in1=s_[:], op0=ALU.mult, op1=ALU.add),
                            R=[acc, s_], W=[acc])
                    if not moe:
                        ffn_expert(h1b, ffn_w1[j], ffn_w3[j], ffn_w2[j], None, acc)
                    else:
                        for jl in range(4):
                            pr = nps()
                            mm_acc(pr[:, 0:8], pr, [(h1b[:, kc, jl * 128:(jl + 1) * 128], wr[:, kc, :]) for kc in range(8)],
                                   [wr, h1b])
                            kb.op(act, lambda e, pr=pr, jl=jl: e.activation(out=lg[:, jl, :], in_=pr[:, 0:8], func=AF.Copy),
                                  R=[pr], W=[lg])
                        kb.op(dve, lambda e: e.tensor_reduce(out=m1[:], in_=lg[:], axis=AX.X, op=ALU.max), R=[lg], W=[m1])
                        kb.op(dve, lambda e: e.tensor_tensor(out=lg2[:], in0=lg[:],
                                                             in1=m1[:].unsqueeze(2).to_broadcast([128, 4, 8]),
                                                             op=ALU.is_equal), R=[lg, m1], W=[lg2])
                        kb.op(dve, lambda e: e.scalar_tensor_tensor(out=lg2[:], in0=lg2[:], scalar=-1e9, in1=lg[:],
                                                                    op0=ALU.mult, op1=ALU.add), R=[lg, lg2], W=[lg2])
                        kb.op(dve, lambda e: e.tensor_reduce(out=m2[:], in_=lg2[:], axis=AX.X, op=ALU.max), R=[lg2], W=[m2])
                        kb.op(dve, lambda e: e.tensor_tensor(out=lg2[:], in0=lg[:],
                                                             in1=m2[:].unsqueeze(2).to_broadcast([128, 4, 8]),
                                                             op=ALU.is_ge), R=[lg, m2], W=[lg2])
                        kb.op(dve, lambda e: e.tensor_tensor(out=gat[:], in0=lg[:],
                                                             in1=m1[:].unsqueeze(2).to_broadcast([128, 4, 8]),
                                                             op=ALU.subtract), R=[lg, m1], W=[gat])
                        kb.op(act, lambda e: e.activation(out=gat[:], in_=gat[:], func=AF.Exp), R=[gat], W=[gat])
                        kb.op(dve, lambda e: e.tensor_tensor(out=gat[:], in0=gat[:], in1=lg2[:], op=ALU.mult),
                              R=[gat, lg2], W=[gat])
                        kb.op(dve, lambda e: e.tensor_reduce(out=m2[:], in_=gat[:], axis=AX.X, op=ALU.add), R=[gat], W=[m2])
                        kb.op(dve, lambda e: e.reciprocal(m1[:], m2[:]), R=[m2], W=[m1])
                        kb.op(dve, lambda e: e.tensor_tensor(out=gat[:], in0=gat[:],
                                                             in1=m1[:].unsqueeze(2).to_broadcast([128, 4, 8]),
                                                             op=ALU.mult), R=[gat, m1], W=[gat])
                        if b == 0:
                            dbg_store("gat", gat[:].rearrange("p a b -> p (a b)"), gat)
                        kb.op(dve, lambda e: e.tensor_copy(gatb[:], gat[:].unsqueeze(3).to_broadcast([128, 4, 8, 128])),
                              R=[gat], W=[gatb])
                        for ex in range(NEXP):
                            pgt = nps()
                            for jl in range(4):
                                kb.op(pe, lambda e, jl=jl, ex=ex, pgt=pgt: e.matmul(
                                    pgt[:, jl * 128:(jl + 1) * 128], gatb[:, jl, ex, :], identb[:], start=True, stop=True),
                                    R=[gatb, identb] if jl in (0, 3) else (), W=[pgt], mark=(jl == 3))
                            kb.op(act, lambda e, ex=ex, pgt=pgt: e.activation(out=gsb[:, ex, :], in_=pgt[:], func=AF.Copy),
                                  R=[pgt], W=[gsb])
                        for ex in range(NEXP):
                            ffn_expert(h1b, moe_w1[j, ex], moe_w3[j, ex], moe_w2[j, ex], ex, acc)

                    def outf2(c, acc=acc):
                        kb.op(act, lambda e: e.activation(out=acc[:, c, :], in_=acc[:, c, :], func=AF.Identity,
                                                          scale=vt[:, C_L2G + c:C_L2G + c + 1],
                                                          bias=vt[:, C_L2B + c:C_L2B + c + 1]), R=[acc, vt], W=[acc])
                    wk["tb"].k = tbq.k
                    wk["tsq"].k = tbq.k
                    ln_fm(ph, acc, 8, ones1k, None, None, wk, outf2)
                    dst = OUT if last_layer else H
                    kb.dma(sp, fm(dst)[:, :, b * 512:(b + 1) * 512], acc[:], R=[acc], owner=acc)
                kb.end_phase()

        I32 = mybir.dt.int32
        CB = 192
        NS = NB * CB
        H1T = dram("H1T", [NB * 4, 128, D], BF16, "Internal")
        YD = dram("YD", [NEXP, NS, D], BF16, "Internal")
        iota_p128 = kb.sb(es, "iota_p128", [128, 1], F32)
        kb.op(dve, lambda e: e.tensor_scalar(out=iota_p128[:], in0=iota_p[:], scalar1=128.0, scalar2=None, op0=ALU.add),
              R=[iota_p], W=[iota_p128])
        iota_p128.k.const = True

        def moe_layer(l, last_layer):
            j = l // 2
            NF = FFE // 128
            NTL = NB * 4
            with ExitStack() as lay:
                posall = kb.sb(lay, "posall", [128, NTL, 8], F32)
                gatall = kb.sb(lay, "gatall", [128, NTL, 8], F32)
                kb.phase_tiles = []
                with ExitStack() as ph:
                    vt = load_vec(ph, l)
                    wg = kb.sb(ph, "wg", [128, 8, D], BF16)
                    wgv = ple_gate_w[l].rearrange("(c p) n -> p c n", p=128)
                    for g in range(2):
                        load_w(wg, wg[:, :, g * 512:(g + 1) * 512], wgv[:, :, g * 512:(g + 1) * 512])
                    wp = kb.sb(ph, "wp", [128, 2, D], BF16)
                    load_w(wp, wp[:], ple_w[l].rearrange("(c p) n -> p c n", p=128))
                    wr = kb.sb(ph, "wr", [128, 8, NEXP], BF16)
                    load_w(wr, wr[:], w_router[j].rearrange("(c p) n -> p c n", p=128))
                    for t_ in (wg, wp, wr):
                        t_.k.const = True
                    lg = kb.sb(ph, "lg", [128, 4, 8], F32)
                    lg2 = kb.sb(ph, "lg2", [128, 4, 8], F32)
                    m1 = kb.sb(ph, "m1", [128, 4], F32)
                    m2 = kb.sb(ph, "m2", [128, 4], F32)
                    gat = kb.sb(ph, "gat", [128, 4, 8], F32)
                    selb = kb.sb(ph, "selb", [128, 4, 8], BF16)
                    posf = kb.sb(ph, "posf", [128, 4, 8], F32)
                    msk = kb.sb(ph, "msk", [128, 4, 8], F32)
                    h1bs = Ring([kb.sb(ph, "h1b%d" % i, [128, 8, 512], BF16) for i in range(2)])
                    pbs = Ring([kb.sb(ph, "pb%d" % i, [128, 2, 512], BF16) for i in range(2)])
                    accs_ = Ring([kb.sb(ph, "accA%d" % i, [128, 8, 512], F32) for i in range(2)])
                    sgl = Ring([kb.sb(ph, "sgl%d" % i, [128, 512], F32) for i in range(2)])
                    h1tm = Ring([kb.sb(ph, "h1tm%d" % i, [128, 4, D], BF16) for i in range(2)])
                    PTR = [Tl(PSB.t[:, i * 512:(i + 1) * 512].bitcast(BF16)) for i in range(4)]
                    Hf = fm(H)

                    def loadsA(b):
                        h1b, pb, acc = h1bs.next(), pbs.next(), accs_.next()
                        kb.dma(pool, h1b[:], Hf[:, :, b * 512:(b + 1) * 512], W=[h1b], owner=h1b)
                        kb.dma(pool, pb[:], pT[l].rearrange("(c p) t -> p c t", p=128)[:, :, b * 512:(b + 1) * 512],
                               W=[pb], owner=pb)
                        kb.dma(sp, acc[:], Hf[:, :, b * 512:(b + 1) * 512], W=[acc], owner=acc)
                        return h1b, pb, acc
                    nxt = loadsA(0)
                    for b in range(NB):
                        h1b, pb, acc = nxt
                        if b + 1 < NB:
                            nxt = loadsA(b + 1)
                        for oc in range(8):
                            pg_ = nps()
                            mm_acc(pg_[:], pg_, [(wg[:, kc, oc * 128:(oc + 1) * 128], h1b[:, kc, :]) for kc in range(8)],
                                   [wg, h1b])
                            pp_ = nps()
                            mm_acc(pp_[:], pp_, [(wp[:, kc, oc * 128:(oc + 1) * 128], pb[:, kc, :]) for kc in range(2)],
                                   [wp, pb])
                            s_ = sgl.next()
                            kb.op(act, lambda e, s_=s_, pg_=pg_, oc=oc: e.activation(
                                out=s_[:], in_=pg_[:], func=AF.Sigmoid, bias=vt[:, C_PGB + oc:C_PGB + oc + 1], scale=1.0),
                                R=[pg_, vt], W=[s_])
                            kb.op(dve, lambda e, s_=s_, pp_=pp_: e.tensor_tensor(out=s_[:], in0=pp_[:], in1=s_[:], op=ALU.mult),
                                  R=[pp_, s_], W=[s_])
                            kb.op(dve, lambda e, s_=s_, oc=oc, acc=acc: e.scalar_tensor_tensor(
                                out=acc[:, oc, :], in0=acc[:, oc, :], scalar=ALPHA, in1=s_[:], op0=ALU.mult, op1=ALU.add),
                                R=[acc, s_], W=[acc])
                        kb.dma(sp, Hf[:, :, b * 512:(b + 1) * 512], acc[:], R=[acc], owner=acc)
                        ht = h1tm.next()
                        for jl in range(4):
                            ptr = PTR[jl]
                            for c in range(8):
                                kb.op(pe, lambda e, jl=jl, c=c, ptr=ptr: e.transpose(
                                    ptr[:, c * 128:(c + 1) * 128], h1b[:, c, jl * 128:(jl + 1) * 128], identb[:]),
                                    R=[h1b, identb] if c in (0, 7) else (), W=[ptr], mark=(c == 7))
                            kb.op(act if jl % 2 == 0 else dve,
                                  (lambda e, jl=jl, ptr=ptr, ht=ht: e.activation(out=ht[:, jl, :], in_=ptr[:], func=AF.Copy))
                                  if jl % 2 == 0 else
                                  (lambda e, jl=jl, ptr=ptr, ht=ht: e.tensor_copy(ht[:, jl, :], ptr[:])),
                                  R=[ptr], W=[ht])
                        kb.dma(sp, H1T[b * 4:(b + 1) * 4].rearrange("j p n -> p j n"), ht[:], R=[ht], owner=ht)
                        for jl in range(4):
                            pr = nps()
                            mm_acc(pr[:, 0:8], pr, [(h1b[:, kc, jl * 128:(jl + 1) * 128], wr[:, kc, :]) for kc in range(8)],
                                   [wr, h1b])
                            kb.op(act, lambda e, pr=pr, jl=jl: e.activation(out=lg[:, jl, :], in_=pr[:, 0:8], func=AF.Copy),
                                  R=[pr], W=[lg])
                        bc = lambda t_: t_[:].unsqueeze(2).to_broadcast([128, 4, 8])
                        kb.op(dve, lambda e: e.tensor_reduce(out=m1[:], in_=lg[:], axis=AX.X, op=ALU.max), R=[lg], W=[m1])
                        kb.op(dve, lambda e: e.tensor_tensor(out=lg2[:], in0=lg[:], in1=bc(m1), op=ALU.is_equal),
                              R=[lg, m1], W=[lg2])
                        kb.op(dve, lambda e: e.scalar_tensor_tensor(out=lg2[:], in0=lg2[:], scalar=-1e9, in1=lg[:],
                                                                    op0=ALU.mult, op1=ALU.add), R=[lg, lg2], W=[lg2])
                        kb.op(dve, lambda e: e.tensor_reduce(out=m2[:], in_=lg2[:], axis=AX.X, op=ALU.max), R=[lg2], W=[m2])
                        kb.op(dve, lambda e: e.tensor_tensor(out=lg2[:], in0=lg[:], in1=bc(m2), op=ALU.is_ge),
                              R=[lg, m2], W=[lg2])
                        kb.op(dve, lambda e: e.tensor_tensor(out=gat[:], in0=lg[:], in1=bc(m1), op=ALU.subtract),
                              R=[lg, m1], W=[gat])
                        kb.op(act, lambda e: e.activation(out=gat[:], in_=gat[:], func=AF.Exp), R=[gat], W=[gat])
                        kb.op(dve, lambda e: e.tensor_tensor(out=gat[:], in0=gat[:], in1=lg2[:], op=ALU.mult),
                              R=[gat, lg2], W=[gat])
                        kb.op(dve, lambda e: e.tensor_reduce(out=m2[:], in_=gat[:], axis=AX.X, op=ALU.add), R=[gat], W=[m2])
                        kb.op(dve, lambda e: e.reciprocal(m1[:], m2[:]), R=[m2], W=[m1])
                        kb.op(dve, lambda e, b=b: e.tensor_tensor(out=gatall[:, b * 4:(b + 1) * 4, :], in0=gat[:], in1=bc(m1),
                                                                  op=ALU.mult), R=[gat, m1], W=[gatall])
                        if b == 0:
                            dbg_store("gat", gatall[:, 0:4, :].rearrange("p a b -> p (a b)"), gatall)
                        kb.op(dve, lambda e: e.tensor_copy(selb[:], lg2[:]), R=[lg2], W=[selb])
                        pp = nps()
                        for jl in range(4):
                            prs = [(UT[:], selb[:, jl, :])] + [(ones1[:], selb[:, j2, :]) for j2 in range(jl)]
                            mm_acc(pp[:, jl * 8:(jl + 1) * 8], pp, prs, [UT, ones1, selb])
                        kb.op(dve, lambda e, pp=pp: e.tensor_scalar(
                            out=msk[:], in0=pp[:, 0:32].rearrange("p (a b) -> p a b", a=4), scalar1=float(CB), scalar2=None,
                            op0=ALU.is_lt), R=[pp], W=[msk])
                        kb.op(dve, lambda e: e.tensor_tensor(out=msk[:], in0=msk[:], in1=lg2[:], op=ALU.mult),
                              R=[msk, lg2], W=[msk])
                        kb.op(dve, lambda e, pp=pp: e.scalar_tensor_tensor(
                            out=posf[:], in0=pp[:, 0:32].rearrange("p (a b) -> p a b", a=4), scalar=1.0, in1=msk[:],
                            op0=ALU.add, op1=ALU.mult), R=[pp, msk], W=[posf])
                        kb.op(dve, lambda e, b=b: e.tensor_scalar(out=posall[:, b * 4:(b + 1) * 4, :], in0=posf[:],
                                                                  scalar1=-1.0, scalar2=None, op0=ALU.add),
                              R=[posf], W=[posall])
                    dbg_store("pos", posall[:].rearrange("p a b -> p (a b)"), posall)
                    kb.end_phase()
                if os.environ.get("K_MOE_STOP") == "A":
                    return
                with ExitStack() as ph:
                    xg = kb.sb(ph, "xg", [128, 8, NS], BF16)
                    actb = kb.sb(ph, "actb", [128, NF, NS], BF16)
                    wsr = Ring([kb.sb(ph, "ws%d" % i, [128, 8, 512], BF16) for i in range(5)])
                    w1s = w3s = w2s = wsr
                    pts = Ring([kb.sb(ph, "pt%d" % i, [128, CB], BF16) for i in range(4)])
                    hts = Ring([kb.sb(ph, "ht%d" % i, [128, 4, D], BF16) for i in range(2)])
                    sgl = Ring([kb.sb(ph, "sgl%d" % i, [128, 512], F32) for i in range(2)])
                    yst = Ring([kb.sb(ph, "yst%d" % i, [128, 512], BF16) for i in range(4)])
                    ACC4 = [Tl(PSB.t[:, i * 512:(i + 1) * 512]) for i in range(4)]
                    ACC8 = PS + ACC4
                    nblk = [(s0, min(512, NS - s0)) for s0 in range(0, NS, 512)]
                    ntile = NS // 128
                    for ex in range(NEXP):
                        def load_ht(b_):
                            ht_ = hts.next()
                            kb.dma(sp, ht_[:], H1T[b_ * 4:(b_ + 1) * 4].rearrange("j p n -> p j n"), W=[ht_], owner=ht_)
                            return ht_
                        htn = load_ht(0)
                        for b in range(NB):
                            htb = htn
                            if b + 1 < NB:
                                htn = load_ht(b + 1)
                            tls = []
                            for t4 in range(4):
                                tl = b * 4 + t4
                                pt_ = pts.next()
                                kb.op(dve, lambda e, pt_=pt_, tl=tl, ex=ex: e.tensor_scalar(
                                    out=pt_[:], in0=iota_f[:, 0:CB], scalar1=posall[:, tl, ex:ex + 1], scalar2=None,
                                    op0=ALU.is_equal), R=[iota_f, posall], W=[pt_])
                                tls.append((pt_, t4))
                            bset = ACC4 if b % 2 == 0 else PS
                            for c in range(8):
                                bk = bset[c // 2]
                                mm_acc(bk[:, (c % 2) * 256:(c % 2) * 256 + CB], bk,
                                       [(htb[:, t4_, c * 128:(c + 1) * 128], pt_[:]) for (pt_, t4_) in tls],
                                       [htb] + [pt_ for (pt_, t4_) in tls])
                            for k_ in range(4):
                                bk = bset[k_]
                                gv = bk[:, :].rearrange("p (c w) -> p c w", w=256)[:, :, 0:CB]
                                kb.op(act if k_ % 2 == 0 else dve,
                                      (lambda e, b=b, k_=k_, gv=gv: e.activation(
                                          out=xg[:, 2 * k_:2 * k_ + 2, b * CB:(b + 1) * CB], in_=gv, func=AF.Copy))
                                      if k_ % 2 == 0 else
                                      (lambda e, b=b, k_=k_, gv=gv: e.tensor_copy(
                                          xg[:, 2 * k_:2 * k_ + 2, b * CB:(b + 1) * CB], gv)),
                                      R=[bk], W=[xg])
                        w1v = moe_w1[j, ex].rearrange("(c p) n -> p c n", p=128)
                        w3v = moe_w3[j, ex].rearrange("(c p) n -> p c n", p=128)
                        for fg in range(NF // 4):
                            a, c3 = w1s.next(), w3s.next()
                            load_w(a, a[:], w1v[:, :, fg * 512:(fg + 1) * 512])
                            load_w(c3, c3[:], w3v[:, :, fg * 512:(fg + 1) * 512])
                            for (s0, n) in nblk:
                                xs = slice(s0, s0 + n)
                                for fc in range(4):
                                    f = fg * 4 + fc
                                    pg_ = nps()
                                    mm_acc(pg_[:, 0:n], pg_, [(a[:, kc, fc * 128:(fc + 1) * 128], xg[:, kc, xs]) for kc in range(8)],
                                           [a, xg])
                                    pu_ = nps()
                                    mm_acc(pu_[:, 0:n], pu_, [(c3[:, kc, fc * 128:(fc + 1) * 128], xg[:, kc, xs]) for kc in range(8)],
                                           [c3, xg])
                                    s_ = sgl.next()
                                    kb.op(act, lambda e, s_=s_, pg_=pg_, n=n: e.activation(out=s_[:, 0:n], in_=pg_[:, 0:n], func=AF.Silu),
                                          R=[pg_], W=[s_])
                                    kb.op(dve, lambda e, s_=s_, pu_=pu_, f=f, xs=xs, n=n: e.tensor_tensor(
                                        out=actb[:, f, xs], in0=pu_[:, 0:n], in1=s_[:, 0:n], op=ALU.mult), R=[pu_, s_], W=[actb])
                        ngrp = (NF + 7) // 8
                        for half in range(2):
                            for t0 in range(0, ntile, 8):
                                tiles = list(range(t0, min(ntile, t0 + 8)))
                                for fg in range(ngrp):
                                    nfc = min(8, NF - fg * 8)
                                    w2t = w2s.next()
                                    load_w(w2t, w2t[:, 0:nfc, :],
                                           moe_w2[j, ex][fg * 1024:fg * 1024 + nfc * 128, half * 512:(half + 1) * 512].rearrange(
                                               "(c p) n -> p c n", p=128))
                                    for st in tiles:
                                        ac = ACC8[st - t0]
                                        for fc in range(nfc):
                                            first = (fg == 0 and fc == 0)
                                            lastm = (fg == ngrp - 1 and fc == nfc - 1)
                                            lastg = (fc == nfc - 1)
                                            kb.op(pe, lambda e, st=st, fc=fc, fg=fg, first=first, lastm=lastm, w2t=w2t, ac=ac:
                                                  e.matmul(ac[:], actb[:, fg * 8 + fc, st * 128:(st + 1) * 128], w2t[:, fc, :],
                                                           start=first, stop=lastm),
                                                  R=[w2t, actb] if (fc == 0 or lastg) else (), W=[ac], mark=lastg)
                                for st in tiles:
                                    ac = ACC8[st - t0]
                                    yt = yst.next()
                                    kb.op(act if st % 2 == 0 else dve,
                                          (lambda e, ac=ac, yt=yt: e.activation(out=yt[:], in_=ac[:], func=AF.Copy))
                                          if st % 2 == 0 else
                                          (lambda e, ac=ac, yt=yt: e.tensor_copy(yt[:], ac[:])),
                                          R=[ac], W=[yt])
                                    kb.dma(sp, YD[ex][st * 128:(st + 1) * 128, half * 512:(half + 1) * 512], yt[:],
                                           R=[yt], owner=yt)
                    kb.end_phase()
                if os.environ.get("K_MOE_STOP") == "B":
                    return
                with ExitStack() as ph:
                    vt = load_vec(ph, l)
                    acc = kb.sb(ph, "acc", [128, 8, 512], F32)
                    brdR = Ring([kb.sb(ph, "brd%d" % i, [128, 2, 4, 8, 128], BF16) for i in range(2)])
                    gsbR = Ring([kb.sb(ph, "gsb%d" % i, [128, 512], F32) for i in range(3)])
                    pgaR = Ring([kb.sb(ph, "pga%d" % i, [128, 8, 512], BF16) for i in range(2)])
                    pgbR = Ring([kb.sb(ph, "pgb%d" % i, [64, 8, 512], BF16) for i in range(2)])
                    yas = Ring([kb.sb(ph, "ya%d" % i, [128, 8, D], BF16) for i in range(2)])
                    ybs = Ring([kb.sb(ph, "yb%d" % i, [64, 8, D], BF16) for i in range(2)])
                    tbq = kb.sb(ph, "tbq", [128, 16, 512], BF16)
                    wk = dict(tb=Tl(tbq.t[:, 0:8, :]), tsq=Tl(tbq.t[:, 8:16, :]),
                              mean=kb.sb(ph, "mean", [128, 512], F32), msq=kb.sb(ph, "msq", [128, 512], F32),
                              rstd=kb.sb(ph, "rstd", [128, 512], F32))
                    wk["tb"].k = tbq.k
                    wk["tsq"].k = tbq.k
                    ACCS = [Tl(PSB.t[:, i * 512:(i + 1) * 512]) for i in range(4)]
                    Hf = fm(H)

                    def loadsC(b):
                        ya, yb_ = yas.next(), ybs.next()
                        for ex in range(NEXP):
                            kb.dma(sp, ya[:, ex, :], YD[ex][b * CB:b * CB + 128, :], W=[ya], owner=ya)
                            kb.dma(sp, yb_[:, ex, :], YD[ex][b * CB + 128:b * CB + CB, :], W=[yb_], owner=yb_)
                        return ya, yb_
                    nxt = loadsC(0)
                    for b in range(NB):
                        ya, yb_ = nxt
                        if b + 1 < NB:
                            nxt = loadsC(b + 1)
                        kb.dma(sp, acc[:], Hf[:, :, b * 512:(b + 1) * 512], W=[acc], owner=acc)
                        brd, pga, pgb = brdR.next(), pgaR.next(), pgbR.next()
                        for qi, src in enumerate((posall, gatall)):
                            kb.op(dve if qi == 0 else pool, lambda e, qi=qi, src=src, b=b, brd=brd: e.tensor_copy(
                                brd[:, qi, :, :, :], src[:, b * 4:(b + 1) * 4, :].unsqueeze(3).to_broadcast([128, 4, 8, 128])),
                                R=[src], W=[brd])
                        for ex in range(NEXP):
                            pbs_ = []
                            for qi in range(2):
                                pq = nps()
                                for jl in range(4):
                                    kb.op(pe, lambda e, jl=jl, ex=ex, qi=qi, pq=pq, brd=brd: e.matmul(
                                        pq[:, jl * 128:(jl + 1) * 128], brd[:, qi, jl, ex, :], identb[:], start=True, stop=True),
                                        R=[brd, identb] if jl in (0, 3) else (), W=[pq], mark=(jl == 3))
                                pbs_.append(pq)
                            gsb = gsbR.next()
                            kb.op(act, lambda e, pq=pbs_[1], gsb=gsb: e.activation(out=gsb[:], in_=pq[:], func=AF.Copy),
                                  R=[pbs_[1]], W=[gsb])
                            kb.op(dve, lambda e, ex=ex, pq=pbs_[0], gsb=gsb, pga=pga: e.scalar_tensor_tensor(
                                out=pga[:, ex, :], in0=pq[:], scalar=iota_p[:, 0:1], in1=gsb[:], op0=ALU.is_equal, op1=ALU.mult),
                                R=[pbs_[0], iota_p, gsb], W=[pga])
                            kb.op(dve, lambda e, ex=ex, pq=pbs_[0], gsb=gsb, pgb=pgb: e.scalar_tensor_tensor(
                                out=pgb[:, ex, :], in0=pq[0:64, :], scalar=iota_p128[0:64, 0:1], in1=gsb[0:64, :],
                                op0=ALU.is_equal, op1=ALU.mult), R=[pbs_[0], iota_p128, gsb], W=[pgb])
                        for half in range(2):
                            for o4 in range(4):
                                oc = half * 4 + o4
                                prs = []
                                for ex in range(NEXP):
                                    prs.append((ya[:, ex, oc * 128:(oc + 1) * 128], pga[:, ex, :]))
                                    prs.append((yb_[0:64, ex, oc * 128:(oc + 1) * 128], pgb[0:64, ex, :]))
                                mm_acc(ACCS[o4][:], ACCS[o4], prs, [ya, yb_, pga, pgb])
                                kb.op(dve, lambda e, o4=o4, oc=oc: e.tensor_tensor(
                                    out=acc[:, oc, :], in0=ACCS[o4][:], in1=acc[:, oc, :], op=ALU.add),
                                    R=[ACCS[o4], acc], W=[acc])

                        def outf2(c):
                            kb.op(act, lambda e: e.activation(out=acc[:, c, :], in_=acc[:, c, :], func=AF.Identity,
                                                              scale=vt[:, C_L2G + c:C_L2G + c + 1],
                                                              bias=vt[:, C_L2B + c:C_L2B + c + 1]), R=[acc, vt], W=[acc])
                        ln_fm(ph, acc, 8, ones1k, None, None, wk, outf2)
                        dst = OUT if last_layer else H
                        kb.dma(sp, fm(dst)[:, :, b * 512:(b + 1) * 512], acc[:], R=[acc], owner=acc)
                    kb.end_phase()

        if phases is None:
            phases = ["0"] + [x for l in layers for x in ("1a%d" % l, "1b%d" % l, "2%d" % l)]
        for phn in phases:
            if phn == "0":
                phase0()
            elif phn.startswith("1a"):
                phase1a(int(phn[2:]))
            elif phn.startswith("1b"):
                phase1b(int(phn[2:]))
            else:
                l_ = int(phn[1:])
                if l_ % 2 == 1 and not os.environ.get("K_DENSE_MOE"):
                    moe_layer(l_, l_ == layers[-1])
                else:
                    phase2(l_, l_ == layers[-1])
        kb.barrier()
    return nc


def _cols(v, C):
    return np.ascontiguousarray(v.reshape(C, 128).T)


def _prep_shared(inp):
    f32 = np.float32
    vecs = np.zeros((DEPTH, 128, NV), f32)
    vb = np.zeros((DEPTH, 128, 512), f32)
    g2 = np.full((DEPTH, 128, 8, 14, 64), MASKV, f32)
    kc = np.arange(64)[:, None]
    qc = np.arange(64)[None, :]
    win0 = np.clip(qc - 8, 0, 48)
    colvalid = (kc >= win0) & (kc < win0 + 16)
    dc = np.clip(kc - qc + 15, 0, 30)
    for l in range(DEPTH):
        v = vecs[l]
        v[:, C_BIN:C_BIN + 20] = _cols(inp["b_in"][l], 20)
        v[:, C_CW:C_CW + 124] = inp["conv_w"][l].T.reshape(4, 128, 31).transpose(1, 0, 2).reshape(128, 124)
        v[:, C_CB:C_CB + 4] = _cols(inp["conv_b"][l], 4)
        v[:, C_CG:C_CG + 4] = _cols(inp["conv_ln_g"][l], 4)
        v[:, C_CBB:C_CBB + 4] = _cols(inp["conv_ln_b"][l], 4)
        v[:, C_BOUT:C_BOUT + 8] = _cols(inp["b_out"][l], 8)
        v[:, C_L1G:C_L1G + 8] = _cols(inp["ln1_g"][l], 8)
        v[:, C_L1B:C_L1B + 8] = _cols(inp["ln1_b"][l], 8)
        v[:, C_PGB:C_PGB + 8] = _cols(inp["ple_gate_b"][l], 8)
        v[:, C_L2G:C_L2G + 8] = _cols(inp["ln2_g"][l], 8)
        v[:, C_L2B:C_L2B + 8] = _cols(inp["ln2_b"][l], 8)
        v[:, C_LIG:C_LIG + 8] = _cols(inp["ln_in_g"], 8)
        v[:, C_LIB:C_LIB + 8] = _cols(inp["ln_in_b"], 8)
        vb[l] = np.broadcast_to(inp["b_in"][l][2048:2560][None, :], (128, 512))
        rpb = inp["rpb"][l]
        for kr in range(2):
            for ei in range(14):
                dr = ei - 7 + kr
                if dr < -7 or dr > 7:
                    continue
                tab = rpb[:, dr + 7, :][:, dc]
                tab = np.where(colvalid[None], tab, f32(MASKV))
                g2[l, kr * 64:(kr + 1) * 64, :, ei, :] = tab.transpose(1, 0, 2)
    return vecs, vb, g2.reshape(DEPTH, 128, 8 * 14 * 64)


_NC_CACHE = {}


def kernel(**inp):
    inp = {k: np.asarray(v) for k, v in inp.items()}
    NB = 10
    NT = NB * 512
    key = ("full",)
    if key not in _NC_CACHE:
        _NC_CACHE[key] = build_nc(NB, (0, 1, 2, 3))
    nc = _NC_CACHE[key]
    vecs, vb, g2 = _prep_shared(inp)
    shared = dict(vecs=vecs, vbias=vb, g2h=g2)
    for n in ("w_in", "w_out", "ffn_w1", "ffn_w3", "ffn_w2", "w_router", "moe_w1", "moe_w3", "moe_w2", "ple_w",
              "ple_gate_w"):
        shared[n] = np.ascontiguousarray(inp[n], dtype=np.float32)
    in_maps = []
    for c in range(8):
        b, half = c // 2, c % 2
        t0 = 0 if half == 0 else 48 * 64
        m = dict(shared)
        m["xT"] = np.ascontiguousarray(inp["x"][b, t0:t0 + NT, :].T)
        m["pT"] = np.ascontiguousarray(inp["p"][:, b, t0:t0 + NT, :].transpose(0, 2, 1))
        in_maps.append(m)
    res = run_bass_kernel_spmd(nc, in_maps, core_ids=list(range(8)))
    out = np.empty((4, 8192, D), np.float32)
    for c in range(8):
        b, half = c // 2, c % 2
        o = res.results[c]["outT"]
        if half == 0:
            out[b, 0:4096, :] = o[:, 0:4096].T
        else:
            out[b, 4096:8192, :] = o[:, 1024:5120].T
    return out
```
